# Optimizing a Trainium2 kernel written in Bass

```python
import math
import jax, jax.numpy as jnp
from jax import lax
import numpy as np

D_MODEL = 2048
BATCH = 4
SEQ = 4096
DEPTH = 2

S5_WIDTH = D_MODEL // 2
S5_GROUP = 16
S5_GROUPS = S5_WIDTH // S5_GROUP
S5_STATE = 64
S5_DT_MIN = 1e-3
S5_DT_MAX = 1e-1

ATT_HEAD_DIM = 64
ATT_Q_HEADS = (D_MODEL - S5_WIDTH) // ATT_HEAD_DIM
ATT_KV_HEADS = max(1, ATT_Q_HEADS // 8)
WINDOW = 128
ROPE_THETA = 500000.0
ROPE_DIM = ATT_HEAD_DIM // 4
MAX_POS_OFFSET = 1024

SSD_INNER = 2 * D_MODEL
SSD_HEAD_DIM = 64
SSD_HEADS = SSD_INNER // SSD_HEAD_DIM
SSD_GROUPS = 8
SSD_STATE = 128
SSD_CONV = 4
SSD_CHUNK = 128

MOE_GROUPS = 4
MOE_EXPERTS_PER_GROUP = 8
MOE_EXPERTS = MOE_GROUPS * MOE_EXPERTS_PER_GROUP
MOE_TOPK = 2
MOE_FF = D_MODEL // 8

EPS = 1e-6

kernel_name = 'hybrid_s5_swa_ssd_hmoe_trunk'


def rmsnorm(x, g):
    xf = x.astype(jnp.float32)
    y = xf * lax.rsqrt(jnp.mean(xf * xf, axis=-1, keepdims=True) + EPS)
    return (y * g.astype(jnp.float32)).astype(x.dtype)


def modulate(h, shift, scale):
    return h * (1.0 + scale[:, None, :]) + shift[:, None, :]


def rope_tables(positions):
    inv = 1.0 / (ROPE_THETA ** (jnp.arange(0, ROPE_DIM, 2, dtype=jnp.float32) / ROPE_DIM))
    ang = positions.astype(jnp.float32)[..., None] * inv
    return jnp.cos(ang), jnp.sin(ang)


def apply_partial_rope(x, cos, sin):
    half = ROPE_DIM // 2
    xr = x[..., :ROPE_DIM].astype(jnp.float32)
    x1, x2 = xr[..., :half], xr[..., half:]
    cs, sn = cos[:, :, None, :], sin[:, :, None, :]
    rot = jnp.concatenate([x1 * cs - x2 * sn, x2 * cs + x1 * sn], axis=-1).astype(x.dtype)
    return jnp.concatenate([rot, x[..., ROPE_DIM:]], axis=-1)


def s5_mixer(u, lam_re, lam_im, log_dt, b_re, b_im, c_re, c_im, d_skip, w_glu):
    bsz, L, _ = u.shape
    f32 = jnp.float32
    lam = lax.complex(lam_re.astype(f32), lam_im.astype(f32))
    dt = jnp.exp(log_dt.astype(f32))[:, None]
    lam_bar = jnp.exp(lam * dt)
    bmat = lax.complex(b_re.astype(f32), b_im.astype(f32))
    b_bar = ((lam_bar - 1.0) / lam)[..., None] * bmat
    ug = u.astype(f32).reshape(bsz, L, S5_GROUPS, S5_GROUP)
    bu = jnp.einsum('gnp,blgp->lbgn', b_bar, ug)
    a = jnp.broadcast_to(lam_bar, (L, 1, S5_GROUPS, S5_STATE))

    def combine(e1, e2):
        a1, b1 = e1
        a2, b2 = e2
        return a1 * a2, a2 * b1 + b2

    _, states = lax.associative_scan(combine, (a, bu), axis=0)
    cmat = lax.complex(c_re.astype(f32), c_im.astype(f32))
    y = jnp.real(jnp.einsum('gpn,lbgn->blgp', cmat, states)).reshape(bsz, L, S5_WIDTH)
    y = y + d_skip.astype(f32) * u.astype(f32)
    y = jax.nn.gelu(y).astype(u.dtype)
    val, gate = jnp.split(y @ w_glu, 2, axis=-1)
    return val * jax.nn.sigmoid(gate)


def swa_sink_attention(q, k, v, sinks):
    bsz, L, hq, dh = q.shape
    nb = L // WINDOW
    grp = hq // ATT_KV_HEADS
    qb = q.reshape(bsz, nb, WINDOW, ATT_KV_HEADS, grp, dh)

    def with_prev(t):
        tb = t.reshape(bsz, nb, WINDOW, ATT_KV_HEADS, dh)
        prev = jnp.pad(tb, ((0, 0), (1, 0), (0, 0), (0, 0), (0, 0)))[:, :-1]
        return jnp.concatenate([prev, tb], axis=2)

    kk, vv = with_prev(k), with_prev(v)
    s = jnp.einsum('bnqhgd,bnkhd->bnhgqk', qb, kk,
                   preferred_element_type=jnp.float32) * (ATT_HEAD_DIM ** -0.5)
    blk = jnp.arange(nb)[:, None, None] * WINDOW
    qpos = blk + jnp.arange(WINDOW)[None, :, None]
    kpos = blk - WINDOW + jnp.arange(2 * WINDOW)[None, None, :]
    rel = qpos - kpos
    mask = (rel >= 0) & (rel < WINDOW) & (kpos >= 0)
    s = jnp.where(mask[None, :, None, None], s, -jnp.inf)
    sink = sinks.astype(jnp.float32).reshape(ATT_KV_HEADS, grp)[None, None, :, :, None, None]
    sink = jnp.broadcast_to(sink, s.shape[:-1] + (1,))
    p = jax.nn.softmax(jnp.concatenate([s, sink], axis=-1), axis=-1)[..., :-1]
    o = jnp.einsum('bnhgqk,bnkhd->bnqhgd', p.astype(v.dtype), vv)
    return o.reshape(bsz, L, hq * dh)


def s5_swa_mixer(h, cos, sin, w_in, lam_re, lam_im, log_dt, b_re, b_im, c_re, c_im,
                 d_skip, w_glu, sinks, w_out):
    bsz, L, _ = h.shape
    hd = ATT_HEAD_DIM
    proj = h @ w_in
    q_end = S5_WIDTH + ATT_Q_HEADS * hd
    k_end = q_end + ATT_KV_HEADS * hd
    u, q, k, v = jnp.split(proj, [S5_WIDTH, q_end, k_end], axis=-1)
    q = apply_partial_rope(q.reshape(bsz, L, ATT_Q_HEADS, hd), cos, sin)
    k = apply_partial_rope(k.reshape(bsz, L, ATT_KV_HEADS, hd), cos, sin)
    v = v.reshape(bsz, L, ATT_KV_HEADS, hd)
    y_s5 = s5_mixer(u, lam_re, lam_im, log_dt, b_re, b_im, c_re, c_im, d_skip, w_glu)
    y_att = swa_sink_attention(q, k, v, sinks)
    return jnp.concatenate([y_s5, y_att.astype(y_s5.dtype)], axis=-1) @ w_out


def causal_depthwise_conv(x, w, b):
    out = lax.conv_general_dilated(
        x, w[:, None, :].astype(x.dtype), window_strides=(1,),
        padding=[(SSD_CONV - 1, 0)], dimension_numbers=('NWC', 'WIO', 'NWC'),
        feature_group_count=x.shape[-1])
    return out + b.astype(x.dtype)


def segsum_exp(a_cs):
    l = a_cs.shape[-1]
    diff = a_cs[..., :, None] - a_cs[..., None, :]
    mask = jnp.tril(jnp.ones((l, l), dtype=bool))
    return jnp.exp(jnp.where(mask, diff, -jnp.inf))


def ssd_scan(x, dt, a, bm, cm):
    bsz, L, H, P = x.shape
    nc = L // SSD_CHUNK
    J = H // SSD_GROUPS
    xc = (x * dt[..., None]).reshape(bsz, nc, SSD_CHUNK, SSD_GROUPS, J, P)
    ac = jnp.moveaxis((dt * a).reshape(bsz, nc, SSD_CHUNK, SSD_GROUPS, J), 2, -1)
    bc = bm.reshape(bsz, nc, SSD_CHUNK, SSD_GROUPS, SSD_STATE)
    cc = cm.reshape(bsz, nc, SSD_CHUNK, SSD_GROUPS, SSD_STATE)
    a_cs = jnp.cumsum(ac, axis=-1)
    lmat = segsum_exp(a_cs)
    cb = jnp.einsum('bclgn,bcsgn->bcgls', cc, bc)
    y_diag = jnp.einsum('bcgls,bcgjls,bcsgjp->bclgjp', cb, lmat, xc)
    decay_states = jnp.exp(a_cs[..., -1:] - a_cs)
    states = jnp.einsum('bclgn,bcgjl,bclgjp->bcgjpn', bc, decay_states, xc)
    chunk_decay = jnp.exp(a_cs[..., -1])

    def step(hs, inp):
        dec, st = inp
        return hs * dec[..., None, None] + st, hs

    h0 = jnp.zeros((bsz, SSD_GROUPS, J, P, SSD_STATE), jnp.float32)
    _, prev = lax.scan(step, h0, (jnp.moveaxis(chunk_decay, 1, 0), jnp.moveaxis(states, 1, 0)))
    prev = jnp.moveaxis(prev, 0, 1)
    y_off = jnp.einsum('bclgn,bcgjpn,bcgjl->bclgjp', cc, prev, jnp.exp(a_cs))
    return (y_diag + y_off).reshape(bsz, L, H, P)


def mamba2_mixer(h, w_in, conv_w, conv_b, dt_bias, a_log, d_skip, norm_w, w_out):
    bsz, L, _ = h.shape
    f32 = jnp.float32
    bc_width = SSD_GROUPS * SSD_STATE
    proj = h @ w_in
    z, xbc, dt = jnp.split(proj, [SSD_INNER, 2 * SSD_INNER + 2 * bc_width], axis=-1)
    xbc = jax.nn.silu(causal_depthwise_conv(xbc, conv_w, conv_b))
    xs, bm, cm = jnp.split(xbc, [SSD_INNER, SSD_INNER + bc_width], axis=-1)
    dt = jax.nn.softplus(dt.astype(f32) + dt_bias.astype(f32))
    a = -jnp.exp(a_log.astype(f32))
    xh = xs.astype(f32).reshape(bsz, L, SSD_HEADS, SSD_HEAD_DIM)
    y = ssd_scan(xh, dt, a,
                 bm.astype(f32).reshape(bsz, L, SSD_GROUPS, SSD_STATE),
                 cm.astype(f32).reshape(bsz, L, SSD_GROUPS, SSD_STATE))
    y = y + d_skip.astype(f32)[:, None] * xh
    y = y.reshape(bsz, L, SSD_INNER) * jax.nn.silu(z.astype(f32))
    yg = y.reshape(bsz, L, SSD_GROUPS, SSD_INNER // SSD_GROUPS)
    yg = yg * lax.rsqrt(jnp.mean(yg * yg, axis=-1, keepdims=True) + EPS)
    y = yg.reshape(bsz, L, SSD_INNER) * norm_w.astype(f32)
    return y.astype(h.dtype) @ w_out


def hier_moe(h, r1_w, r1_b, r2_w, r2_b, w_gate, w_up, w_down):
    bsz, L, D = h.shape
    f32 = jnp.float32
    t = h.reshape(-1, D)
    g_logits = (t @ r1_w).astype(f32) + r1_b.astype(f32)
    g_prob = jax.nn.softmax(g_logits, axis=-1)
    g_val, g_idx = lax.top_k(g_prob, 1)
    e_logits = ((t @ r2_w).astype(f32) + r2_b.astype(f32)).reshape(-1, MOE_GROUPS, MOE_EXPERTS_PER_GROUP)
    e_logits = jnp.take_along_axis(e_logits, g_idx[:, :, None], axis=1)[:, 0]
    top_v, top_i = lax.top_k(e_logits, MOE_TOPK)
    top_w = jax.nn.softmax(top_v, axis=-1) * g_val
    local = jnp.sum(jax.nn.one_hot(top_i, MOE_EXPERTS_PER_GROUP, dtype=f32) * top_w[..., None], axis=1)
    combine = (jax.nn.one_hot(g_idx[:, 0], MOE_GROUPS, dtype=f32)[:, :, None]
               * local[:, None, :]).reshape(-1, MOE_EXPERTS)
    hid = jax.nn.silu(jnp.einsum('td,edf->tef', t, w_gate)) * jnp.einsum('td,edf->tef', t, w_up)
    out = jnp.einsum('tef,te,efd->td', hid, combine.astype(hid.dtype), w_down)
    return out.reshape(bsz, L, D)


def setup_inputs(seed: int = 0) -> dict:
    key = jax.random.key(seed)
    keys = iter(jax.random.split(key, 48))
    f32 = jnp.float32
    D = D_MODEL
    ne = (DEPTH + 1) // 2
    no = DEPTH // 2

    def normal(shape, scale):
        return jax.random.normal(next(keys), shape, f32) * scale

    def gain(shape):
        return 1.0 + normal(shape, 0.02)

    mix_a_cols = S5_WIDTH + (ATT_Q_HEADS + 2 * ATT_KV_HEADS) * ATT_HEAD_DIM
    conv_ch = SSD_INNER + 2 * SSD_GROUPS * SSD_STATE
    ssd_cols = SSD_INNER + conv_ch + SSD_HEADS

    x = normal((BATCH, SEQ, D), 1.0)
    c = normal((BATCH, D), 1.0)
    offset = jax.random.randint(next(keys), (BATCH, 1), 0, MAX_POS_OFFSET, jnp.int32)
    positions = offset + jnp.arange(SEQ, dtype=jnp.int32)[None, :]

    ada_w = normal((DEPTH, D, 6 * D), 0.5 * D ** -0.5)
    ada_b = normal((DEPTH, 6 * D), 0.02)
    norm_mix = gain((DEPTH, D))
    norm_ffn = gain((DEPTH, D))

    mix_a_w_in = normal((ne, D, mix_a_cols), D ** -0.5)
    s5_lam_re = -0.5 + normal((ne, S5_GROUPS, S5_STATE), 0.005)
    s5_lam_im = jnp.pi * jnp.arange(S5_STATE, dtype=f32) + normal((ne, S5_GROUPS, S5_STATE), 0.01)
    s5_log_dt = jax.random.uniform(next(keys), (ne, S5_GROUPS), f32,
                                   math.log(S5_DT_MIN), math.log(S5_DT_MAX))
    s5_b_re = normal((ne, S5_GROUPS, S5_STATE, S5_GROUP), (2 * S5_GROUP) ** -0.5)
    s5_b_im = normal((ne, S5_GROUPS, S5_STATE, S5_GROUP), (2 * S5_GROUP) ** -0.5)
    s5_c_re = normal((ne, S5_GROUPS, S5_GROUP, S5_STATE), 4.0 * S5_STATE ** -0.5)
    s5_c_im = normal((ne, S5_GROUPS, S5_GROUP, S5_STATE), 4.0 * S5_STATE ** -0.5)
    s5_d = normal((ne, S5_WIDTH), 1.0)
    s5_w_glu = normal((ne, S5_WIDTH, 2 * S5_WIDTH), S5_WIDTH ** -0.5)
    attn_sinks = normal((ne, ATT_Q_HEADS), 0.5)
    mix_a_w_out = normal((ne, D, D), D ** -0.5)

    ssd_w_in = normal((no, D, ssd_cols), D ** -0.5)
    ssd_conv_w = normal((no, SSD_CONV, conv_ch), 0.5 * SSD_CONV ** -0.5)
    ssd_conv_b = normal((no, conv_ch), 0.02)
    dt0 = jnp.exp(jax.random.uniform(next(keys), (no, SSD_HEADS), f32, math.log(1e-3), math.log(1e-1)))
    ssd_dt_bias = dt0 + jnp.log(-jnp.expm1(-dt0))
    ssd_a_log = jnp.log(jax.random.uniform(next(keys), (no, SSD_HEADS), f32, 1.0, 16.0))
    ssd_d = gain((no, SSD_HEADS))
    ssd_norm_w = gain((no, SSD_INNER))
    ssd_w_out = normal((no, SSD_INNER, D), SSD_INNER ** -0.5)

    moe_r1_w = normal((DEPTH, D, MOE_GROUPS), D ** -0.5)
    moe_r1_b = normal((DEPTH, MOE_GROUPS), 0.01)
    moe_r2_w = normal((DEPTH, D, MOE_EXPERTS), D ** -0.5)
    moe_r2_b = normal((DEPTH, MOE_EXPERTS), 0.01)
    moe_w_gate = normal((DEPTH, MOE_EXPERTS, D, MOE_FF), D ** -0.5)
    moe_w_up = normal((DEPTH, MOE_EXPERTS, D, MOE_FF), D ** -0.5)
    moe_w_down = normal((DEPTH, MOE_EXPERTS, MOE_FF, D), MOE_FF ** -0.5)
    final_norm = gain((D,))

    return {'x': x, 'c': c, 'positions': positions,
            'ada_w': ada_w, 'ada_b': ada_b, 'norm_mix': norm_mix, 'norm_ffn': norm_ffn,
            'mix_a_w_in': mix_a_w_in, 's5_lam_re': s5_lam_re, 's5_lam_im': s5_lam_im,
            's5_log_dt': s5_log_dt, 's5_b_re': s5_b_re, 's5_b_im': s5_b_im,
            's5_c_re': s5_c_re, 's5_c_im': s5_c_im, 's5_d': s5_d, 's5_w_glu': s5_w_glu,
            'attn_sinks': attn_sinks, 'mix_a_w_out': mix_a_w_out,
            'ssd_w_in': ssd_w_in, 'ssd_conv_w': ssd_conv_w, 'ssd_conv_b': ssd_conv_b,
            'ssd_dt_bias': ssd_dt_bias, 'ssd_a_log': ssd_a_log, 'ssd_d': ssd_d,
            'ssd_norm_w': ssd_norm_w, 'ssd_w_out': ssd_w_out,
            'moe_r1_w': moe_r1_w, 'moe_r1_b': moe_r1_b, 'moe_r2_w': moe_r2_w, 'moe_r2_b': moe_r2_b,
            'moe_w_gate': moe_w_gate, 'moe_w_up': moe_w_up, 'moe_w_down': moe_w_down,
            'final_norm': final_norm}


def reference(x, c, positions, ada_w, ada_b, norm_mix, norm_ffn,
              mix_a_w_in, s5_lam_re, s5_lam_im, s5_log_dt, s5_b_re, s5_b_im,
              s5_c_re, s5_c_im, s5_d, s5_w_glu, attn_sinks, mix_a_w_out,
              ssd_w_in, ssd_conv_w, ssd_conv_b, ssd_dt_bias, ssd_a_log, ssd_d,
              ssd_norm_w, ssd_w_out,
              moe_r1_w, moe_r1_b, moe_r2_w, moe_r2_b, moe_w_gate, moe_w_up, moe_w_down,
              final_norm):
    cos, sin = rope_tables(positions)
    c_act = jax.nn.silu(c)
    for i in range(DEPTH):
        mod = c_act @ ada_w[i] + ada_b[i]
        sh1, sc1, g1, sh2, sc2, g2 = jnp.split(mod, 6, axis=-1)
        h = modulate(rmsnorm(x, norm_mix[i]), sh1, sc1)
        j = i // 2
        if i % 2 == 0:
            y = s5_swa_mixer(h, cos, sin, mix_a_w_in[j], s5_lam_re[j], s5_lam_im[j], s5_log_dt[j],
                             s5_b_re[j], s5_b_im[j], s5_c_re[j], s5_c_im[j], s5_d[j], s5_w_glu[j],
                             attn_sinks[j], mix_a_w_out[j])
        else:
            y = mamba2_mixer(h, ssd_w_in[j], ssd_conv_w[j], ssd_conv_b[j], ssd_dt_bias[j],
                             ssd_a_log[j], ssd_d[j], ssd_norm_w[j], ssd_w_out[j])
        x = x + g1[:, None, :] * y.astype(x.dtype)
        h = modulate(rmsnorm(x, norm_ffn[i]), sh2, sc2)
        y = hier_moe(h, moe_r1_w[i], moe_r1_b[i], moe_r2_w[i], moe_r2_b[i],
                     moe_w_gate[i], moe_w_up[i], moe_w_down[i])
        x = x + g2[:, None, :] * y.astype(x.dtype)
    return rmsnorm(x, final_norm)
```

```python
from contextlib import ExitStack, contextmanager
import numpy as np
import concourse.bass as bass
import concourse.mybir as mybir
from concourse.bass_utils import run_bass_kernel_spmd

F32 = mybir.dt.float32
BF16 = mybir.dt.bfloat16
I32 = mybir.dt.int32
ALU = mybir.AluOpType
AF = mybir.ActivationFunctionType
AX = mybir.AxisListType

SEM_CHUNK = 30000
DMA_SLOTS = 8
DMA_MAXJ = 1800
ENGS = ("pe", "act", "dve", "pool", "sp")


def _key(x):
    if isinstance(x, (str, tuple)):
        return x
    t = getattr(x, "tensor", None)
    if t is not None:
        if "DRam" in type(t).__name__:
            return (t.name, int(x.offset))
        return t.name
    if "DRam" in type(x).__name__:
        return (x.name, 0)
    return x.name


class MK:
    def __init__(self, nc):
        self.nc = nc
        self.root = ExitStack()
        self.cur = self.root
        self.ops = []
        self.cnt = {e: 0 for e in ENGS}
        self.sems = {e: [] for e in ENGS}
        self.dslots = {}
        self.drr = {}
        self.lastw = {}
        self.readers = {}
        self.seen = {e: {} for e in ENGS}
        self.ncc = 0
        self.uid = 0

    def sb(self, name, shape, dtype=F32):
        self.uid += 1
        return self.cur.enter_context(self.nc.sbuf_tensor(f"{name}_{self.uid}", list(shape), dtype))

    def ps(self, name, shape, dtype=F32):
        self.uid += 1
        nm = f"{name}_{self.uid}"
        if not hasattr(self, "psum_keys"):
            self.psum_keys = set()
        self.psum_keys.add(nm)
        return self.cur.enter_context(self.nc.psum_tensor(nm, list(shape), dtype))

    def _sem(self, name):
        return self.root.enter_context(self.nc.semaphore(name))

    @contextmanager
    def scope(self):
        prev = self.cur
        st = ExitStack()
        self.cur = st
        yield
        self.barrier()
        self.flush()
        st.close()
        self.cur = prev

    def _eng_tok(self, eng, seq):
        i = (seq - 1) // SEM_CHUNK
        while len(self.sems[eng]) <= i:
            self.sems[eng].append(self._sem(f"s_{eng}_{len(self.sems[eng])}"))
        return (("c", eng, i), self.sems[eng][i], (seq - 1) % SEM_CHUNK + 1)

    def _need(self, eng, tok, waits):
        k, h, v = tok
        if eng == "pe" and k[0] == "c" and k[1] == "pe":
            return
        if self.seen[eng].get(k, 0) >= v:
            return
        self.seen[eng][k] = v
        for i, (kk, hh, vv) in enumerate(waits):
            if kk == k:
                if vv < v:
                    waits[i] = (k, h, v)
                return
        waits.append((k, h, v))

    def _deps(self, eng, reads, writes):
        waits = []
        for r in reads:
            k = _key(r)
            t = self.lastw.get(k)
            if t is not None:
                self._need(eng, t, waits)
            if k in getattr(self, "psum_keys", ()):
                for t in self.readers.get(k, ()):
                    if not (t[0][0] == "c" and t[0][1] == eng):
                        self._need(eng, t, waits)
        for w in writes:
            k = _key(w)
            t = self.lastw.get(k)
            if t is not None:
                self._need(eng, t, waits)
            for t in self.readers.get(k, ()):
                self._need(eng, t, waits)
        return waits

    def _commit(self, tok, reads, writes):
        for r in reads:
            self.readers.setdefault(_key(r), []).append(tok)
        for w in writes:
            k = _key(w)
            self.lastw[k] = tok
            self.readers[k] = []

    def op(self, eng, fn, reads=(), writes=()):
        waits = self._deps(eng, reads, writes)
        self.cnt[eng] += 1
        tok = self._eng_tok(eng, self.cnt[eng])
        self.ops.append((eng, fn, waits, (tok[1], 1)))
        self._commit(tok, reads, writes)

    def dma(self, q, out, in_, reads=None, writes=None, **kw):
        reads = [in_] if reads is None else reads
        writes = [out] if writes is None else writes
        waits = self._deps(q, reads, writes)
        slots = self.dslots.setdefault(q, [])
        rr = self.drr.get(q, 0)
        if len(slots) < DMA_SLOTS:
            slots.append([self._sem(f"d_{q}_{len(slots)}"), 0, len(slots)])
            slot = slots[-1]
        else:
            slot = slots[rr % len(slots)]
            if slot[1] >= DMA_MAXJ:
                slot[0] = self._sem(f"d_{q}_r{rr}")
                slot[1] = 0
                slot[2] = 1000 + rr
        self.drr[q] = rr + 1
        key = ("d", q, slot[2])
        if slot[1] > 0:
            self._need(q, (key, slot[0], 16 * slot[1]), waits)
        slot[1] += 1
        tok = (key, slot[0], 16 * slot[1])

        def fn(e, out=out, in_=in_, kw=kw):
            return e.dma_start(out=out, in_=in_, **kw)
        self.ops.append((q, fn, waits, (slot[0], 16)))
        self._commit(tok, reads, writes)
        return tok

    def cc_allgather(self, out_t, in_t, groups):
        waits = self._deps("pool", [in_t], [out_t])
        self.ncc += 1
        sem = self._sem(f"cc_{self.ncc}")
        tok = (("cc", self.ncc), sem, 1)

        def fn(e):
            return e.collective_compute("AllGather", ALU.bypass, replica_groups=groups,
                                        ins=[in_t.ap().opt()], outs=[out_t.ap().opt()])
        self.ops.append(("pool", fn, waits, (sem, 1)))
        self._commit(tok, [in_t], [out_t])

    def barrier(self):
        toks = []
        for e in ENGS:
            if self.cnt[e] > 0:
                toks.append(self._eng_tok(e, self.cnt[e]))
        for q, slots in self.dslots.items():
            for s in slots:
                if s[1] > 0:
                    toks.append((("d", q, s[2]), s[0], 16 * s[1]))
        for k, t in self.lastw.items():
            if t[0][0] == "cc":
                toks.append(t)
        for e in ENGS:
            waits = []
            for t in toks:
                self._need(e, t, waits)
            if waits:
                self.ops.append((e, None, waits, None))

    def wait_all(self, eng, keys):
        waits = []
        for k in keys:
            t = self.lastw.get(_key(k))
            if t is not None:
                self._need(eng, t, waits)
        self.ops.append((eng, None, waits, None))

    def flush(self):
        if not self.ops:
            return
        if not hasattr(self, "_semval"):
            self._semval = {}
        for (eng, fn, waits, inc) in self.ops:
            for (k, h, v) in waits:
                cur = self._semval.get(id(h), 0)
                assert cur >= v, f"wait on future/unreachable value: eng={eng} key={k} need={v} have={cur}"
            if inc is not None:
                self._semval[id(inc[0])] = self._semval.get(id(inc[0]), 0) + inc[1]
        import os as _os2
        if _os2.environ.get("DUMPOPS"):
            for (eng, fn, waits, inc) in self.ops:
                nm = getattr(fn, "__qualname__", str(fn)) if fn is not None else "-"
                print("OP", eng, nm.split(".")[-3:] if fn else "-", [(k, v) for (k, h, v) in waits], "inc", (inc[1] if inc else None))
        per = {e: [] for e in ENGS}
        for o in self.ops:
            per[o[0]].append(o)
        self.ops = []

        def run(engine, lst):
            for (_, fn, waits, inc) in lst:
                for (k, h, v) in waits:
                    engine.wait_ge(h, v)
                if fn is not None:
                    ins = fn(engine)
                    ins.then_inc(inc[0], inc[1])

        with self.nc.Block() as block:
            @block.tensor
            def _(e):
                run(e, per["pe"])

            @block.scalar
            def _(e):
                run(e, per["act"])

            @block.vector
            def _(e):
                run(e, per["dve"])

            @block.gpsimd
            def _(e):
                run(e, per["pool"])

            @block.sync
            def _(e):
                run(e, per["sp"])

    def finish(self):
        self.flush()
        self.root.close()


D = 2048
KC = 16
NTOK = 2048
NCORES = 8
PAIRS = [[0, 1], [2, 3], [4, 5], [6, 7]]
EPS = 1e-6
NE = 32
FF = 256


class Ctx:
    pass


def declare_io(nc, cfg):
    io = Ctx()

    def inp(name, shape, dt=F32):
        t = nc.dram_tensor(name, list(shape), dt, kind="ExternalInput")
        setattr(io, name, t)
        return t
    inp("xT", [D, NTOK])
    inp("cT", [128, KC])
    inp("ada_w", [2, D, 6 * D])
    inp("ada_bT", [2, 128, 96])
    inp("nmix", [2, 128, KC])
    inp("nffn", [2, 128, KC])
    inp("fnorm", [128, KC])
    inp("router_w", [2, D, 36])
    inp("router_b", [2, 128, 36])
    inp("moe_w_gate", [2, NE, D, FF])
    inp("moe_w_up", [2, NE, D, FF])
    inp("moe_w_down", [2, NE, FF, D])
    inp("ident_f", [128, 128])
    io.outT = nc.dram_tensor("outT", [D, NTOK], F32, kind="ExternalOutput")
    io.X = [nc.dram_tensor(f"xs{i}", [D, NTOK], F32) for i in range(4)]
    io.cb = nc.dram_tensor("cb_scr", [2, 2, NE, 1024], F32, kind=("ExternalOutput" if cfg.get("dump") else "Internal"))
    return io


def setup_consts(mk, io, C):
    C.ones_b = mk.sb("ones_b", [128, 128], BF16)
    C.eps = mk.sb("eps", [128, 1], F32)
    C.ident_f = mk.sb("ident_f", [128, 128], F32)
    C.ident_b = mk.sb("ident_b", [128, 128], BF16)
    mk.op("pool", lambda e: e.memset(C.ones_b[:], 1.0), writes=[C.ones_b])
    mk.op("pool", lambda e: e.memset(C.eps[:], EPS), writes=[C.eps])
    mk.dma("sp", C.ident_f[:], io.ident_f.ap())
    mk.op("dve", lambda e: e.tensor_copy(out=C.ident_b[:], in_=C.ident_f[:]), reads=[C.ident_f], writes=[C.ident_b])
    C.MOD = [mk.sb(f"MOD{i}", [128, 96], F32) for i in range(2)]
    C.G1 = [mk.sb(f"G1{i}", [128, KC], F32) for i in range(2)]
    C.G2 = [mk.sb(f"G2{i}", [128, KC], F32) for i in range(2)]
    C.FN = mk.sb("FN", [128, KC], F32)
    C.ZERO = mk.sb("ZERO", [128, KC], F32)
    C.one = mk.sb("one", [128, 1], F32)
    mk.op("pool", lambda e: e.memset(C.one[:], 1.0), writes=[C.one])
    mk.dma("sp", C.FN[:], io.fnorm.ap())
    mk.op("pool", lambda e: e.memset(C.ZERO[:], 0.0), writes=[C.ZERO])


def phase_adaln(mk, io, C):
    with mk.scope():
        CT = mk.sb("CT", [128, KC], F32)
        CA = mk.sb("CA", [128, KC], BF16)
        WA = [mk.sb(f"WA{j}", [128, 6 * D], BF16) for j in range(2)]
        AB = mk.sb("AB", [128, 96], F32)
        NM = mk.sb("NM", [128, KC], F32)
        NF = mk.sb("NF", [128, KC], F32)
        PM = mk.ps("PM", [128, 96], F32)
        mk.dma("sp", CT[:], io.cT.ap())
        mk.op("act", lambda e: e.activation(out=CA[:], in_=CT[:], func=AF.Silu), reads=[CT], writes=[CA])
        for i in range(2):
            mk.dma("sp", AB[:], io.ada_bT.ap()[i])
            mk.dma("sp", NM[:], io.nmix.ap()[i])
            mk.dma("sp", NF[:], io.nffn.ap()[i])
            for kc in range(KC):
                wa = WA[kc % 2]
                mk.dma("pool", wa[:], io.ada_w.ap()[i, kc * 128:(kc + 1) * 128, :])

                def f(e, wa=wa, kc=kc):
                    ins = None
                    for j in range(96):
                        ins = e.matmul(PM[:, j:j + 1], wa[:, j * 128:(j + 1) * 128], CA[:, kc:kc + 1],
                                       start=(kc == 0 and j == 0), stop=(kc == KC - 1 and j == 95),
                                       skip_group_check=True)
                    return ins
                mk.op("pe", f, reads=[wa, CA], writes=[PM])
            MOD = C.MOD[i]
            mk.op("dve", lambda e, MOD=MOD: e.tensor_tensor(out=MOD[:], in0=PM[:], in1=AB[:], op=ALU.add),
                  reads=[PM, AB], writes=[MOD])
            mk.op("dve", lambda e, MOD=MOD, i=i: e.scalar_tensor_tensor(
                out=C.G1[i][:], in0=MOD[:, 16:32], scalar=1.0, in1=NM[:], op0=ALU.add, op1=ALU.mult),
                reads=[MOD, NM], writes=[C.G1[i]])
            mk.op("dve", lambda e, MOD=MOD, i=i: e.scalar_tensor_tensor(
                out=C.G2[i][:], in0=MOD[:, 64:80], scalar=1.0, in1=NF[:], op0=ALU.add, op1=ALU.mult),
                reads=[MOD, NF], writes=[C.G2[i]])


def norm_piece(mk, C, src, col0, ncols, G, SH, XT, SQ, PS, RS, HT_dst, HLO_dst=None, key=None, hlo_key=None):
    hk = key if key is not None else HT_dst
    mk.dma("sp", XT[:], src.ap()[:, col0:col0 + ncols].rearrange("(kc p) t -> p kc t", p=128))
    mk.op("act", lambda e: e.activation(out=SQ[:], in_=XT[:], func=AF.Square), reads=[XT], writes=[SQ])

    def f(e):
        ins = None
        for kc in range(KC):
            ins = e.matmul(PS[:], C.ones_b[:], SQ[:, kc, :], start=(kc == 0), stop=(kc == KC - 1))
        return ins
    mk.op("pe", f, reads=[SQ, C.ones_b], writes=[PS])
    mk.op("act", lambda e: e.activation(out=RS[:], in_=PS[:], func=AF.Sqrt, bias=C.eps[:, 0:1], scale=1.0 / D),
          reads=[PS, C.eps], writes=[RS])
    mk.op("dve", lambda e: e.reciprocal(out=RS[:], in_=RS[:]), reads=[RS], writes=[RS])

    def g(e):
        ins = None
        for kc in range(KC):
            ins = e.scalar_tensor_tensor(out=XT[:, kc, :], in0=XT[:, kc, :], scalar=G[:, kc:kc + 1], in1=RS[:],
                                         op0=ALU.mult, op1=ALU.mult)
        return ins
    mk.op("dve", g, reads=[XT, RS, G], writes=[XT])
    if HLO_dst is None:
        def h(e):
            ins = None
            for kc in range(KC):
                ins = e.activation(out=HT_dst[:, kc, :], in_=XT[:, kc, :], func=AF.Identity,
                                   bias=SH[:, kc:kc + 1], scale=1.0)
            return ins
        mk.op("act", h, reads=[XT, SH], writes=[hk])
    else:
        def h(e):
            ins = None
            for kc in range(KC):
                ins = e.activation(out=HT_dst[:, kc, :], in_=XT[:, kc, :], func=AF.Identity,
                                   bias=SH[:, kc:kc + 1], scale=1.0)
            return ins
        mk.op("act", h, reads=[XT, SH], writes=[hk])

        def hl(e):
            ins = None
            for kc in range(KC):
                ins = e.scalar_tensor_tensor(out=HLO_dst[:, kc, :], in0=XT[:, kc, :], scalar=SH[:, kc:kc + 1],
                                             in1=HT_dst[:, kc, :], op0=ALU.add, op1=ALU.subtract)
            return ins
        mk.op("dve", hl, reads=[XT, SH, hk], writes=[hlo_key if hlo_key is not None else HLO_dst])


def phase_moe(mk, io, C, li, src, dst):
    BLK = 1024
    NP = 256
    G2, SH2, g2 = C.G2[li], C.MOD[li][:, 48:64], C.MOD[li][:, 80:96]
    BIG = 30000.0
    for b in range(NTOK // BLK):
        with mk.scope():
            HT = mk.sb("HT", [128, KC, BLK], BF16)
            WG = [mk.sb(f"WG{j}", [128, KC, FF], BF16) for j in range(2)]
            WU = [mk.sb(f"WU{j}", [128, KC, FF], BF16) for j in range(2)]
            WD = [mk.sb(f"WD{j}", [128, 2, D], BF16) for j in range(4)]

            def load_wts(ex):
                mk.dma("pool", WG[ex % 2][:], io.moe_w_gate.ap()[li, ex].rearrange("(kc p) f -> p kc f", p=128))
                mk.dma("pool", WU[ex % 2][:], io.moe_w_up.ap()[li, ex].rearrange("(kc p) f -> p kc f", p=128))
                mk.dma("pool", WD[ex % 4][:], io.moe_w_down.ap()[li, ex].rearrange("(fc p) d -> p fc d", p=128))
            load_wts(0)
            with mk.scope():
                XTs = [mk.sb(f"XT{j}", [128, KC, NP], F32) for j in range(2)]
                SQs = [mk.sb(f"SQ{j}", [128, KC, NP], BF16) for j in range(2)]
                HLO = mk.sb("HLO", [128, KC, BLK], BF16)
                RSs = [mk.sb(f"RS{j}", [128, NP], F32) for j in range(2)]
                WR = mk.sb("WR", [128, KC, 36], F32)
                WRH = mk.sb("WRH", [128, KC, 36], BF16)
                WRL = mk.sb("WRL", [128, KC, 36], BF16)
                RB = mk.sb("RB", [128, 36], F32)
                COMB = mk.sb("COMB", [128, 8, NE], F32)
                CBT = mk.sb("CBT", [NE, BLK], F32)
                PSs = [mk.ps(f"PSn{j}", [128, NP], F32) for j in range(2)]
                LG = mk.ps("LG", [128, 8, 36], F32)
                PT = mk.ps("PTc", [NE, 512], F32)
                L = mk.sb("L", [128, 8, 36], F32)
                sm = {n: mk.sb(n, [128, 8] + w, F32) for n, w in
                      [("gmax", []), ("gd", [4]), ("gsum", []), ("gval", []), ("ohg", [4]), ("pen", [4]),
                       ("elm", [32]), ("m1", []), ("oh1", [32]), ("elm2", [32]), ("m2", []), ("oh2", [32]), ("dd", []),
                       ("w1", []), ("w2", []), ("t32", [32])]}
                mk.dma("sp", WR[:], io.router_w.ap()[li].rearrange("(kc p) n -> p kc n", p=128))
                mk.dma("sp", RB[:], io.router_b.ap()[li])
                mk.op("dve", lambda e: e.tensor_copy(out=WRH[:], in_=WR[:]), reads=[WR], writes=[WRH])
                mk.op("dve", lambda e: e.tensor_tensor(out=WRL[:], in0=WR[:], in1=WRH[:], op=ALU.subtract),
                      reads=[WR, WRH], writes=[WRL])
                for pc in range(BLK // NP):
                    c0 = b * BLK + pc * NP
                    hts = HT[:, :, pc * NP:(pc + 1) * NP]
                    norm_piece(mk, C, src, c0, NP, G2, SH2, XTs[pc % 2], SQs[pc % 2], PSs[pc % 2], RSs[pc % 2], hts,
                               HLO[:, :, pc * NP:(pc + 1) * NP], key=("HT", pc), hlo_key=("HLO", pc))
                    for sub in range(NP // 128):
                        st = pc * (NP // 128) + sub
                        t0 = pc * NP + sub * 128

                        def f(e, t0=t0, st=st):
                            ins = None
                            n = 0
                            for (a, w) in ((0, WRH), (1, WRH), (0, WRL)):
                                for kc in range(KC):
                                    lhs = HT[:, kc, t0:t0 + 128] if a == 0 else HLO[:, kc, t0:t0 + 128]
                                    ins = e.matmul(LG[:, st, :], lhs, w[:, kc, :], start=(n == 0), stop=(n == 3 * KC - 1))
                                    n += 1
                            return ins
                        mk.op("pe", f, reads=[("HT", pc), ("HLO", pc), WRH, WRL], writes=[LG])
                s = sm
                b8 = lambda ap, w: ap.unsqueeze(2).to_broadcast([128, 8, w])
                mk.op("dve", lambda e: e.tensor_tensor(out=L[:], in0=LG[:], in1=RB[:].unsqueeze(1).to_broadcast([128, 8, 36]), op=ALU.add),
                      reads=[LG, RB], writes=[L])
                mk.op("dve", lambda e: e.tensor_reduce(out=s["gmax"][:], in_=L[:, :, 0:4], axis=AX.X, op=ALU.max), reads=[L], writes=[s["gmax"]])
                mk.op("dve", lambda e: e.tensor_tensor(out=s["gd"][:], in0=L[:, :, 0:4], in1=b8(s["gmax"][:], 4), op=ALU.subtract),
                      reads=[L, s["gmax"]], writes=[s["gd"]])
                mk.op("dve", lambda e: e.tensor_scalar(out=s["ohg"][:], in0=s["gd"][:], scalar1=0.0, scalar2=None, op0=ALU.is_ge),
                      reads=[s["gd"]], writes=[s["ohg"]])
                mk.op("act", lambda e: e.activation(out=s["gd"][:], in_=s["gd"][:], func=AF.Exp), reads=[s["gd"], s["ohg"]], writes=[s["gd"]])
                mk.op("dve", lambda e: e.tensor_reduce(out=s["gsum"][:], in_=s["gd"][:], axis=AX.X, op=ALU.add), reads=[s["gd"]], writes=[s["gsum"]])
                mk.op("dve", lambda e: e.reciprocal(out=s["gval"][:], in_=s["gsum"][:]), reads=[s["gsum"]], writes=[s["gval"]])
                mk.op("dve", lambda e: e.tensor_scalar(out=s["pen"][:], in0=s["ohg"][:], scalar1=-1.0, scalar2=BIG, op0=ALU.add, op1=ALU.mult),
                      reads=[s["ohg"]], writes=[s["pen"]])
                mk.op("dve", lambda e: e.tensor_tensor(
                    out=s["elm"][:].rearrange("p s (g e) -> p s g e", g=4), in0=L[:, :, 4:36].rearrange("p s (g e) -> p s g e", g=4),
                    in1=s["pen"][:].unsqueeze(3).to_broadcast([128, 8, 4, 8]), op=ALU.add), reads=[L, s["pen"]], writes=[s["elm"]])
                mk.op("dve", lambda e: e.tensor_reduce(out=s["m1"][:], in_=s["elm"][:], axis=AX.X, op=ALU.max), reads=[s["elm"]], writes=[s["m1"]])
                mk.op("dve", lambda e: e.tensor_tensor(out=s["oh1"][:], in0=s["elm"][:], in1=b8(s["m1"][:], 32), op=ALU.is_ge),
                      reads=[s["elm"], s["m1"]], writes=[s["oh1"]])
                mk.op("dve", lambda e: e.tensor_scalar(out=s["t32"][:], in0=s["oh1"][:], scalar1=-BIG, scalar2=None, op0=ALU.mult),
                      reads=[s["oh1"]], writes=[s["t32"]])
                mk.op("dve", lambda e: e.tensor_tensor(out=s["elm2"][:], in0=s["elm"][:], in1=s["t32"][:], op=ALU.add),
                      reads=[s["elm"], s["t32"]], writes=[s["elm2"]])
                mk.op("dve", lambda e: e.tensor_reduce(out=s["m2"][:], in_=s["elm2"][:], axis=AX.X, op=ALU.max), reads=[s["elm2"]], writes=[s["m2"]])
                mk.op("dve", lambda e: e.tensor_tensor(out=s["oh2"][:], in0=s["elm2"][:], in1=b8(s["m2"][:], 32), op=ALU.is_ge),
                      reads=[s["elm2"], s["m2"]], writes=[s["oh2"]])
                mk.op("dve", lambda e: e.tensor_tensor(out=s["dd"][:], in0=s["m1"][:], in1=s["m2"][:], op=ALU.subtract),
                      reads=[s["m1"], s["m2"]], writes=[s["dd"]])
                mk.op("act", lambda e: e.activation(out=s["w1"][:], in_=s["dd"][:], func=AF.Sigmoid), reads=[s["dd"]], writes=[s["w1"]])
                mk.op("dve", lambda e: e.tensor_tensor(out=s["w1"][:], in0=s["w1"][:], in1=s["gval"][:], op=ALU.mult),
                      reads=[s["w1"], s["gval"]], writes=[s["w1"]])
                mk.op("dve", lambda e: e.tensor_tensor(out=s["w2"][:], in0=s["gval"][:], in1=s["w1"][:], op=ALU.subtract),
                      reads=[s["w1"], s["gval"]], writes=[s["w2"]])
                mk.op("dve", lambda e: e.tensor_tensor(out=s["t32"][:], in0=s["oh1"][:], in1=b8(s["w1"][:], 32), op=ALU.mult),
                      reads=[s["oh1"], s["w1"], s["elm2"]], writes=[s["t32"]])
                mk.op("dve", lambda e: e.tensor_tensor(out=s["elm2"][:], in0=s["oh2"][:], in1=b8(s["w2"][:], 32), op=ALU.mult),
                      reads=[s["oh2"], s["w2"]], writes=[s["elm2"]])
                mk.op("dve", lambda e: e.tensor_tensor(out=COMB[:], in0=s["t32"][:], in1=s["elm2"][:], op=ALU.add),
                      reads=[s["t32"], s["elm2"]], writes=[COMB])
                for hf in range(2):
                    def f(e, hf=hf):
                        ins = None
                        for j in range(4):
                            ins = e.matmul(PT[:, j * 128:(j + 1) * 128], COMB[:, hf * 4 + j, :], C.ident_f[:],
                                           start=True, stop=True)
                        return ins
                    mk.op("pe", f, reads=[COMB, C.ident_f], writes=[PT])
                    mk.op("dve", lambda e, hf=hf: e.tensor_copy(out=CBT[:, hf * 512:(hf + 1) * 512], in_=PT[:]),
                          reads=[PT], writes=[CBT])
                mk.dma("sp", io.cb.ap()[li, b], CBT[:], writes=[("cb", li, b)])
            accscope = mk.scope()
            accscope.__enter__()
            ACC = mk.sb("ACC", [128, KC, BLK], F32)
            with mk.scope():
                BC = [mk.sb(f"BC{j}", [128, BLK], F32) for j in range(2)]
                HID = [mk.sb(f"HID{j}", [128, 2, 2, 2, 512], BF16) for j in range(2)]
                SG = [mk.sb(f"SG{j}", [128, 512], F32) for j in range(2)]
                T1 = [mk.sb(f"T1{j}", [128, 512], F32) for j in range(2)]
                PA = [mk.ps(f"PA{j}", [128, 512], F32) for j in range(2)]
                PB = [mk.ps(f"PB{j}", [128, 512], F32) for j in range(2)]
                PO = [mk.ps(f"PO{j}", [128, 512], F32) for j in range(2)]
                cnt = 0

                def load_w(ex, wts=True):
                    if wts:
                        load_wts(ex)
                    mk.dma("sp", BC[ex % 2][:], io.cb.ap()[li, b, ex].partition_broadcast(128),
                           reads=[("cb", li, b)])
                load_w(0, wts=False)
                for eg in range(NE // 2):
                    hid = HID[eg % 2]
                    for e2 in range(2):
                        ex = eg * 2 + e2
                        wg, wu, wd, bc = WG[ex % 2], WU[ex % 2], WD[ex % 4], BC[ex % 2]
                        if ex + 1 < NE:
                            load_w(ex + 1)
                        for t in range(2):
                            for fc in range(2):
                                pa, pb, sg, t1 = PA[cnt % 2], PB[cnt % 2], SG[cnt % 2], T1[cnt % 2]
                                cnt += 1

                                def fg(e, w=wg, p=pa, t=t, fc=fc):
                                    ins = None
                                    for kc in range(KC):
                                        ins = e.matmul(p[:], w[:, kc, fc * 128:(fc + 1) * 128],
                                                       HT[:, kc, t * 512:(t + 1) * 512],
                                                       start=(kc == 0), stop=(kc == KC - 1))
                                    return ins
                                mk.op("pe", lambda e, fg=fg: fg(e), reads=[wg, HT], writes=[pa])
                                mk.op("pe", lambda e, fg=fg, wu=wu, pb=pb: fg(e, w=wu, p=pb), reads=[wu, HT], writes=[pb])
                                mk.op("act", lambda e, sg=sg, pa=pa: e.activation(out=sg[:], in_=pa[:], func=AF.Silu),
                                      reads=[pa], writes=[sg])
                                mk.op("dve", lambda e, t1=t1, pb=pb, sg=sg: e.tensor_tensor(
                                    out=t1[:], in0=pb[:], in1=sg[:], op=ALU.mult), reads=[pb, sg], writes=[t1])
                                mk.op("dve", lambda e, t1=t1, bc=bc, hid=hid, t=t, e2=e2, fc=fc: e.tensor_tensor(
                                    out=hid[:, t, e2, fc, :], in0=t1[:], in1=bc[:, t * 512:(t + 1) * 512], op=ALU.mult),
                                    reads=[t1, bc], writes=[(hid.name, t)])
                    for t in range(2):
                        for dc in range(KC):
                            po = PO[dc % 2]

                            def fd(e, po=po, t=t, dc=dc, eg=eg, hid=hid):
                                ins = None
                                n = 0
                                for e2 in range(2):
                                    wd = WD[(eg * 2 + e2) % 4]
                                    for fc in range(2):
                                        ins = e.matmul(po[:], wd[:, fc, dc * 128:(dc + 1) * 128], hid[:, t, e2, fc, :],
                                                       start=(n == 0), stop=(n == 3))
                                        n += 1
                                return ins
                            mk.op("pe", fd, reads=[WD[(eg * 2) % 4], WD[(eg * 2 + 1) % 4], (hid.name, t)], writes=[po])
                            acc = ACC[:, dc, t * 512:(t + 1) * 512]
                            if eg == 0:
                                mk.op("dve", lambda e, acc=acc, po=po: e.tensor_copy(out=acc, in_=po[:]),
                                      reads=[po], writes=[("ACC", t, dc)])
                            else:
                                mk.op("dve", lambda e, acc=acc, po=po: e.tensor_tensor(out=acc, in0=acc, in1=po[:], op=ALU.add),
                                      reads=[po, ("ACC", t, dc)], writes=[("ACC", t, dc)])
            with mk.scope():
                XR = [mk.sb(f"XR{j}", [128, KC, 256], F32) for j in range(2)]

                def r_ld(pc):
                    c0 = b * BLK + pc * 256
                    mk.dma("sp", XR[pc % 2][:], src.ap()[:, c0:c0 + 256].rearrange("(kc p) t -> p kc t", p=128))

                def r_st(pc):
                    xr = XR[pc % 2]
                    c0 = b * BLK + pc * 256

                    def fr(e, xr=xr, pc=pc):
                        ins = None
                        for kc in range(KC):
                            ins = e.scalar_tensor_tensor(out=xr[:, kc, :], in0=ACC[:, kc, pc * 256:(pc + 1) * 256],
                                                         scalar=g2[:, kc:kc + 1], in1=xr[:, kc, :],
                                                         op0=ALU.mult, op1=ALU.add)
                        return ins
                    mk.op("dve", fr, reads=[xr, C.MOD[li]] + [("ACC", pc // 2, dc) for dc in range(KC)], writes=[xr])
                    mk.dma("sp", dst.ap()[:, c0:c0 + 256].rearrange("(kc p) t -> p kc t", p=128), xr[:])
                r_ld(0); r_ld(1); r_st(0); r_ld(2); r_st(1); r_ld(3); r_st(2); r_st(3)
            accscope.__exit__(None, None, None)


def phase_final(mk, io, C, src):
    NP = 256
    with mk.scope():
        XTs = [mk.sb(f"XTf{j}", [128, KC, NP], F32) for j in range(3)]
        SQs = [mk.sb(f"SQf{j}", [128, KC, NP], BF16) for j in range(2)]
        RSs = [mk.sb(f"RSf{j}", [128, NP], F32) for j in range(2)]
        PSs = [mk.ps(f"PSf{j}", [128, NP], F32) for j in range(2)]
        for pc in range(NTOK // NP):
            XT = XTs[pc % 3]
            SQ, RS, PS = SQs[pc % 2], RSs[pc % 2], PSs[pc % 2]
            c0 = pc * NP
            mk.dma("sp", XT[:], src.ap()[:, c0:c0 + NP].rearrange("(kc p) t -> p kc t", p=128))
            mk.op("act", lambda e, XT=XT, SQ=SQ: e.activation(out=SQ[:], in_=XT[:], func=AF.Square), reads=[XT], writes=[SQ])

            def f(e, SQ=SQ, PS=PS):
                ins = None
                for kc in range(KC):
                    ins = e.matmul(PS[:], C.ones_b[:], SQ[:, kc, :], start=(kc == 0), stop=(kc == KC - 1))
                return ins
            mk.op("pe", f, reads=[SQ, C.ones_b], writes=[PS])
            mk.op("act", lambda e, RS=RS, PS=PS: e.activation(out=RS[:], in_=PS[:], func=AF.Sqrt, bias=C.eps[:, 0:1], scale=1.0 / D),
                  reads=[PS, C.eps], writes=[RS])
            mk.op("dve", lambda e, RS=RS: e.reciprocal(out=RS[:], in_=RS[:]), reads=[RS], writes=[RS])

            def g(e, XT=XT, RS=RS):
                ins = None
                for kc in range(KC):
                    ins = e.scalar_tensor_tensor(out=XT[:, kc, :], in0=XT[:, kc, :], scalar=C.FN[:, kc:kc + 1],
                                                 in1=RS[:], op0=ALU.mult, op1=ALU.mult)
                return ins
            mk.op("dve", g, reads=[XT, RS, C.FN], writes=[XT])
            mk.dma("sp", io.outT.ap()[:, c0:c0 + NP].rearrange("(kc p) t -> p kc t", p=128), XT[:])
    mk.wait_all("sp", [io.outT])


def copy_x(mk, src, dst):
    with mk.scope():
        T = [mk.sb(f"cp{j}", [128, KC, 256], F32) for j in range(2)]
        for pc in range(NTOK // 256):
            t = T[pc % 2]
            mk.dma("sp", t[:], src.ap()[:, pc * 256:(pc + 1) * 256].rearrange("(kc p) t -> p kc t", p=128))
            mk.dma("sp", dst.ap()[:, pc * 256:(pc + 1) * 256].rearrange("(kc p) t -> p kc t", p=128), t[:])


def build_program(cfg=None):
    cfg = cfg or {}
    nc = bass.Bass("TRN2", target_bir_lowering=False)
    io = declare_io(nc, cfg)
    declare_ssd_io(nc, io, bool(cfg.get("dump")))
    declare_mix0_io(nc, io, bool(cfg.get("dump")))
    mk = MK(nc)
    C = Ctx()
    setup_consts(mk, io, C)
    phase_adaln(mk, io, C)
    phases = cfg.get("phases", ("mix0", "moe0", "mix1", "moe1"))
    cur = io.xT
    k = 0
    for li in range(2):
        if f"mix{li}" in phases:
            if li == 0:
                phase_mix0(mk, io, C, cur, io.X[k], cfg.get("mix0_parts", ("s5", "attn", "out")))
                cur = io.X[k]
                k += 1
            if li == 1:
                phase_ssd(mk, io, C, cur, io.X[k], cfg.get("ssd_stages", 3), cfg.get("s2mode", "full"))
                cur = io.X[k]
                k += 1
        if f"moe{li}" in phases:
            phase_moe(mk, io, C, li, cur, io.X[k])
            cur = io.X[k]
            k += 1
    if cfg.get("dump"):
        io.dbgm = nc.dram_tensor("dbgM", [2, 128, 96], F32, kind="ExternalOutput")
        for i in range(2):
            mk.dma("sp", io.dbgm.ap()[i], C.MOD[i][:])
        io.dbg = nc.dram_tensor("dbgX", [D, NTOK], F32, kind="ExternalOutput")
        copy_x(mk, cur, io.dbg)
    phase_final(mk, io, C, cur)
    mk.finish()
    return nc


def _consts():
    i = np.arange(128)[:, None]
    j = np.arange(256)[None, :]
    valid = (j > i) & (j <= i + 128)
    am = np.where(valid, 0.0, -30000.0).astype(np.float32)
    am0 = np.where(valid & (j >= 128), 0.0, -30000.0).astype(np.float32)
    r = np.arange(128) // 16
    cm = (r[None, :] >= r[:, None]).astype(np.float32)
    jm = np.zeros((128, 128), np.float32)
    jm[np.arange(64) + 64, np.arange(64)] = -1.0
    jm[np.arange(64), np.arange(64) + 64] = 1.0
    kv = np.array([0, -1, -2, -3, -4, -5, -6, -7, 0, 1, 2, 3, 4, 5, 6, 7, 8], np.float32)
    mv = np.array(list(range(-1, -33, -1)) + list(range(0, 32)), np.float32)
    return am, am0, cm, jm, np.ascontiguousarray(np.broadcast_to(kv[None], (128, 17))), np.ascontiguousarray(np.broadcast_to(mv[None], (128, 64)))


_AMASK, _AMASK0, _CMASK, _JMAT, _KVALS, _MVALS = _consts()


def prep_inputs(inputs):
    f = lambda a: np.ascontiguousarray(a, dtype=np.float32)
    x = inputs["x"]
    shared = {
        "ada_w": f(inputs["ada_w"]),
        "ada_bT": f(inputs["ada_b"].reshape(2, 96, 128).transpose(0, 2, 1)),
        "nmix": f(inputs["norm_mix"].reshape(2, KC, 128).transpose(0, 2, 1)),
        "nffn": f(inputs["norm_ffn"].reshape(2, KC, 128).transpose(0, 2, 1)),
        "fnorm": f(inputs["final_norm"].reshape(KC, 128).T),
        "router_w": f(np.concatenate([inputs["moe_r1_w"], inputs["moe_r2_w"]], axis=-1)),
        "router_b": f(np.broadcast_to(np.concatenate([inputs["moe_r1_b"], inputs["moe_r2_b"]], axis=-1)[:, None, :],
                                      (2, 128, 36))),
        "moe_w_gate": f(inputs["moe_w_gate"]),
        "moe_w_up": f(inputs["moe_w_up"]),
        "moe_w_down": f(inputs["moe_w_down"]),
        "ident_f": np.eye(128, dtype=np.float32),
        "ssd_w_in": f(inputs["ssd_w_in"][0]),
        "ssd_convw": f(inputs["ssd_conv_w"][0].T.reshape(48, 128, 4).transpose(1, 0, 2)),
        "ssd_convb": f(inputs["ssd_conv_b"][0].reshape(48, 128).T),
        "ssd_dtb": f(inputs["ssd_dt_bias"][0].reshape(64, 1)),
        "ssd_alog": f(np.broadcast_to(inputs["ssd_a_log"][0][None, :], (128, 64))),
        "ssd_dsk": f(np.broadcast_to(inputs["ssd_d"][0][None, :], (128, 64))),
        "ssd_nw": f(inputs["ssd_norm_w"][0].reshape(32, 128).T),
        "ssd_w_out": f(inputs["ssd_w_out"][0]),
        "tri_f": np.triu(np.ones((128, 128), np.float32)),
        "w_in0": f(inputs["mix_a_w_in"][0]),
        "w_glu": f(inputs["s5_w_glu"][0]),
        "w_out0": f(inputs["mix_a_w_out"][0]),
        "rope_inv": f(np.broadcast_to((1.0 / (np.float32(ROPE_THETA) ** (np.arange(0, 16, 2, dtype=np.float32) / np.float32(16))))[None, :], (128, 8))),
        "sinks": f(np.broadcast_to(inputs["attn_sinks"][0][None, :], (128, 16))),
        "s5_lam_re": f(np.concatenate([inputs["s5_lam_re"][0].T] * 2, axis=0)),
        "s5_lam_im": f(np.concatenate([inputs["s5_lam_im"][0].T] * 2, axis=0)),
        "s5_logdt": f(np.broadcast_to(inputs["s5_log_dt"][0][None, :], (128, 64))),
        "s5_b_re": f(np.concatenate([inputs["s5_b_re"][0].transpose(1, 0, 2)] * 2, axis=0)),
        "s5_b_im": f(np.concatenate([inputs["s5_b_im"][0].transpose(1, 0, 2)] * 2, axis=0)),
        "s5_c_re": f(np.concatenate([inputs["s5_c_re"][0].transpose(2, 0, 1)] * 2, axis=0)),
        "s5_c_im": f(np.concatenate([inputs["s5_c_im"][0].transpose(2, 0, 1)] * 2, axis=0)),
        "s5_d": f(np.broadcast_to(inputs["s5_d"][0][None, :], (128, 1024))),
        "cmask": _CMASK, "jmat": _JMAT, "kvals": _KVALS, "mvals": _MVALS, "amask": _AMASK,
        "negm4": f(np.tile(np.where(np.triu(np.ones((128, 128))) > 0, 0.0, -30000.0), (1, 4))),
    }
    maps = []
    for core in range(NCORES):
        b, s = core // 2, core % 2
        m = dict(shared)
        m["xT"] = f(x[b, s * NTOK:(s + 1) * NTOK, :].T)
        m["cT"] = f(inputs["c"][b].reshape(KC, 128).T)
        m["pmask"] = np.full((128, 1), float(s), np.float32)
        pos = np.ascontiguousarray(inputs["positions"][b, s * NTOK:(s + 1) * NTOK], dtype=np.int32)
        m["pos_tm"] = np.ascontiguousarray(pos.reshape(NB, 128).T)
        m["pos_last"] = np.ascontiguousarray(pos[-128:].reshape(128, 1))
        m["amask_first"] = _AMASK if s == 1 else _AMASK0
        maps.append(m)
    return maps


def kernel(**inputs):
    nc = build_program()
    maps = prep_inputs(inputs)
    res = run_bass_kernel_spmd(nc, maps, core_ids=list(range(NCORES)))
    out = np.empty((4, 4096, D), np.float32)
    for core in range(NCORES):
        b, s = core // 2, core % 2
        out[b, s * NTOK:(s + 1) * NTOK, :] = res.results[core]["outT"].T
    return out


SSD_IN = 4096
SSD_COLS = 10304
NCH = NTOK // 128


def declare_ssd_io(nc, io, dump=False):
    kd = "ExternalOutput" if dump else "Internal"
    def inp(name, shape, dt=F32):
        t = nc.dram_tensor(name, list(shape), dt, kind="ExternalInput")
        setattr(io, name, t)
    inp("ssd_w_in", [D, SSD_COLS])
    inp("ssd_convw", [128, 48, 4])
    inp("ssd_convb", [128, 48])
    inp("ssd_dtb", [64, 1])
    inp("ssd_alog", [128, 64])
    inp("ssd_dsk", [128, 64])
    inp("ssd_nw", [128, 32])
    inp("ssd_w_out", [SSD_IN, D])
    inp("tri_f", [128, 128])
    inp("negm4", [128, 512])
    inp("pmask", [128, 1])
    io.ZS = nc.dram_tensor("ssd_zs", [SSD_IN, NTOK], BF16)
    io.XBC = nc.dram_tensor("ssd_xbc", [6144, NTOK], BF16, kind=kd)
    io.DTT = nc.dram_tensor("ssd_dtt", [64, NTOK], F32, kind=kd)
    io.YN = nc.dram_tensor("ssd_yn", [SSD_IN, NTOK], BF16, kind=kd)
    io.st_src = nc.dram_tensor("ssd_st_src", [128, 4096], F32)
    io.st_dst = nc.dram_tensor("ssd_st_dst", [256, 4096], F32)
    io.ct_src = nc.dram_tensor("ssd_ct_src", [128, 144], F32)
    io.ct_dst = nc.dram_tensor("ssd_ct_dst", [256, 144], F32)


def ssd_stage1(mk, io, C, src):
    li = 1
    G1, SH1 = C.G1[li], C.MOD[li][:, 0:16]
    NP = 256
    with mk.scope():
        HT = mk.sb("HTs", [128, KC, NTOK], BF16)
        if True:
            XT = [mk.sb(f"XTs{j}", [128, KC, NP], F32) for j in range(2)]
            SQ = [mk.sb(f"SQs{j}", [128, KC, NP], BF16) for j in range(2)]
            RS = [mk.sb(f"RSs{j}", [128, NP], F32) for j in range(2)]
            PS = [mk.ps(f"PSs{j}", [128, NP], F32) for j in range(2)]
            W = [mk.sb(f"Wi{j}", [128, KC, 512], BF16) for j in range(2)]
            mk.dma("pool", W[0][:, :, 0:512], io.ssd_w_in.ap()[:, 0:512].rearrange("(kc p) n -> p kc n", p=128))
            for pc in range(NTOK // NP):
                norm_piece(mk, C, src, pc * NP, NP, G1, SH1, XT[pc % 2], SQ[pc % 2], PS[pc % 2], RS[pc % 2],
                           HT[:, :, pc * NP:(pc + 1) * NP], key=("HTs", pc))
        if True:
            CW = mk.sb("CW", [128, 48, 4], F32)
            CB = mk.sb("CB", [128, 48], F32)
            DTB = mk.sb("DTB", [64, 1], F32)
            RAW = [mk.sb(f"RAW{m}", [128, 515], F32) for m in range(4)]
            TAILS = mk.sb("TAILS", [128, 48, 3], F32)
            HEADS = mk.sb("HEADS", [128, 48, 3], F32)
            A1 = [mk.sb(f"A1{j}", [128, 512], F32) for j in range(2)]
            OUT = [mk.sb(f"OUTs{j}", [128, 512], BF16) for j in range(3)]
            DTO = mk.sb("DTO", [64, 512], F32)
            DT1 = mk.sb("DT1", [64, 512], F32)
            PP = [mk.ps(f"PPs{j}", [128, 512], F32) for j in range(3)]
            mk.dma("sp", CW[:], io.ssd_convw.ap())
            mk.dma("sp", CB[:], io.ssd_convb.ap())
            mk.dma("sp", DTB[:], io.ssd_dtb.ap())
            nblk = 21
            cnt = 0
            oc = 0

            def loadw(cb):
                c0 = cb * 512
                wcols = min(512, SSD_COLS - c0)
                mk.dma("pool", W[cb % 2][:, :, 0:wcols],
                       io.ssd_w_in.ap()[:, c0:c0 + wcols].rearrange("(kc p) n -> p kc n", p=128))
            for cb in range(nblk):
                if cb + 1 < nblk:
                    loadw(cb + 1)
                w = W[cb % 2]
                nm = 4 if cb < 20 else 1
                for t in range(NTOK // 512):
                    for m in range(nm):
                        pp = PP[cnt % 3]
                        cnt += 1
                        mrows = 128 if cb < 20 else 64

                        def f(e, pp=pp, w=w, m=m, t=t, mrows=mrows):
                            ins = None
                            for kc in range(KC):
                                ins = e.matmul(pp[0:mrows, :], w[:, kc, m * 128:m * 128 + mrows],
                                               HT[:, kc, t * 512:(t + 1) * 512], start=(kc == 0), stop=(kc == KC - 1))
                            return ins
                        mk.op("pe", f, reads=[w, ("HTs", 2 * t), ("HTs", 2 * t + 1)], writes=[pp])
                        ch = cb * 4 + m
                        if cb < 8:
                            o = OUT[oc % 3]
                            oc += 1
                            mk.op("act", lambda e, o=o, pp=pp: e.activation(out=o[:], in_=pp[:], func=AF.Silu),
                                  reads=[pp], writes=[o])
                            mk.dma("sp", io.ZS.ap()[ch * 128:(ch + 1) * 128, t * 512:(t + 1) * 512], o[:])
                        elif cb < 20:
                            cc = ch - 32
                            raw = RAW[m]
                            eng = "dve"
                            if t == 0:
                                mk.op("pool", lambda e, raw=raw: e.memset(raw[:, 0:3], 0.0), writes=[raw])
                            mk.op("act", lambda e, raw=raw, pp=pp: e.activation(out=raw[:, 3:515], in_=pp[:], func=AF.Identity),
                                  reads=[pp], writes=[raw])
                            if t == 0:
                                mk.op("pool", lambda e, raw=raw, cc=cc: e.tensor_copy(out=HEADS[:, cc, :], in_=raw[:, 3:6]),
                                      reads=[raw], writes=[("HEADS", cc)])
                            a1 = A1[m % 2]

                            def fc(e, raw=raw, a1=a1, cc=cc):
                                e.tensor_scalar(out=a1[:], in0=raw[:, 0:512], scalar1=CW[:, cc, 0:1], scalar2=CB[:, cc:cc + 1],
                                                op0=ALU.mult, op1=ALU.add)
                                return None
                            mk.op(eng, lambda e, raw=raw, a1=a1, cc=cc: e.tensor_scalar(
                                out=a1[:], in0=raw[:, 0:512], scalar1=CW[:, cc, 0:1], scalar2=CB[:, cc:cc + 1],
                                op0=ALU.mult, op1=ALU.add), reads=[raw, CW, CB], writes=[a1])
                            for j in range(1, 4):
                                mk.op(eng, lambda e, raw=raw, a1=a1, cc=cc, j=j: e.scalar_tensor_tensor(
                                    out=a1[:], in0=raw[:, j:j + 512], scalar=CW[:, cc, j:j + 1], in1=a1[:],
                                    op0=ALU.mult, op1=ALU.add), reads=[raw, a1, CW], writes=[a1])
                            o = OUT[oc % 3]
                            oc += 1
                            mk.op("act", lambda e, o=o, a1=a1: e.activation(out=o[:], in_=a1[:], func=AF.Silu),
                                  reads=[a1], writes=[o])
                            mk.dma("sp", io.XBC.ap()[cc * 128:(cc + 1) * 128, t * 512:(t + 1) * 512], o[:])
                            if t < NTOK // 512 - 1:
                                mk.op("pool", lambda e, raw=raw: e.tensor_copy(out=raw[:, 0:3], in_=raw[:, 512:515]),
                                      reads=[raw], writes=[raw])
                            else:
                                mk.op("pool", lambda e, raw=raw, cc=cc: e.tensor_copy(out=TAILS[:, cc, :], in_=raw[:, 512:515]),
                                      reads=[raw], writes=[("TAILS", cc)])
                        else:
                            mk.op("act", lambda e, pp=pp: e.activation(out=DTO[:], in_=pp[0:64, :], func=AF.Identity,
                                                                      bias=DTB[:, 0:1], scale=1.0),
                                  reads=[pp, DTB], writes=[DTO])
                            mk.op("act", lambda e: e.activation(out=DT1[:], in_=DTO[:], func=AF.Abs),
                                  reads=[DTO], writes=[DT1])
                            mk.op("act", lambda e: e.activation(out=DT1[:], in_=DT1[:], func=AF.Exp, scale=-1.0),
                                  reads=[DT1], writes=[DT1])
                            mk.op("act", lambda e: e.activation(out=DT1[:], in_=DT1[:], func=AF.Ln, bias=C.one[0:64, 0:1], scale=1.0),
                                  reads=[DT1, C.one], writes=[DT1])
                            mk.op("dve", lambda e: e.scalar_tensor_tensor(out=DTO[:], in0=DTO[:], scalar=0.0, in1=DT1[:],
                                                                          op0=ALU.max, op1=ALU.add),
                                  reads=[DTO, DT1], writes=[DTO])
                            mk.dma("sp", io.DTT.ap()[:, t * 512:(t + 1) * 512], DTO[:])
            mk.dma("sp", io.ct_src.ap(), TAILS[:].rearrange("p c t -> p (c t)"),
                   reads=[("TAILS", c) for c in range(48)], writes=[io.ct_src])
            mk.cc_allgather(io.ct_dst, io.ct_src, PAIRS)
            PRV = mk.sb("PRV", [128, 48, 6], F32)
            PM_ = mk.sb("PMk", [128, 1], F32)
            FX = mk.sb("FX", [128, 48, 3], F32)
            FXT = mk.sb("FXT", [128, 48, 3], F32)
            FXB = mk.sb("FXB", [128, 48, 3], BF16)
            mk.dma("sp", PM_[:], io.pmask.ap())
            mk.dma("sp", PRV[:, :, 0:3], io.ct_dst.ap()[0:128, :].rearrange("p (c t) -> p c t", t=3))
            mk.op("dve", lambda e: e.tensor_scalar(out=PRV[:, :, 0:3], in0=PRV[:, :, 0:3], scalar1=PM_[:, 0:1], scalar2=None,
                                                   op0=ALU.mult), reads=[PRV, PM_], writes=[PRV])
            mk.op("dve", lambda e: e.tensor_copy(out=PRV[:, :, 3:6], in_=HEADS[:]),
                  reads=[("HEADS", c) for c in range(48)] + [PRV], writes=[PRV])
            mk.op("dve", lambda e: e.tensor_tensor(out=FX[:], in0=PRV[:, :, 0:3], in1=CW[:, :, 0:1].to_broadcast([128, 48, 3]),
                                                   op=ALU.mult), reads=[PRV, CW], writes=[FX])
            for j in range(1, 4):
                mk.op("dve", lambda e, j=j: e.tensor_tensor(out=FXT[:], in0=PRV[:, :, j:j + 3],
                                                            in1=CW[:, :, j:j + 1].to_broadcast([128, 48, 3]), op=ALU.mult),
                      reads=[PRV, CW], writes=[FXT])
                mk.op("dve", lambda e: e.tensor_tensor(out=FX[:], in0=FX[:], in1=FXT[:], op=ALU.add),
                      reads=[FX, FXT], writes=[FX])
            mk.op("dve", lambda e: e.tensor_tensor(out=FX[:], in0=FX[:], in1=CB[:].unsqueeze(2).to_broadcast([128, 48, 3]),
                                                   op=ALU.add), reads=[FX, CB], writes=[FX])
            mk.op("act", lambda e: e.activation(out=FXB[:], in_=FX[:], func=AF.Silu), reads=[FX], writes=[FXB])
            for cc in range(48):
                mk.dma("sp", io.XBC.ap()[cc * 128:(cc + 1) * 128, 0:3], FXB[:, cc, :], reads=[FXB],
                       writes=[io.XBC.ap()[cc * 128:(cc + 1) * 128, 0:512]], allow_slow_non_contiguous=True)


def ssd_stage2(mk, io, C, mode="full"):
    with mk.scope():
        STATE = mk.sb("STATE", [128, 4096], F32)
        STBF = mk.sb("STBF", [128, 4096], BF16)
        TRI = mk.sb("TRI", [128, 128], F32)
        NEGM4 = mk.sb("NEGM4", [128, 512], F32)
        ONESF = mk.sb("ONESF", [128, 128], F32)
        ABC = mk.sb("ABC", [128, 64], F32)
        DSK = mk.sb("DSK", [128, 64], F32)
        NW = mk.sb("NW", [128, 32], F32)
        PMK = mk.sb("PMK2", [128, 1], F32)
        XTc = [mk.sb(f"XTc{j}", [128, 32, 128], BF16) for j in range(2)]
        ZSc = [mk.sb(f"ZSc{j}", [128, 32, 128], BF16) for j in range(2)]
        BTc = [mk.sb(f"BTc{j}", [128, 8, 128], BF16) for j in range(3)]
        CTc = [mk.sb(f"CTc{j}", [128, 8, 128], BF16) for j in range(3)]
        DTc = [mk.sb(f"DTc{j}", [64, 128], F32) for j in range(3)]
        DTt_ = [mk.sb(f"DTt{j}", [128, 64], F32) for j in range(2)]
        ADT_ = [mk.sb(f"ADT{j}", [128, 64], F32) for j in range(2)]
        ACS_ = [mk.sb(f"ACS{j}", [128, 64], F32) for j in range(2)]
        NACS_ = [mk.sb(f"NACS{j}", [128, 64], F32) for j in range(2)]
        ACST_ = [mk.sb(f"ACST{j}", [64, 128], F32) for j in range(2)]
        AEND_ = [mk.sb(f"AEND{j}", [128, 64], F32) for j in range(2)]
        DECST_ = [mk.sb(f"DECST{j}", [128, 64], F32) for j in range(2)]
        EA_ = [mk.sb(f"EA{j}", [128, 64], F32) for j in range(2)]
        DEC_ = [mk.sb(f"DEC{j}", [128, 64], F32) for j in range(2)]
        SEL = mk.sb("SEL", [64, 64, 128], BF16)
        NEGMB = mk.sb("NEGMB", [128, 128], BF16)
        HS_ = [[mk.sb(f"HS{k}_{j}", [64, 128], BF16) for k in range(3)] for j in range(2)]
        RSD = mk.sb("RSD", [64, 128], F32)
        LT = [mk.sb(f"LT{j}", [128, 4, 128], BF16) for j in range(2)]
        MT = [mk.sb(f"MT{j}", [128, 8, 128], BF16) for j in range(2)]
        GT_ = [mk.sb(f"GT{j}", [128, 8, 128], F32) for j in range(2)]
        XTOK = mk.sb("XTOK", [128, 4096], BF16)
        XDT = mk.sb("XDT", [128, 4096], BF16)
        XDD = mk.sb("XDD", [128, 4096], BF16)
        BTOK = mk.sb("BTOK", [128, 8, 128], BF16)
        YB = mk.sb("YB", [128, 4096], BF16)
        TMP = [mk.sb(f"TMPy{j}", [128, 512], F32) for j in range(2)]
        TM2 = [mk.sb(f"TM2y{j}", [128, 512], F32) for j in range(2)]
        YG = mk.sb("YG", [128, 32, 128], F32)
        SQG = mk.sb("SQG", [128, 32, 128], BF16)
        RSTD = mk.sb("RSTD", [128, 8, 128], F32)
        YNo = mk.sb("YNo", [128, 32, 128], BF16)
        B = [mk.ps(f"Bk{j}", [128, 512], F32) for j in range(8)]
        PXb = [mk.ps(f"PXb{j}", [128, 8, 128], BF16) for j in range(0)]

        mk.dma("sp", TRI[:], io.tri_f.ap())
        mk.dma("sp", NEGM4[:], io.negm4.ap())
        mk.dma("sp", ABC[:], io.ssd_alog.ap())
        mk.dma("sp", DSK[:], io.ssd_dsk.ap())
        mk.dma("sp", NW[:], io.ssd_nw.ap())
        mk.dma("sp", PMK[:], io.pmask.ap())
        mk.op("pool", lambda e: e.memset(ONESF[:], 1.0), writes=[ONESF])
        mk.op("dve", lambda e: e.tensor_copy(out=SEL[:], in_=C.ident_f[0:64, 0:64].unsqueeze(2).to_broadcast([64, 64, 128])),
              reads=[C.ident_f], writes=[SEL])
        mk.op("dve", lambda e: e.tensor_copy(out=NEGMB[:], in_=NEGM4[:, 0:128]), reads=[NEGM4], writes=[NEGMB])
        mk.op("act", lambda e: e.activation(out=ABC[:], in_=ABC[:], func=AF.Exp), reads=[ABC], writes=[ABC])
        mk.op("dve", lambda e: e.tensor_scalar(out=ABC[:], in0=ABC[:], scalar1=-1.0, scalar2=None, op0=ALU.mult),
              reads=[ABC], writes=[ABC])

        def bf(bank, shape):
            return bank[:].bitcast(BF16).rearrange("p (a b) -> p a b", a=8)[:, :, 0:128]

        def loads_small(c):
            k3 = c % 3
            cs = slice(c * 128, (c + 1) * 128)
            mk.dma("sp", BTc[k3][:], io.XBC.ap()[4096:5120, cs].rearrange("(kc p) t -> p kc t", p=128))
            mk.dma("sp", CTc[k3][:], io.XBC.ap()[5120:6144, cs].rearrange("(kc p) t -> p kc t", p=128))
            mk.dma("sp", DTc[k3][:], io.DTT.ap()[:, cs])

        def loads(c):
            j = c % 2
            cs = slice(c * 128, (c + 1) * 128)
            for qq in range(4):
                mk.dma("sp", XTc[j][:, qq * 8:(qq + 1) * 8, :],
                       io.XBC.ap()[qq * 1024:(qq + 1) * 1024, cs].rearrange("(kc p) t -> p kc t", p=128),
                       writes=[(XTc[j].name, qq)])

        def loadz(c):
            j = c % 2
            cs = slice(c * 128, (c + 1) * 128)
            for qq in range(4):
                mk.dma("sp", ZSc[j][:, qq * 8:(qq + 1) * 8, :],
                       io.ZS.ap()[qq * 1024:(qq + 1) * 1024, cs].rearrange("(kc p) t -> p kc t", p=128),
                       writes=[(ZSc[j].name, qq)])

        import os as _os
        CUT = int(_os.environ.get("CUT", "99"))
        SUBCUT = int(_os.environ.get("SUBCUT", "99"))

        def chunk(c, full, part):
            j = c % 2
            xt, bt, ct, dt = XTc[j], BTc[c % 3], CTc[c % 3], DTc[c % 3]
            DTt, ADT, ACS, NACS, ACST = DTt_[j], ADT_[j], ACS_[j], NACS_[j], ACST_[j]
            AEND, DECST, EA, DEC, GT = AEND_[j], DECST_[j], EA_[j], DEC_[j], GT_[j]
            if part == "dt":
                chunk_dt(c, full, xt, bt, ct, dt, DTt, ADT, ACS, NACS, ACST, AEND, DECST, EA, DEC, GT)
            else:
                chunk_main(c, full, j, xt, bt, ct, dt, DTt, ADT, ACS, NACS, ACST, AEND, DECST, EA, DEC, GT)

        def chunk_dt(c, full, xt, bt, ct, dt, DTt, ADT, ACS, NACS, ACST, AEND, DECST, EA, DEC, GT):
            mk.op("pe", lambda e: e.matmul(B[7][:, 0:64], dt[:, :], C.ident_f[0:64, 0:64], start=True, stop=True),
                  reads=[dt, C.ident_f], writes=[B[7]])
            if CUT <= 0:
                return
            mk.op("act", lambda e: e.activation(out=DTt[:], in_=B[7][:, 0:64], func=AF.Identity), reads=[B[7]], writes=[DTt])
            mk.op("dve", lambda e: e.tensor_tensor(out=ADT[:], in0=DTt[:], in1=ABC[:], op=ALU.mult),
                  reads=[DTt, ABC], writes=[ADT])

            def f(e):
                e.matmul(B[7][:, 64:128], TRI[:], ADT[:], start=True, stop=True)
                e.matmul(B[7][0:64, 128:256], ADT[:], TRI[:], start=True, stop=True)
                return e.matmul(B[7][:, 256:320], ONESF[:], ADT[:], start=True, stop=True)
            if CUT <= 1:
                return
            mk.op("pe", f, reads=[TRI, ADT, ONESF], writes=[B[7]])
            if CUT <= 2:
                return
            if CUT == 3 and SUBCUT <= 0:
                return
            mk.op("act", lambda e: e.activation(out=ACS[:], in_=B[7][:, 64:128], func=AF.Identity), reads=[B[7]], writes=[ACS])
            if CUT == 3 and SUBCUT <= 1:
                return
            mk.op("dve", lambda e: e.tensor_scalar(out=NACS[:], in0=ACS[:], scalar1=-1.0, scalar2=None, op0=ALU.mult),
                  reads=[ACS], writes=[NACS])
            if CUT == 3 and SUBCUT <= 2:
                return
            mk.op("act", lambda e: e.activation(out=ACST[:], in_=B[7][0:64, 128:256], func=AF.Identity), reads=[B[7]], writes=[ACST])
            if CUT == 3 and SUBCUT <= 3:
                return
            mk.op("dve", lambda e: e.tensor_copy(out=AEND[:], in_=B[7][:, 256:320]), reads=[B[7]], writes=[AEND])
            if CUT == 3 and SUBCUT <= 4:
                return
            mk.op("dve", lambda e: e.tensor_tensor(out=DECST[:], in0=AEND[:], in1=ACS[:], op=ALU.subtract),
                  reads=[AEND, ACS], writes=[DECST])
            if CUT == 3 and SUBCUT <= 5:
                return
            mk.op("act", lambda e: e.activation(out=DECST[:], in_=DECST[:], func=AF.Exp), reads=[DECST], writes=[DECST])
            if CUT == 3 and SUBCUT <= 6:
                return
            mk.op("act", lambda e: e.activation(out=DEC[:], in_=AEND[:], func=AF.Exp), reads=[AEND], writes=[DEC])
            if full:
                HS = HS_[c % 2]
                mk.op("dve", lambda e: e.tensor_copy(out=HS[0][:], in_=ACST[:]), reads=[ACST], writes=[HS[0]])
                mk.op("dve", lambda e: e.tensor_tensor(out=RSD[:], in0=ACST[:], in1=HS[0][:], op=ALU.subtract), reads=[ACST, HS[0]], writes=[RSD])
                mk.op("dve", lambda e: e.tensor_copy(out=HS[1][:], in_=RSD[:]), reads=[RSD], writes=[HS[1]])
                mk.op("dve", lambda e: e.tensor_tensor(out=RSD[:], in0=RSD[:], in1=HS[1][:], op=ALU.subtract), reads=[RSD, HS[1]], writes=[RSD])
                mk.op("dve", lambda e: e.tensor_copy(out=HS[2][:], in_=RSD[:]), reads=[RSD], writes=[HS[2]])
            if full:
                mk.op("act", lambda e: e.activation(out=EA[:], in_=ACS[:], func=AF.Exp), reads=[ACS], writes=[EA])
                for hf in range(2):
                    def fg(e, hf=hf):
                        ins = None
                        for g4 in range(4):
                            g = hf * 4 + g4
                            ins = e.matmul(B[2 + hf][:, g4 * 128:(g4 + 1) * 128], bt[:, g, :], ct[:, g, :], start=True, stop=True)
                        return ins
                    mk.op("pe", fg, reads=[bt, ct], writes=[B[2 + hf]])
                    mk.op("act", lambda e, hf=hf: e.activation(out=GT[:, hf * 4:(hf + 1) * 4, :].rearrange("p a b -> p (a b)"),
                                                                 in_=B[2 + hf][:], func=AF.Identity),
                          reads=[B[2 + hf]], writes=[GT])

        def chunk_main(c, full, j, xt, bt, ct, dt, DTt, ADT, ACS, NACS, ACST, AEND, DECST, EA, DEC, GT):
            for q in range(4):
                bank = B[q % 2]
                pv = bf(bank, None)

                def ft(e, q=q, pv=pv):
                    ins = None
                    for i in range(8):
                        ins = e.transpose(pv[:, i, :], xt[:, q * 8 + i, :], C.ident_b[:])
                    return ins
                mk.op("pe", ft, reads=[(xt.name, q), C.ident_b], writes=[bank])
                if full:
                    mk.op("act", lambda e, q=q, pv=pv: e.activation(
                        out=XTOK[:, q * 1024:(q + 1) * 1024].rearrange("p (a b) -> p a b", a=8), in_=pv, func=AF.Identity),
                        reads=[bank], writes=[("XTOK", q)])
                mk.op("dve", lambda e, q=q, pv=pv: e.tensor_tensor(
                    out=XDT[:, q * 1024:(q + 1) * 1024].rearrange("p (h d) -> p h d", h=16),
                    in0=pv.rearrange("p a (u d) -> p (a u) d", u=2),
                    in1=DTt[:, q * 16:(q + 1) * 16].unsqueeze(2).to_broadcast([128, 16, 64]), op=ALU.mult),
                    reads=[bank, DTt], writes=[("XDT", q)])
            if CUT <= 4:
                return
            mk.op("dve", lambda e: e.tensor_tensor(
                out=XDD[:].rearrange("p (h d) -> p h d", h=64), in0=XDT[:].rearrange("p (h d) -> p h d", h=64),
                in1=DECST[:].unsqueeze(2).to_broadcast([128, 64, 64]), op=ALU.mult),
                reads=[("XDT", q) for q in range(4)] + [DECST], writes=[XDD])
            if CUT <= 5:
                return
            bank = B[0]
            pv = bf(bank, None)

            def fb(e, pv=pv):
                ins = None
                for g in range(8):
                    ins = e.transpose(pv[:, g, :], bt[:, g, :], C.ident_b[:])
                return ins
            mk.op("pe", fb, reads=[bt, C.ident_b], writes=[bank])
            mk.op("act", lambda e, pv=pv: e.activation(out=BTOK[:], in_=pv, func=AF.Identity), reads=[bank], writes=[BTOK])
            if CUT <= 6:
                return
            acnt = [0]

            def lmat(g):
                mt = MT[g % 2]
                HS = HS_[c % 2]
                for q2 in range(2):
                    pa = B[4 + acnt[0] % 2]
                    lt = LT[acnt[0] % 2]
                    acnt[0] += 1

                    def fa(e, pa=pa, g=g, q2=q2):
                        ins = None
                        for i in range(4):
                            h = g * 8 + q2 * 4 + i
                            o = pa[:, i * 128:(i + 1) * 128]
                            for k in range(3):
                                e.matmul(o, SEL[:, h, :], HS[k][:], start=(k == 0), stop=False)
                            ins = e.matmul(o, C.ident_b[:], NEGMB[:], start=False, stop=True)
                        return ins
                    mk.op("pe", fa, reads=[HS[0], HS[1], HS[2], SEL, NEGMB, C.ident_b], writes=[pa])

                    def fe(e, pa=pa, lt=lt, g=g, q2=q2):
                        ins = None
                        for i in range(4):
                            h = g * 8 + q2 * 4 + i
                            ins = e.activation(out=lt[:, i, :], in_=pa[:, i * 128:(i + 1) * 128], func=AF.Exp,
                                               bias=NACS[:, h:h + 1], scale=1.0)
                        return ins
                    mk.op("act", fe, reads=[pa, NACS], writes=[lt])
                    mk.op("dve", lambda e, lt=lt, mt=mt, g=g, q2=q2: e.tensor_tensor(
                        out=mt[:, q2 * 4:(q2 + 1) * 4, :], in0=lt[:],
                        in1=GT[:, g, :].unsqueeze(1).to_broadcast([128, 4, 128]), op=ALU.mult),
                        reads=[lt, GT], writes=[(mt.name, q2)])

            if full:
                lmat(0)
            for g in range(8):
                if full:
                    mt = MT[g % 2]
                    if g + 1 < 8:
                        lmat(g + 1)

                    def fy(e, mt=mt, g=g):
                        ins = None
                        for hh in range(8):
                            h = g * 8 + hh
                            ins = e.matmul(B[6][:, hh * 64:(hh + 1) * 64], mt[:, hh, :], XDT[:, h * 64:(h + 1) * 64],
                                           start=True, stop=True)
                        return ins
                    mk.op("pe", fy, reads=[(mt.name, 0), (mt.name, 1), ("XDT", g // 2)], writes=[B[6]])
                    mk.op("pe", lambda e, g=g: e.matmul(B[7][:], ct[:, g, :], STBF[:, g * 512:(g + 1) * 512], start=True, stop=True),
                          reads=[ct, ("STBF", g)], writes=[B[7]])
                    tmp, tm2 = TMP[g % 2], TM2[g % 2]
                    mk.op("dve", lambda e, tmp=tmp, g=g: e.tensor_tensor(
                        out=tmp[:].rearrange("p (h d) -> p h d", h=8), in0=B[7][:].rearrange("p (h d) -> p h d", h=8),
                        in1=EA[:, g * 8:(g + 1) * 8].unsqueeze(2).to_broadcast([128, 8, 64]), op=ALU.mult),
                        reads=[B[7], EA], writes=[tmp])
                    mk.op("dve", lambda e, tm2=tm2, g=g: e.tensor_tensor(
                        out=tm2[:].rearrange("p (h d) -> p h d", h=8),
                        in0=XTOK[:, g * 512:(g + 1) * 512].rearrange("p (h d) -> p h d", h=8),
                        in1=DSK[:, g * 8:(g + 1) * 8].unsqueeze(2).to_broadcast([128, 8, 64]), op=ALU.mult),
                        reads=[("XTOK", g // 2), DSK], writes=[tm2])
                    mk.op("pool", lambda e, tmp=tmp, tm2=tm2: e.tensor_tensor(out=tm2[:], in0=tm2[:], in1=tmp[:], op=ALU.add),
                          reads=[tmp, tm2], writes=[tm2])
                    mk.op("dve", lambda e, tm2=tm2, g=g: e.tensor_tensor(out=YB[:, g * 512:(g + 1) * 512], in0=B[6][:], in1=tm2[:],
                                                                         op=ALU.add),
                          reads=[B[6], tm2], writes=[("YB", g)])
                pst = B[4 + g % 2] if not full else B[g % 2]
                mk.op("pe", lambda e, pst=pst, g=g: e.matmul(pst[:], BTOK[:, g, :], XDD[:, g * 512:(g + 1) * 512], start=True, stop=True),
                      reads=[BTOK, XDD], writes=[pst])
                eng = "dve"
                mk.op(eng, lambda e, g=g: e.tensor_tensor(
                    out=STATE[:, g * 512:(g + 1) * 512].rearrange("p (h d) -> p h d", h=8),
                    in0=STATE[:, g * 512:(g + 1) * 512].rearrange("p (h d) -> p h d", h=8),
                    in1=DEC[:, g * 8:(g + 1) * 8].unsqueeze(2).to_broadcast([128, 8, 64]), op=ALU.mult),
                    reads=[("ST", g), DEC], writes=[("ST", g)])
                mk.op("dve", lambda e, g=g, pst=pst: e.tensor_tensor(out=STATE[:, g * 512:(g + 1) * 512],
                                                                     in0=STATE[:, g * 512:(g + 1) * 512], in1=pst[:], op=ALU.add),
                      reads=[("ST", g), pst], writes=[("ST", g)])
                if full:
                    mk.op("act", lambda e, g=g: e.activation(out=STBF[:, g * 512:(g + 1) * 512], in_=STATE[:, g * 512:(g + 1) * 512],
                                                             func=AF.Identity),
                          reads=[("ST", g)], writes=[("STBF", g)])
            if not full:
                return
            if CUT <= 28:
                return
            zs = ZSc[j]
            for q in range(4):
                bank = B[q % 2]
                pv = bf(bank, None)

                def fq(e, q=q, pv=pv):
                    ins = None
                    for i in range(8):
                        kc = q * 8 + i
                        ins = e.transpose(pv[:, i, :], YB[:, kc * 128:(kc + 1) * 128], C.ident_b[:])
                    return ins
                mk.op("pe", fq, reads=[("YB", 2 * q), ("YB", 2 * q + 1), C.ident_b], writes=[bank])
                mk.op("dve", lambda e, q=q, pv=pv: e.tensor_tensor(out=YG[:, q * 8:(q + 1) * 8, :], in0=pv,
                                                                    in1=zs[:, q * 8:(q + 1) * 8, :], op=ALU.mult),
                      reads=[bank, (zs.name, q)], writes=[("YG", q)])
                mk.op("act", lambda e, q=q: e.activation(out=SQG[:, q * 8:(q + 1) * 8, :], in_=YG[:, q * 8:(q + 1) * 8, :], func=AF.Square),
                      reads=[("YG", q)], writes=[("SQG", q)])
            if CUT <= 29:
                return
            for hf in range(2):
                def fs(e, hf=hf):
                    ins = None
                    for g4 in range(4):
                        g = hf * 4 + g4
                        for i in range(4):
                            ins = e.matmul(B[2 + hf][:, g4 * 128:(g4 + 1) * 128], C.ones_b[:], SQG[:, g * 4 + i, :],
                                           start=(i == 0), stop=(i == 3))
                    return ins
                mk.op("pe", fs, reads=[("SQG", 2 * hf), ("SQG", 2 * hf + 1), C.ones_b], writes=[B[2 + hf]])
                rs = RSTD[:, hf * 4:(hf + 1) * 4, :].rearrange("p a b -> p (a b)")
                mk.op("act", lambda e, hf=hf, rs=rs: e.activation(out=rs, in_=B[2 + hf][:], func=AF.Sqrt, bias=C.eps[:, 0:1],
                                                                   scale=1.0 / 512.0),
                      reads=[B[2 + hf], C.eps], writes=[("RSTD", hf)])
                mk.op("dve", lambda e, rs=rs: e.reciprocal(out=rs, in_=rs), reads=[("RSTD", hf)], writes=[("RSTD", hf)])

            if CUT <= 30:
                return

            def fn(e):
                ins = None
                for kc in range(32):
                    ins = e.scalar_tensor_tensor(out=YNo[:, kc, :], in0=YG[:, kc, :], scalar=NW[:, kc:kc + 1],
                                                 in1=RSTD[:, kc // 4, :], op0=ALU.mult, op1=ALU.mult)
                return ins
            mk.op("dve", fn, reads=[("YG", q) for q in range(4)] + [("RSTD", 0), ("RSTD", 1), NW], writes=[YNo])
            mk.dma("sp", io.YN.ap()[:, c * 128:(c + 1) * 128].rearrange("(kc p) t -> p kc t", p=128), YNo[:])

        mk.op("pool", lambda e: e.memset(STATE[:], 0.0), writes=[("ST", g) for g in range(8)])

        def run_pass(full):
            loads_small(0)
            if NCH > 1:
                loads_small(1)
            loads(0)
            if full:
                loadz(0)
            chunk(0, full, "dt")
            for c in range(NCH):
                if c + 2 < NCH:
                    loads_small(c + 2)
                if c + 1 < NCH:
                    loads(c + 1)
                    if full:
                        loadz(c + 1)
                    chunk(c + 1, full, "dt")
                chunk(c, full, "main")
        run_pass(False)
        if mode == "pre":
            mk.dma("sp", io.st_src.ap(), STATE[:], reads=[("ST", g) for g in range(8)], writes=[io.st_src])
            return
        mk.dma("sp", io.st_src.ap(), STATE[:], reads=[("ST", g) for g in range(8)], writes=[io.st_src])
        mk.cc_allgather(io.st_dst, io.st_src, PAIRS)
        mk.dma("sp", STATE[:], io.st_dst.ap()[0:128, :], reads=[io.st_dst], writes=[("ST", g) for g in range(8)])
        mk.op("dve", lambda e: e.tensor_scalar(out=STATE[:], in0=STATE[:], scalar1=PMK[:, 0:1], scalar2=None, op0=ALU.mult),
              reads=[("ST", g) for g in range(8)] + [PMK], writes=[("ST", g) for g in range(8)])
        mk.op("act", lambda e: e.activation(out=STBF[:], in_=STATE[:], func=AF.Identity),
              reads=[("ST", g) for g in range(8)], writes=[("STBF", g) for g in range(8)])
        if mode == "precc":
            return
        run_pass(True)


def ssd_stage3(mk, io, C, src, dst):
    li = 1
    g1 = C.MOD[li][:, 32:48]
    with mk.scope():
        YT = [mk.sb(f"YT3{j}", [128, 32, 512], BF16) for j in range(2)]
        W = mk.sb("W3", [128, 32, D], BF16)
        PO = [mk.ps(f"PO3{j}", [128, 512], F32) for j in range(2)]
        for qq in range(4):
            mk.dma("pool", W[:, :, qq * 512:(qq + 1) * 512],
                   io.ssd_w_out.ap()[:, qq * 512:(qq + 1) * 512].rearrange("(kc p) d -> p kc d", p=128), writes=[("W3", qq)])
        XR = [mk.sb(f"XR3b{j}", [128, 512], F32) for j in range(4)]
        its = [(t, dc) for t in range(NTOK // 512) for dc in range(KC)]

        def ld_yt(t):
            mk.dma("sp", YT[t % 2][:], io.YN.ap()[:, t * 512:(t + 1) * 512].rearrange("(kc p) t -> p kc t", p=128))

        def ld_x(n):
            t, dc = its[n]
            mk.dma("sp", XR[n % 4][:], src.ap()[dc * 128:(dc + 1) * 128, t * 512:(t + 1) * 512])
        ld_yt(0)
        ld_x(0)
        ld_x(1)
        for n, (t, dc) in enumerate(its):
            yt = YT[t % 2]
            if dc == 0 and t + 1 < NTOK // 512:
                ld_yt(t + 1)
            if n + 2 < len(its):
                ld_x(n + 2)
            xr = XR[n % 4]
            po = PO[n % 2]

            def f(e, yt=yt, po=po, dc=dc):
                ins = None
                for kc in range(32):
                    ins = e.matmul(po[:], W[:, kc, dc * 128:(dc + 1) * 128], yt[:, kc, :], start=(kc == 0), stop=(kc == 31))
                return ins
            mk.op("pe", f, reads=[("W3", dc // 4), yt], writes=[po])
            mk.op("dve", lambda e, xr=xr, po=po, dc=dc: e.scalar_tensor_tensor(
                out=xr[:], in0=po[:], scalar=g1[:, dc:dc + 1], in1=xr[:], op0=ALU.mult, op1=ALU.add),
                reads=[po, xr, C.MOD[li]], writes=[xr])
            mk.dma("sp", dst.ap()[dc * 128:(dc + 1) * 128, t * 512:(t + 1) * 512], xr[:])


def phase_ssd(mk, io, C, src, dst, stages=3, mode="full"):
    ssd_stage1(mk, io, C, src)
    if stages >= 2:
        ssd_stage2(mk, io, C, mode)
    if stages >= 3:
        ssd_stage3(mk, io, C, src, dst)


ROPE_THETA = 500000.0
TWO_PI = 6.283185307179586
NB = NTOK // 128


def declare_mix0_io(nc, io, dump=False):
    kd = "ExternalOutput" if dump else "Internal"
    def inp(name, shape, dt=F32):
        t = nc.dram_tensor(name, list(shape), dt, kind="ExternalInput")
        setattr(io, name, t)
    inp("w_in0", [D, 2304])
    inp("w_glu", [1024, 2048])
    inp("w_out0", [D, D])
    inp("pos_tm", [128, NB], I32)
    inp("pos_last", [128, 1], I32)
    inp("rope_inv", [128, 8])
    inp("amask", [128, 256])
    inp("amask_first", [128, 256])
    inp("sinks", [128, 16])
    inp("s5_lam_re", [128, 64])
    inp("s5_lam_im", [128, 64])
    inp("s5_logdt", [128, 64])
    inp("s5_b_re", [128, 64, 16])
    inp("s5_b_im", [128, 64, 16])
    inp("s5_c_re", [128, 64, 16])
    inp("s5_c_im", [128, 64, 16])
    inp("s5_d", [128, 1024])
    inp("cmask", [128, 128])
    inp("jmat", [128, 128])
    inp("kvals", [128, 17])
    inp("mvals", [128, 64])
    io.YACT = nc.dram_tensor("m0_yact", [1024, NTOK], BF16, kind=kd)
    io.YATT = nc.dram_tensor("m0_yatt", [1024, NTOK], BF16, kind=kd)
    io.kv_src = nc.dram_tensor("m0_kv_src", [128, 256], F32)
    io.kv_dst = nc.dram_tensor("m0_kv_dst", [256, 256], F32)
    io.s5_src = nc.dram_tensor("m0_s5_src", [128, 64], F32)
    io.s5_dst = nc.dram_tensor("m0_s5_dst", [256, 64], F32)
    io.SL = nc.dram_tensor("m0_sl", [8, 2, 128, 2048], F32)
    io.UT = nc.dram_tensor("m0_ut2", [8, 128, 2048], BF16)
    io.UCM = nc.dram_tensor("m0_ucm2", [2, 128, 8192], BF16)
    io.S5M = nc.dram_tensor("m0_s5m", [4, 128, 8192], BF16)
    io.YGB = nc.dram_tensor("m0_ygb", [2, 128, 8192], BF16)


def sincos(mk, ang, shape, tmp, tmpi, out_sin, out_cos):
    for (off, dst) in ((0.0, out_sin), (0.25, out_cos)):
        mk.op("dve", lambda e, off=off: e.tensor_scalar(out=tmp, in0=ang, scalar1=1.0 / TWO_PI, scalar2=off,
                                                        op0=ALU.mult, op1=ALU.add), reads=[ang], writes=[tmp])
        mk.op("dve", lambda e: e.tensor_copy(out=tmpi, in_=tmp), reads=[tmp], writes=[tmpi])
        mk.op("dve", lambda e, dst=dst: e.tensor_copy(out=dst, in_=tmpi), reads=[tmpi], writes=[dst])
        mk.op("dve", lambda e, dst=dst: e.tensor_tensor(out=tmp, in0=tmp, in1=dst, op=ALU.subtract), reads=[tmp, dst], writes=[tmp])
        mk.op("dve", lambda e, dst=dst: e.tensor_scalar(out=dst, in0=tmp, scalar1=0.5, scalar2=None, op0=ALU.is_gt),
              reads=[tmp], writes=[dst])
        mk.op("dve", lambda e, dst=dst: e.tensor_tensor(out=tmp, in0=tmp, in1=dst, op=ALU.subtract), reads=[tmp, dst], writes=[tmp])
        mk.op("dve", lambda e, dst=dst: e.tensor_scalar(out=dst, in0=tmp, scalar1=-0.5, scalar2=None, op0=ALU.is_lt),
              reads=[tmp], writes=[dst])
        mk.op("dve", lambda e, dst=dst: e.tensor_tensor(out=tmp, in0=tmp, in1=dst, op=ALU.add), reads=[tmp, dst], writes=[tmp])
        mk.op("act", lambda e, dst=dst: e.activation(out=dst, in_=tmp, func=AF.Sin, scale=TWO_PI), reads=[tmp], writes=[dst])


def phase_attn(mk, io, C, src):
    li = 0
    G1, SH1 = C.G1[li], C.MOD[li][:, 0:16]
    with mk.scope():
        W = mk.sb("Wqkv", [128, KC, 1280], BF16)
        KT = mk.sb("KTall", [128, (NB + 1) * 128], BF16)
        VA = mk.sb("VAll", [128, NB + 1, 128], BF16)
        XT = mk.sb("XTa", [128, KC, 128], F32)
        SQ = mk.sb("SQa", [128, KC, 128], BF16)
        RS = mk.sb("RSa", [128, 128], F32)
        HT = mk.sb("HTa", [128, KC, 128], BF16)
        POSI = mk.sb("POSI", [128, NB + 1], I32)
        POSF = mk.sb("POSF", [128, NB + 1], F32)
        INV = mk.sb("INV", [128, 8], F32)
        ANG = mk.sb("ANG", [128, NB + 1, 8], F32)
        TMPA = mk.sb("TMPA", [128, NB + 1, 8], F32)
        TMPI = mk.sb("TMPI", [128, NB + 1, 8], I32)
        SIN = mk.sb("SINt", [128, NB + 1, 8], F32)
        COS = mk.sb("COSt", [128, NB + 1, 8], F32)
        MASK = mk.sb("MASK", [128, 256], F32)
        MASKF = mk.sb("MASKF", [128, 256], F32)
        SINK = mk.sb("SINK", [128, 16], F32)
        PMK = mk.sb("PMKa", [128, 1], F32)
        QF = mk.sb("QF", [128, 1280], F32)
        R1 = mk.sb("R1", [128, 18, 8], F32)
        R2 = mk.sb("R2", [128, 18, 8], F32)
        R3 = mk.sb("R3", [128, 18, 8], F32)
        QB = mk.sb("QB", [128, 8, 2, 64], BF16)
        KB = mk.sb("KB", [128, 128], BF16)
        QT = mk.sb("QT", [128, 8, 128], BF16)
        SC = mk.sb("SC", [128, 8, 256], F32)
        MX = mk.sb("MX", [128, 8], F32)
        NEG = mk.sb("NEGa", [128, 8], F32)
        SUM = mk.sb("SUMa", [128, 8], F32)
        ES = mk.sb("ESa", [128, 8], F32)
        P = mk.sb("Pa", [128, 8, 256], BF16)
        PT = mk.sb("PTa", [128, 2, 8, 128], BF16)
        YTOK = mk.sb("YTOKa", [128, 1024], BF16)
        YTT = mk.sb("YTTa", [128, 8, 128], BF16)
        KVX = mk.sb("KVX", [128, 256], F32)
        B = [mk.ps(f"Ba{j}", [128, 512], F32) for j in range(8)]

        def bfv(bank):
            return bank[:].bitcast(BF16).rearrange("p (a b) -> p a b", a=8)

        mk.dma("pool", W[:], io.w_in0.ap()[:, 1024:2304].rearrange("(kc p) n -> p kc n", p=128))
        mk.dma("sp", POSI[:, 1:NB + 1], io.pos_tm.ap())
        mk.dma("sp", POSI[:, 0:1], io.pos_last.ap())
        mk.dma("sp", INV[:], io.rope_inv.ap())
        mk.dma("sp", MASK[:], io.amask.ap())
        mk.dma("sp", MASKF[:], io.amask_first.ap())
        mk.dma("sp", SINK[:], io.sinks.ap())
        mk.dma("sp", PMK[:], io.pmask.ap())
        mk.op("dve", lambda e: e.tensor_copy(out=POSF[:], in_=POSI[:]), reads=[POSI], writes=[POSF])
        mk.op("dve", lambda e: e.tensor_tensor(out=ANG[:], in0=POSF[:].unsqueeze(2).to_broadcast([128, NB + 1, 8]),
                                               in1=INV[:].unsqueeze(1).to_broadcast([128, NB + 1, 8]), op=ALU.mult),
              reads=[POSF, INV], writes=[ANG])
        sincos(mk, ANG[:], None, TMPA[:], TMPI[:], SIN[:], COS[:])

        def project(col0, ncols_tok, slot, do_q):
            norm_piece(mk, C, src, col0, 128, G1, SH1, XT, SQ, B[7][:, 0:128], RS, HT[:], key=HT)
            ranges = ([(0, 512, B[4]), (512, 512, B[5])] if do_q else []) + [(1024, 256, B[6])]
            for (c0, w, bank) in ranges:
                def f(e, c0=c0, w=w, bank=bank):
                    ins = None
                    for kc in range(KC):
                        ins = e.matmul(bank[:, 0:w], HT[:, kc, :], W[:, kc, c0:c0 + w], start=(kc == 0), stop=(kc == KC - 1))
                    return ins
                mk.op("pe", f, reads=[HT, W], writes=[bank])
                sc = 0.125 if c0 < 1024 else 1.0
                mk.op("act", lambda e, c0=c0, w=w, bank=bank, sc=sc: e.activation(out=QF[:, c0:c0 + w], in_=bank[:, 0:w],
                                                                                func=AF.Identity, scale=sc),
                      reads=[bank], writes=[("QF", c0)])
            h0 = 0 if do_q else 16
            nh = 18 - h0
            qv = QF[:, 0:1152].rearrange("p (h d) -> p h d", d=64)[:, h0:18, :]
            x1, x2 = qv[:, :, 0:8], qv[:, :, 8:16]
            cs = COS[:, slot, :].unsqueeze(1).to_broadcast([128, nh, 8])
            sn = SIN[:, slot, :].unsqueeze(1).to_broadcast([128, nh, 8])
            r1, r2, r3 = R1[:, 0:nh, :], R2[:, 0:nh, :], R3[:, 0:nh, :]
            rd = [("QF", 0), ("QF", 512), ("QF", 1024), COS, SIN]
            mk.op("dve", lambda e: e.tensor_tensor(out=r1, in0=x1, in1=cs, op=ALU.mult), reads=rd, writes=[R1])
            mk.op("dve", lambda e: e.tensor_tensor(out=r2, in0=x2, in1=sn, op=ALU.mult), reads=rd, writes=[R2])
            mk.op("dve", lambda e: e.tensor_tensor(out=r1, in0=r1, in1=r2, op=ALU.subtract), reads=[R1, R2], writes=[R1])
            mk.op("dve", lambda e: e.tensor_tensor(out=r2, in0=x2, in1=cs, op=ALU.mult), reads=rd + [R1], writes=[R2])
            mk.op("dve", lambda e: e.tensor_tensor(out=r3, in0=x1, in1=sn, op=ALU.mult), reads=rd, writes=[R3])
            mk.op("dve", lambda e: e.tensor_tensor(out=x2, in0=r2, in1=r3, op=ALU.add), reads=[R2, R3],
                  writes=[("QF", 0), ("QF", 512), ("QF", 1024)])
            mk.op("dve", lambda e: e.tensor_copy(out=x1, in_=r1), reads=[R1], writes=[("QF", 0), ("QF", 512), ("QF", 1024)])
            mk.op("dve", lambda e: e.tensor_copy(out=KB[:], in_=QF[:, 1024:1152]), reads=[("QF", 1024)], writes=[KB])
            mk.op("pe", lambda e: e.transpose(bfv(B[7])[:, 0, :], KB[:], C.ident_b[:]), reads=[KB, C.ident_b], writes=[B[7]])
            mk.op("act", lambda e, slot=slot: e.activation(out=KT[:, slot * 128:(slot + 1) * 128], in_=bfv(B[7])[:, 0, :],
                                                           func=AF.Identity), reads=[B[7]], writes=[("KT", slot)])
            mk.op("act", lambda e, slot=slot: e.activation(out=VA[:, slot, :], in_=QF[:, 1152:1280], func=AF.Identity),
                  reads=[("QF", 1024)], writes=[("VA", slot)])

        project(NTOK - 128, 128, NB, False)
        mk.op("dve", lambda e: e.tensor_copy(out=KVX[:, 0:128], in_=KT[:, NB * 128:(NB + 1) * 128]), reads=[("KT", NB)], writes=[KVX])
        mk.op("dve", lambda e: e.tensor_copy(out=KVX[:, 128:256], in_=VA[:, NB, :]), reads=[("VA", NB), KVX], writes=[KVX])
        mk.dma("sp", io.kv_src.ap(), KVX[:])
        mk.cc_allgather(io.kv_dst, io.kv_src, PAIRS)
        mk.dma("sp", KVX[:], io.kv_dst.ap()[0:128, :])
        mk.op("dve", lambda e: e.tensor_copy(out=KT[:, 0:128], in_=KVX[:, 0:128]), reads=[KVX], writes=[("KT", 0)])
        mk.op("dve", lambda e: e.tensor_copy(out=VA[:, 0, :], in_=KVX[:, 128:256]), reads=[KVX], writes=[("VA", 0)])

        for blk in range(NB):
            project(blk * 128, 128, blk + 1, True)
            mk.op("dve", lambda e: e.tensor_copy(out=QB[:], in_=QF[:, 0:1024].rearrange("p (u i d) -> p i u d", u=2, i=8)),
                  reads=[("QF", 0), ("QF", 512)], writes=[QB])

            def ftq(e):
                ins = None
                for i in range(8):
                    ins = e.transpose(bfv(B[7])[:, i, :], QB[:, i, :, :].rearrange("p u d -> p (u d)"), C.ident_b[:])
                return ins
            mk.op("pe", ftq, reads=[QB, C.ident_b], writes=[B[7]])
            mk.op("act", lambda e: e.activation(out=QT[:], in_=bfv(B[7]), func=AF.Identity), reads=[B[7]], writes=[QT])
            msk = MASKF if blk == 0 else MASK
            for half in range(2):
                def fs(e, half=half, blk=blk):
                    ins = None
                    for i in range(8):
                        ins = e.matmul(B[i // 2][:, (i % 2) * 256:(i % 2) * 256 + 256],
                                       QT[half * 64:(half + 1) * 64, i, :],
                                       KT[half * 64:(half + 1) * 64, blk * 128:blk * 128 + 256], start=True, stop=True)
                    return ins
                mk.op("pe", fs, reads=[QT, ("KT", blk), ("KT", blk + 1)], writes=[B[0], B[1], B[2], B[3]])
                for jb in range(4):
                    mk.op("dve", lambda e, jb=jb, msk=msk: e.tensor_tensor(
                        out=SC[:, jb * 2:jb * 2 + 2, :], in0=B[jb][:].rearrange("p (a b) -> p a b", a=2),
                        in1=msk[:].unsqueeze(1).to_broadcast([128, 2, 256]), op=ALU.add),
                        reads=[B[jb], msk], writes=[("SC", jb)])
                scr = [("SC", jb) for jb in range(4)]
                mk.op("dve", lambda e: e.tensor_reduce(out=MX[:], in_=SC[:], axis=AX.X, op=ALU.max), reads=scr, writes=[MX])
                mk.op("dve", lambda e, half=half: e.tensor_tensor(out=MX[:], in0=MX[:], in1=SINK[:, half * 8:(half + 1) * 8], op=ALU.max),
                      reads=[MX, SINK], writes=[MX])
                mk.op("dve", lambda e: e.tensor_scalar(out=NEG[:], in0=MX[:], scalar1=-1.0, scalar2=None, op0=ALU.mult),
                      reads=[MX], writes=[NEG])
                mk.op("pool", lambda e: e.memset(SUM[:], 0.0), writes=[SUM])

                def fe(e):
                    ins = None
                    for i in range(8):
                        ins = e.activation(out=P[:, i, :], in_=SC[:, i, :], func=AF.Exp, bias=NEG[:, i:i + 1], scale=1.0,
                                           accum_out=SUM[:, i:i + 1])
                    return ins
                mk.op("act", fe, reads=scr + [NEG, SUM], writes=[P, SUM])
                mk.op("dve", lambda e, half=half: e.tensor_tensor(out=ES[:], in0=SINK[:, half * 8:(half + 1) * 8], in1=MX[:], op=ALU.subtract),
                      reads=[SINK, MX], writes=[ES])
                mk.op("act", lambda e: e.activation(out=ES[:], in_=ES[:], func=AF.Exp), reads=[ES], writes=[ES])
                mk.op("dve", lambda e: e.tensor_tensor(out=ES[:], in0=ES[:], in1=SUM[:], op=ALU.add), reads=[ES, SUM], writes=[ES])
                mk.op("dve", lambda e: e.reciprocal(out=ES[:], in_=ES[:]), reads=[ES], writes=[ES])
                for kj in range(2):
                    bank = B[4 + kj]

                    def fpt(e, kj=kj, bank=bank):
                        ins = None
                        for i in range(8):
                            ins = e.transpose(bfv(bank)[:, i, :], P[:, i, kj * 128:(kj + 1) * 128], C.ident_b[:])
                        return ins
                    mk.op("pe", fpt, reads=[P, C.ident_b], writes=[bank])
                    mk.op("act" if kj == 0 else "dve", (lambda e, kj=kj, bank=bank: e.activation(out=PT[:, kj, :, :], in_=bfv(bank), func=AF.Identity))
                          if kj == 0 else (lambda e, kj=kj, bank=bank: e.tensor_copy(out=PT[:, kj, :, :], in_=bfv(bank))),
                          reads=[bank], writes=[("PT", kj)])

                def fpv(e, half=half, blk=blk):
                    ins = None
                    for i in range(8):
                        for kj in range(2):
                            ins = e.matmul(B[6][:, i * 64:(i + 1) * 64], PT[:, kj, i, :],
                                           VA[:, blk + kj, half * 64:(half + 1) * 64], start=(kj == 0), stop=(kj == 1))
                    return ins
                mk.op("pe", fpv, reads=[("PT", 0), ("PT", 1), ("VA", blk), ("VA", blk + 1)], writes=[B[6]])
                mk.op("dve", lambda e, half=half: e.tensor_tensor(
                    out=YTOK[:, half * 512:(half + 1) * 512].rearrange("p (h d) -> p h d", h=8),
                    in0=B[6][:].rearrange("p (h d) -> p h d", h=8),
                    in1=ES[:].unsqueeze(2).to_broadcast([128, 8, 64]), op=ALU.mult),
                    reads=[B[6], ES], writes=[("YTOK", half)])

            def fty(e):
                ins = None
                for i in range(8):
                    ins = e.transpose(bfv(B[7])[:, i, :], YTOK[:, i * 128:(i + 1) * 128], C.ident_b[:])
                return ins
            mk.op("pe", fty, reads=[("YTOK", 0), ("YTOK", 1), C.ident_b], writes=[B[7]])
            mk.op("act", lambda e: e.activation(out=YTT[:], in_=bfv(B[7]), func=AF.Identity), reads=[B[7]], writes=[YTT])
            mk.dma("sp", io.YATT.ap()[:, blk * 128:(blk + 1) * 128].rearrange("(kc p) t -> p kc t", p=128), YTT[:])


def wrap_turns(mk, t, tmpi, tmpf):
    mk.op("dve", lambda e: e.tensor_copy(out=tmpi, in_=t), reads=[t], writes=[tmpi])
    mk.op("dve", lambda e: e.tensor_copy(out=tmpf, in_=tmpi), reads=[tmpi], writes=[tmpf])
    mk.op("dve", lambda e: e.tensor_tensor(out=t, in0=t, in1=tmpf, op=ALU.subtract), reads=[t, tmpf], writes=[t])
    mk.op("dve", lambda e: e.tensor_scalar(out=tmpf, in0=t, scalar1=0.5, scalar2=None, op0=ALU.is_gt), reads=[t], writes=[tmpf])
    mk.op("dve", lambda e: e.tensor_tensor(out=t, in0=t, in1=tmpf, op=ALU.subtract), reads=[t, tmpf], writes=[t])
    mk.op("dve", lambda e: e.tensor_scalar(out=tmpf, in0=t, scalar1=-0.5, scalar2=None, op0=ALU.is_lt), reads=[t], writes=[tmpf])
    mk.op("dve", lambda e: e.tensor_tensor(out=t, in0=t, in1=tmpf, op=ALU.add), reads=[t, tmpf], writes=[t])


def sincos_turns(mk, t, tmpi, tmpf, t2, out_sin, out_cos):
    mk.op("dve", lambda e: e.tensor_scalar(out=t2, in0=t, scalar1=0.25, scalar2=None, op0=ALU.add), reads=[t], writes=[t2])
    wrap_turns(mk, t, tmpi, tmpf)
    mk.op("act", lambda e: e.activation(out=out_sin, in_=t, func=AF.Sin, scale=TWO_PI), reads=[t], writes=[out_sin])
    wrap_turns(mk, t2, tmpi, tmpf)
    mk.op("act", lambda e: e.activation(out=out_cos, in_=t2, func=AF.Sin, scale=TWO_PI), reads=[t2], writes=[out_cos])


def cmul(mk, outr, outi, ar, ai, br, bi, t1, t2):
    rd = [ar, ai, br, bi]
    mk.op("dve", lambda e: e.tensor_tensor(out=t1, in0=ar, in1=br, op=ALU.mult), reads=rd, writes=[t1])
    mk.op("dve", lambda e: e.tensor_tensor(out=t2, in0=ai, in1=bi, op=ALU.mult), reads=rd, writes=[t2])
    mk.op("dve", lambda e: e.tensor_tensor(out=outr, in0=t1, in1=t2, op=ALU.subtract), reads=[t1, t2], writes=[outr])
    mk.op("dve", lambda e: e.tensor_tensor(out=t1, in0=ar, in1=bi, op=ALU.mult), reads=rd + [outr], writes=[t1])
    mk.op("dve", lambda e: e.tensor_tensor(out=t2, in0=ai, in1=br, op=ALU.mult), reads=rd + [outr], writes=[t2])
    mk.op("dve", lambda e: e.tensor_tensor(out=outi, in0=t1, in1=t2, op=ALU.add), reads=[t1, t2], writes=[outi])


def phase_s5(mk, io, C, src):
    li = 0
    G1, SH1 = C.G1[li], C.MOD[li][:, 0:16]
    NBLK = NTOK // 1024
    NSB = NTOK // 256
    with mk.scope():
        E1R = mk.sb("E1R", [128, 64, 32]); E1I = mk.sb("E1I", [128, 64, 32])
        E2R = mk.sb("E2R", [128, 64, 32]); E2I = mk.sb("E2I", [128, 64, 32])
        RHO0 = mk.sb("RHO0", [128, 64, 32]); RHO = mk.sb("RHO", [128, 64])
        E32R = mk.sb("E32R", [128, 64]); E32I = mk.sb("E32I", [128, 64])
        JM = mk.sb("JM", [128, 128]); W0 = mk.sb("W0", [128, 64]); PMK = mk.sb("PMK5", [128, 1])
        mk.dma("sp", JM[:], io.jmat.ap())
        mk.dma("sp", PMK[:], io.pmask.ap())
        with mk.scope():
            LR = mk.sb("LR", [128, 64]); LI = mk.sb("LI", [128, 64]); DTv = mk.sb("DTv", [128, 64])
            Av = mk.sb("Av", [128, 64]); F0 = mk.sb("F0", [128, 64]); F8 = mk.sb("F8", [128, 64])
            KV = mk.sb("KV", [128, 17]); MV = mk.sb("MV", [128, 64]); CMK = mk.sb("CMK", [128, 128])
            MAG = mk.sb("MAG", [128, 17, 64]); TT = mk.sb("TTp", [128, 17, 64]); TT2 = mk.sb("TT2p", [128, 17, 64])
            TTI = mk.sb("TTIp", [128, 17, 64], I32); TTF = mk.sb("TTFp", [128, 17, 64])
            PR = mk.sb("PRp", [128, 17, 64]); PI = mk.sb("PIp", [128, 17, 64])
            s64 = {n: mk.sb(n, [128, 64]) for n in ("ca", "cb", "den", "cr", "ci", "t1", "t2", "t3")}
            s64i = mk.sb("s64i", [128, 64], I32)
            BR = mk.sb("BRs", [128, 64, 16]); BI = mk.sb("BIs", [128, 64, 16])
            CR = mk.sb("CRs", [128, 64, 16]); CI = mk.sb("CIs", [128, 64, 16])
            BBR = mk.sb("BBR", [128, 64, 16]); BBI = mk.sb("BBI", [128, 64, 16])
            X1 = mk.sb("X1s", [128, 64, 16]); X2 = mk.sb("X2s", [128, 64, 16])
            for t, s in ((LR, io.s5_lam_re), (LI, io.s5_lam_im), (DTv, io.s5_logdt), (KV, io.kvals), (MV, io.mvals),
                         (CMK, io.cmask), (BR, io.s5_b_re), (BI, io.s5_b_im), (CR, io.s5_c_re), (CI, io.s5_c_im)):
                mk.dma("sp", t[:], s.ap())
            mk.op("act", lambda e: e.activation(out=DTv[:], in_=DTv[:], func=AF.Exp), reads=[DTv], writes=[DTv])
            mk.op("dve", lambda e: e.tensor_tensor(out=Av[:], in0=DTv[:], in1=LR[:], op=ALU.mult), reads=[DTv, LR], writes=[Av])
            mk.op("dve", lambda e: e.tensor_tensor(out=F0[:], in0=DTv[:], in1=LI[:], op=ALU.mult), reads=[DTv, LI], writes=[F0])
            mk.op("dve", lambda e: e.tensor_scalar(out=F0[:], in0=F0[:], scalar1=1.0 / TWO_PI, scalar2=None, op0=ALU.mult),
                  reads=[F0], writes=[F0])
            wrap_turns(mk, F0[:], s64i[:], s64["t1"][:])
            mk.op("dve", lambda e: e.tensor_scalar(out=F8[:], in0=F0[:], scalar1=8.0, scalar2=None, op0=ALU.mult), reads=[F0], writes=[F8])
            wrap_turns(mk, F8[:], s64i[:], s64["t1"][:])
            kvb = KV[:].unsqueeze(2).to_broadcast([128, 17, 64])
            mk.op("dve", lambda e: e.tensor_tensor(out=MAG[:], in0=kvb, in1=Av[:].unsqueeze(1).to_broadcast([128, 17, 64]), op=ALU.mult),
                  reads=[KV, Av], writes=[MAG])
            mk.op("act", lambda e: e.activation(out=MAG[:], in_=MAG[:], func=AF.Exp), reads=[MAG], writes=[MAG])
            mk.op("dve", lambda e: e.tensor_tensor(out=TT[:], in0=kvb, in1=F0[:].unsqueeze(1).to_broadcast([128, 17, 64]), op=ALU.mult),
                  reads=[KV, F0], writes=[TT])
            sincos_turns(mk, TT[:], TTI[:], TTF[:], TT2[:], PI[:], PR[:])
            mk.op("dve", lambda e: e.tensor_tensor(out=PR[:], in0=PR[:], in1=MAG[:], op=ALU.mult), reads=[PR, MAG], writes=[PR])
            mk.op("dve", lambda e: e.tensor_tensor(out=PI[:], in0=PI[:], in1=MAG[:], op=ALU.mult), reads=[PI, MAG], writes=[PI])
            mk.op("dve", lambda e: e.tensor_copy(out=RHO[:], in_=MAG[:, 16, :]), reads=[MAG], writes=[RHO])
            mk.op("dve", lambda e: e.tensor_copy(out=RHO0[:], in_=RHO[:].unsqueeze(2).to_broadcast([128, 64, 32])), reads=[RHO], writes=[RHO0])
            mk.op("dve", lambda e: e.memset(RHO0[:, :, 0:1], 0.0), reads=[RHO0], writes=[RHO0])
            s = s64
            mk.op("dve", lambda e: e.tensor_scalar(out=s["ca"][:], in0=PR[:, 9, :], scalar1=-1.0, scalar2=None, op0=ALU.add), reads=[PR], writes=[s["ca"]])
            mk.op("dve", lambda e: e.tensor_copy(out=s["cb"][:], in_=PI[:, 9, :]), reads=[PI], writes=[s["cb"]])
            mk.op("dve", lambda e: e.tensor_tensor(out=s["den"][:], in0=LR[:], in1=LR[:], op=ALU.mult), reads=[LR], writes=[s["den"]])
            mk.op("dve", lambda e: e.tensor_tensor(out=s["t1"][:], in0=LI[:], in1=LI[:], op=ALU.mult), reads=[LI], writes=[s["t1"]])
            mk.op("dve", lambda e: e.tensor_tensor(out=s["den"][:], in0=s["den"][:], in1=s["t1"][:], op=ALU.add), reads=[s["den"], s["t1"]], writes=[s["den"]])
            mk.op("dve", lambda e: e.reciprocal(out=s["den"][:], in_=s["den"][:]), reads=[s["den"]], writes=[s["den"]])
            mk.op("dve", lambda e: e.tensor_scalar(out=s["t3"][:], in0=LI[:], scalar1=-1.0, scalar2=None, op0=ALU.mult), reads=[LI], writes=[s["t3"]])
            cmul(mk, s["cr"][:], s["ci"][:], s["ca"][:], s["cb"][:], LR[:], s["t3"][:], s["t1"][:], s["t2"][:])
            mk.op("dve", lambda e: e.tensor_tensor(out=s["cr"][:], in0=s["cr"][:], in1=s["den"][:], op=ALU.mult), reads=[s["cr"], s["den"]], writes=[s["cr"]])
            mk.op("dve", lambda e: e.tensor_tensor(out=s["ci"][:], in0=s["ci"][:], in1=s["den"][:], op=ALU.mult), reads=[s["ci"], s["den"]], writes=[s["ci"]])
            crb = s["cr"][:].unsqueeze(2).to_broadcast([128, 64, 16])
            cib = s["ci"][:].unsqueeze(2).to_broadcast([128, 64, 16])
            cmul(mk, BBR[:], BBI[:], crb, cib, BR[:], BI[:], X1[:], X2[:])
            with mk.scope():
                ET = mk.sb("ETs", [128, 64, 32]); ET2 = mk.sb("ET2s", [128, 64, 32]); ETF = mk.sb("ETFs", [128, 64, 32])
                ETI = mk.sb("ETIs", [128, 64, 32], I32)
                for (m0, ER_, EI_) in ((0, E1R, E1I), (32, E2R, E2I)):
                    mk.op("dve", lambda e, m0=m0: e.tensor_tensor(out=ET[:], in0=F8[:].unsqueeze(2).to_broadcast([128, 64, 32]),
                                                                  in1=MV[:, m0:m0 + 32].unsqueeze(1).to_broadcast([128, 64, 32]), op=ALU.mult),
                          reads=[F8, MV], writes=[ET])
                    sincos_turns(mk, ET[:], ETI[:], ETF[:], ET2[:], EI_[:], ER_[:])
            mk.op("dve", lambda e: e.tensor_scalar(out=s["t3"][:], in0=F8[:], scalar1=32.0, scalar2=None, op0=ALU.mult), reads=[F8], writes=[s["t3"]])
            sincos_turns(mk, s["t3"][:], s64i[:], s["t1"][:], s["t2"][:], E32I[:], E32R[:])
            sh = [128, 8, 8, 16]
            names = ("UR", "UI", "US", "VR", "VI", "CLR", "CLI", "WR", "WI", "WA", "WB", "Q1", "Q2")
            T = {n: mk.sb("s5" + n, sh) for n in names}
            OUTM = [mk.sb(f"OUTM{j}", [128, 8, 128], BF16) for j in range(4)]
            PB_ = [mk.ps(f"PBs{j}", [128, 512], F32) for j in range(3)]
            for gb in range(8):
                gs = slice(gb * 8, gb * 8 + 8)

                def pw(tab, k0):
                    return tab[:, k0:k0 + 8, gs].rearrange("p r g -> p g r").unsqueeze(3).to_broadcast(sh)

                def bq(tab):
                    return tab[:, gs, :].unsqueeze(2).to_broadcast(sh)
                cmul(mk, T["UR"][:], T["UI"][:], pw(PR, 0), pw(PI, 0), bq(BBR), bq(BBI), T["Q1"][:], T["Q2"][:])
                cmul(mk, T["VR"][:], T["VI"][:], bq(CR), bq(CI), pw(PR, 8), pw(PI, 8), T["Q1"][:], T["Q2"][:])
                cmul(mk, T["CLR"][:], T["CLI"][:], bq(CR), bq(CI), pw(PR, 9), pw(PI, 9), T["Q1"][:], T["Q2"][:])
                p7r = PR[:, 15, gs].unsqueeze(2).unsqueeze(3).to_broadcast(sh)
                p7i = PI[:, 15, gs].unsqueeze(2).unsqueeze(3).to_broadcast(sh)
                cmul(mk, T["WR"][:], T["WI"][:], p7r, p7i, T["UR"][:], T["UI"][:], T["Q1"][:], T["Q2"][:])

                def stack(dst, top, bot, neg_top=False, neg_bot=False):
                    for (ps_, srcT, ng) in ((slice(0, 64), top, neg_top), (slice(64, 128), bot, neg_bot)):
                        if ng:
                            mk.op("dve", lambda e, ps_=ps_, srcT=srcT: e.tensor_scalar(out=dst[ps_], in0=srcT[ps_], scalar1=-1.0,
                                                                                     scalar2=None, op0=ALU.mult),
                                  reads=[srcT], writes=[dst])
                        else:
                            mk.op("dve", lambda e, ps_=ps_, srcT=srcT: e.tensor_copy(out=dst[ps_], in_=srcT[ps_]), reads=[srcT], writes=[dst])
                stack(T["US"], T["UR"], T["UI"])
                stack(T["VR"], T["VR"], T["VI"], neg_bot=True)
                stack(T["CLR"], T["CLR"], T["CLI"], neg_bot=True)
                stack(T["WA"], T["WR"], T["WI"])
                stack(T["WB"], T["WI"], T["WR"], neg_top=True)
                mk.op("act", lambda e: e.activation(out=OUTM[1][:], in_=T["CLR"][:].rearrange("p g r q -> p g (r q)"), func=AF.Identity),
                      reads=[T["CLR"]], writes=[OUTM[1]])
                for g4 in range(2):
                    def fk(e, g4=g4):
                        ins = None
                        for i in range(4):
                            g = g4 * 4 + i
                            us = T["US"][:, g].rearrange("p r q -> p (r q)")
                            e.matmul(PB_[0][:, i * 128:(i + 1) * 128], us, T["VR"][:, g].rearrange("p r q -> p (r q)"), start=True, stop=True)
                            e.matmul(PB_[1][:, i * 128:(i + 1) * 128], T["WA"][:, g].rearrange("p r q -> p (r q)"), C.ident_f[:], start=True, stop=True)
                            ins = e.matmul(PB_[2][:, i * 128:(i + 1) * 128], T["WB"][:, g].rearrange("p r q -> p (r q)"), C.ident_f[:], start=True, stop=True)
                        return ins
                    mk.op("pe", fk, reads=[T["US"], T["VR"], T["WA"], T["WB"], C.ident_f], writes=[PB_[0], PB_[1], PB_[2]])
                    mk.op("dve", lambda e, g4=g4: e.tensor_tensor(
                        out=OUTM[0][:, g4 * 4:(g4 + 1) * 4, :], in0=PB_[0][:].rearrange("p (a b) -> p a b", a=4),
                        in1=CMK[:].unsqueeze(1).to_broadcast([128, 4, 128]), op=ALU.mult), reads=[PB_[0], CMK], writes=[OUTM[0]])
                    mk.op("act", lambda e, g4=g4: e.activation(out=OUTM[2][:, g4 * 4:(g4 + 1) * 4, :].rearrange("p a b -> p (a b)"),
                                                                 in_=PB_[1][:], func=AF.Identity), reads=[PB_[1]], writes=[OUTM[2]])
                    mk.op("act", lambda e, g4=g4: e.activation(out=OUTM[3][:, g4 * 4:(g4 + 1) * 4, :].rearrange("p a b -> p (a b)"),
                                                                 in_=PB_[2][:], func=AF.Identity), reads=[PB_[2]], writes=[OUTM[3]])
                for x in range(4):
                    mk.dma("sp", io.S5M.ap()[x, :, gb * 1024:(gb + 1) * 1024], OUTM[x][:].rearrange("p a b -> p (a b)"))
        with mk.scope():
            WU = mk.sb("WU", [128, KC, 1024], BF16)
            HTb = mk.sb("HTb", [128, KC, 1024], BF16)
            XT = mk.sb("XT5", [128, KC, 256], F32); SQ = mk.sb("SQ5", [128, KC, 256], BF16); RS = mk.sb("RS5", [128, 256], F32)
            UCM = mk.sb("UCM", [128, 8, 1024], BF16)
            UCP = mk.sb("UCP", [128, 64, 8, 16], BF16)
            UT = mk.sb("UT", [128, 64, 128], BF16)
            PS = mk.ps("PS5", [128, 256], F32)
            PU = [mk.ps(f"PU{j}", [128, 512], F32) for j in range(2)]
            PTb = [mk.ps(f"PTb{j}", [128, 512], F32) for j in range(2)]
            mk.dma("pool", WU[:], io.w_in0.ap()[:, 0:1024].rearrange("(kc p) n -> p kc n", p=128))
            for b in range(NBLK):
                for pc in range(4):
                    norm_piece(mk, C, src, b * 1024 + pc * 256, 256, G1, SH1, XT, SQ, PS, RS, HTb[:, :, pc * 256:(pc + 1) * 256], key=HTb)
                n = 0
                for r in range(8):
                    for nh in range(2):
                        pu = PU[n % 2]
                        n += 1

                        def f(e, pu=pu, r=r, nh=nh):
                            ins = None
                            for kc in range(KC):
                                ins = e.matmul(pu[:], HTb[:, kc, r:1024:8], WU[:, kc, nh * 512:(nh + 1) * 512],
                                               start=(kc == 0), stop=(kc == KC - 1))
                            return ins
                        mk.op("pe", f, reads=[HTb, WU], writes=[pu])
                        mk.op("act", lambda e, pu=pu, r=r, nh=nh: e.activation(out=UCM[:, r, nh * 512:(nh + 1) * 512], in_=pu[:], func=AF.Identity),
                              reads=[pu], writes=[("UCM", r, nh)])
                mk.op("dve", lambda e: e.tensor_copy(out=UCP[:], in_=UCM[:].rearrange("p r (g q) -> p g r q", q=16)),
                      reads=[("UCM", r, nh) for r in range(8) for nh in range(2)], writes=[UCP])
                mk.dma("sp", io.UCM.ap()[b], UCP[:].rearrange("p g r q -> p (g r q)"))
                for g8 in range(8):
                    bank = PTb[g8 % 2]
                    pv = bank[:].bitcast(BF16).rearrange("p (a b) -> p a b", a=8)

                    def ft(e, g8=g8, pv=pv):
                        ins = None
                        for i in range(8):
                            ins = e.transpose(pv[:, i, :], UCP[:, g8 * 8 + i].rearrange("p r q -> p (r q)"), C.ident_b[:])
                        return ins
                    mk.op("pe", ft, reads=[UCP, C.ident_b], writes=[bank])
                    mk.op("act" if g8 % 2 == 0 else "dve",
                          (lambda e, g8=g8, pv=pv: e.activation(out=UT[:, g8 * 8:(g8 + 1) * 8, :], in_=pv, func=AF.Identity)) if g8 % 2 == 0
                          else (lambda e, g8=g8, pv=pv: e.tensor_copy(out=UT[:, g8 * 8:(g8 + 1) * 8, :], in_=pv)),
                          reads=[bank], writes=[("UT", g8)])
                for sb in range(4):
                    mk.dma("sp", io.UT.ap()[b * 4 + sb].rearrange("p (g c) -> p g c", c=32), UT[:, :, sb * 32:(sb + 1) * 32],
                           reads=[("UT", g8) for g8 in range(8)], writes=[io.UT.ap()[b * 4 + sb]])

        def scan_step(SLA, SLB, Z, TZ, OUT, need_S, WSH=None, SBF=None, PJ=None, O31=None, PJ1=None, t64=None):
            mk.op("dve", lambda e: e.tensor_tensor(out=Z[:], in0=E1R[:], in1=SLA[:], op=ALU.mult), reads=[E1R, SLA], writes=[Z])
            mk.op("dve", lambda e: e.tensor_tensor(out=TZ[:], in0=E1I[:], in1=SLB[:], op=ALU.mult), reads=[E1I, SLB], writes=[TZ])
            mk.op("dve", lambda e: e.tensor_tensor(out=Z[:], in0=Z[:], in1=TZ[:], op=ALU.add), reads=[Z, TZ], writes=[Z])
            mk.op("dve", lambda e: e.tensor_tensor(out=t64[:], in0=RHO[:], in1=W0[:], op=ALU.mult), reads=[RHO, W0], writes=[t64])
            mk.op("dve", lambda e: e.tensor_tensor(out=Z[:, :, 0], in0=Z[:, :, 0], in1=t64[:], op=ALU.add), reads=[Z, t64], writes=[Z])
            mk.op("dve", lambda e: e.tensor_tensor_scan(out=OUT[:].rearrange("p g c -> p (g c)"), data0=RHO0[:].rearrange("p g c -> p (g c)"),
                                                        data1=Z[:].rearrange("p g c -> p (g c)"), initial=0.0, op0=ALU.mult, op1=ALU.add),
                  reads=[RHO0, Z], writes=[OUT])
            if need_S:
                mk.op("dve", lambda e: e.tensor_copy(out=WSH[:, :, 0], in_=W0[:]), reads=[W0], writes=[("WSH", 0)])
                mk.op("dve", lambda e: e.tensor_copy(out=WSH[:, :, 1:32], in_=OUT[:, :, 0:31]), reads=[OUT], writes=[("WSH", 1)])
                for q in range(4):
                    mk.op("pe", lambda e, q=q: e.matmul(PJ[q][:], JM[:], WSH[:, q * 16:(q + 1) * 16, :].rearrange("p g c -> p (g c)"),
                                                        start=True, stop=True), reads=[JM, ("WSH", 0), ("WSH", 1)], writes=[PJ[q]])
                    mk.op("dve", lambda e, q=q: e.tensor_tensor(out=TZ[:, q * 16:(q + 1) * 16, :].rearrange("p g c -> p (g c)"), in0=PJ[q][:],
                                                                in1=E2I[:, q * 16:(q + 1) * 16, :].rearrange("p g c -> p (g c)"), op=ALU.mult),
                          reads=[PJ[q], E2I], writes=[("TZ", q)])
                mk.op("dve", lambda e: e.tensor_tensor(out=Z[:], in0=E2R[:], in1=WSH[:], op=ALU.mult),
                      reads=[E2R, ("WSH", 0), ("WSH", 1), Z], writes=[Z])
                mk.op("dve", lambda e: e.tensor_tensor(out=SBF[:], in0=Z[:], in1=TZ[:], op=ALU.add),
                      reads=[Z] + [("TZ", q) for q in range(4)] + [TZ], writes=[SBF])
            mk.op("dve", lambda e: e.tensor_copy(out=O31[:], in_=OUT[:, :, 31]), reads=[OUT], writes=[O31])
            mk.op("pe", lambda e: e.matmul(PJ1[:, 0:64], JM[:], O31[:], start=True, stop=True), reads=[JM, O31], writes=[PJ1])
            mk.op("dve", lambda e: e.tensor_tensor(out=t64[:], in0=PJ1[:, 0:64], in1=E32I[:], op=ALU.mult), reads=[PJ1, E32I], writes=[t64])
            mk.op("dve", lambda e: e.tensor_tensor(out=W0[:], in0=O31[:], in1=E32R[:], op=ALU.mult), reads=[O31, E32R], writes=[W0])
            mk.op("dve", lambda e: e.tensor_tensor(out=W0[:], in0=W0[:], in1=t64[:], op=ALU.add), reads=[W0, t64], writes=[W0])

        with mk.scope():
            BLA = mk.sb("BLA", [128, 64, 128], BF16); BLB = mk.sb("BLB", [128, 64, 128], BF16)
            UTs = [mk.sb(f"UTs{j}", [128, 64, 32], BF16) for j in range(2)]
            SLA = mk.sb("SLA", [128, 64, 32]); SLB = mk.sb("SLB", [128, 64, 32])
            Z = mk.sb("Zs", [128, 64, 32]); TZ = mk.sb("TZs", [128, 64, 32]); OUT = mk.sb("OUTs5", [128, 64, 32])
            O31 = mk.sb("O31", [128, 64]); t64 = mk.sb("t64", [128, 64])
            PA_ = [mk.ps(f"PA5{j}", [128, 512], F32) for j in range(2)]
            PB2 = [mk.ps(f"PB5{j}", [128, 512], F32) for j in range(2)]
            PJ1 = mk.ps("PJ1", [128, 512], F32)
            mk.dma("sp", BLA[:].rearrange("p a b -> p (a b)"), io.S5M.ap()[2])
            mk.dma("sp", BLB[:].rearrange("p a b -> p (a b)"), io.S5M.ap()[3])
            mk.op("pool", lambda e: e.memset(W0[:], 0.0), writes=[W0])
            for sbi in range(NSB):
                uts = UTs[sbi % 2]
                mk.dma("sp", uts[:].rearrange("p g c -> p (g c)"), io.UT.ap()[sbi])
                for gq in range(4):
                    pa, pb = PA_[gq % 2], PB2[gq % 2]

                    def f1(e, gq=gq, pa=pa, pb=pb, uts=uts):
                        ins = None
                        for i in range(16):
                            g = gq * 16 + i
                            e.matmul(pa[:, i * 32:(i + 1) * 32], BLA[:, g, :], uts[:, g, :], start=True, stop=True)
                            ins = e.matmul(pb[:, i * 32:(i + 1) * 32], BLB[:, g, :], uts[:, g, :], start=True, stop=True)
                        return ins
                    mk.op("pe", f1, reads=[BLA, BLB, uts], writes=[pa, pb])
                    mk.op("act", lambda e, gq=gq, pa=pa: e.activation(out=SLA[:, gq * 16:(gq + 1) * 16, :].rearrange("p g c -> p (g c)"),
                                                                       in_=pa[:], func=AF.Identity), reads=[pa], writes=[("SLA", gq)])
                    mk.op("dve", lambda e, gq=gq, pb=pb: e.tensor_copy(out=SLB[:, gq * 16:(gq + 1) * 16, :].rearrange("p g c -> p (g c)"), in_=pb[:]),
                          reads=[pb], writes=[("SLB", gq)])
                mk.dma("sp", io.SL.ap()[sbi, 0], SLA[:].rearrange("p g c -> p (g c)"), reads=[("SLA", q) for q in range(4)], writes=[io.SL.ap()[sbi, 0]])
                mk.dma("sp", io.SL.ap()[sbi, 1], SLB[:].rearrange("p g c -> p (g c)"), reads=[("SLB", q) for q in range(4)], writes=[io.SL.ap()[sbi, 1]])
                mk.op("dve", lambda e: e.tensor_copy(out=t64[:], in_=W0[:]),
                      reads=[("SLA", q) for q in range(4)] + [("SLB", q) for q in range(4)] + [W0], writes=[t64, SLA, SLB])
                scan_step(SLA, SLB, Z, TZ, OUT, False, O31=O31, PJ1=PJ1, t64=t64)
            mk.dma("sp", io.s5_src.ap(), W0[:])
            mk.cc_allgather(io.s5_dst, io.s5_src, PAIRS)
            mk.dma("sp", W0[:], io.s5_dst.ap()[0:128, :])
            mk.op("dve", lambda e: e.tensor_scalar(out=W0[:], in0=W0[:], scalar1=PMK[:, 0:1], scalar2=None, op0=ALU.mult),
                  reads=[W0, PMK], writes=[W0])
        with mk.scope():
            KM = mk.sb("KM", [128, 64, 128], BF16); CL = mk.sb("CLm", [128, 64, 128], BF16)
            UTs = [mk.sb(f"UTt{j}", [128, 64, 32], BF16) for j in range(2)]
            SLAs = [mk.sb(f"SLAt{j}", [128, 64, 32]) for j in range(2)]
            SLBs = [mk.sb(f"SLBt{j}", [128, 64, 32]) for j in range(2)]
            Z = mk.sb("Zt", [128, 64, 32]); TZ = mk.sb("TZt", [128, 64, 32]); OUT = mk.sb("OUTt", [128, 64, 32])
            WSH = mk.sb("WSH", [128, 64, 32]); SBF = mk.sb("SBF", [128, 64, 32], BF16)
            YGB = mk.sb("YGB", [128, 64, 128], BF16)
            O31 = mk.sb("O31t", [128, 64]); t64 = mk.sb("t64t", [128, 64])
            PJ = [mk.ps(f"PJ{j}", [128, 512], F32) for j in range(4)]
            PY = [mk.ps(f"PY5{j}", [128, 512], F32) for j in range(2)]
            PJ1 = mk.ps("PJ1t", [128, 512], F32)
            mk.dma("sp", KM[:].rearrange("p a b -> p (a b)"), io.S5M.ap()[0])
            mk.dma("sp", CL[:].rearrange("p a b -> p (a b)"), io.S5M.ap()[1])
            for sbi in range(NSB):
                uts, sla, slb = UTs[sbi % 2], SLAs[sbi % 2], SLBs[sbi % 2]
                mk.dma("sp", uts[:].rearrange("p g c -> p (g c)"), io.UT.ap()[sbi])
                mk.dma("sp", sla[:].rearrange("p g c -> p (g c)"), io.SL.ap()[sbi, 0])
                mk.dma("sp", slb[:].rearrange("p g c -> p (g c)"), io.SL.ap()[sbi, 1])
                scan_step(sla, slb, Z, TZ, OUT, True, WSH=WSH, SBF=SBF, PJ=PJ, O31=O31, PJ1=PJ1, t64=t64)
                sb = sbi % 4
                for gq in range(4):
                    py = PY[gq % 2]

                    def f2(e, gq=gq, py=py, uts=uts):
                        ins = None
                        for i in range(16):
                            g = gq * 16 + i
                            e.matmul(py[:, i * 32:(i + 1) * 32], KM[:, g, :], uts[:, g, :], start=True, stop=False)
                            ins = e.matmul(py[:, i * 32:(i + 1) * 32], CL[:, g, :], SBF[:, g, :], start=False, stop=True)
                        return ins
                    mk.op("pe", f2, reads=[KM, CL, uts, SBF], writes=[py])
                    mk.op("act", lambda e, gq=gq, py=py, sb=sb: e.activation(
                        out=YGB[:, gq * 16:(gq + 1) * 16, sb * 32:(sb + 1) * 32], in_=py[:].rearrange("p (g c) -> p g c", c=32), func=AF.Identity),
                        reads=[py], writes=[("YGB", gq)])
                if sb == 3:
                    b = sbi // 4
                    mk.dma("sp", io.YGB.ap()[b], YGB[:].rearrange("p a b -> p (a b)"), reads=[("YGB", q) for q in range(4)], writes=[io.YGB.ap()[b]])
                    mk.op("act", lambda e: e.activation(out=t64[:, 0:1], in_=t64[:, 0:1], func=AF.Identity),
                          reads=[t64, io.YGB.ap()[b]], writes=[t64] + [("YGB", q) for q in range(4)])
        with mk.scope():
            YGB = mk.sb("YGBl", [128, 64, 128], BF16)
            UCP = mk.sb("UCPl", [128, 64, 8, 16], BF16)
            DBC = mk.sb("DBC", [128, 64, 16], F32)
            Y32 = [mk.sb(f"Y32{j}", [128, 16, 8, 16]) for j in range(2)]
            TU = [mk.sb(f"TU{j}", [128, 16, 8, 16]) for j in range(2)]
            X2_ = [mk.sb(f"X2g{j}", [128, 16, 8, 16]) for j in range(2)]
            YRM = mk.sb("YRM", [128, 8, 1024], BF16)
            YT = mk.sb("YT5", [128, 8, 1024], BF16)
            PTb = [mk.ps(f"PTc{j}", [128, 512], F32) for j in range(4)]
            mk.dma("sp", DBC[:].rearrange("p g q -> p (g q)"), io.s5_d.ap())
            for b in range(NBLK):
                mk.dma("sp", YGB[:].rearrange("p a b -> p (a b)"), io.YGB.ap()[b])
                mk.dma("sp", UCP[:].rearrange("p g r q -> p (g r q)"), io.UCM.ap()[b])
                for gb in range(4):
                    y32, tu, x2 = Y32[gb % 2], TU[gb % 2], X2_[gb % 2]
                    for hh in range(2):
                        bank = PTb[(gb * 2 + hh) % 4]
                        pv = bank[:].bitcast(BF16).rearrange("p (a b) -> p a b", a=8)

                        def fbt(e, gb=gb, hh=hh, pv=pv):
                            ins = None
                            for i in range(8):
                                ins = e.transpose(pv[:, i, :], YGB[:, gb * 16 + hh * 8 + i, :], C.ident_b[:])
                            return ins
                        mk.op("pe", fbt, reads=[YGB, C.ident_b], writes=[bank])
                        mk.op("act", lambda e, hh=hh, pv=pv, y32=y32: e.activation(
                            out=y32[:, hh * 8:(hh + 1) * 8].rearrange("p g r q -> p g (r q)"), in_=pv, func=AF.Identity),
                            reads=[bank], writes=[(y32.name, hh)])
                    gs = slice(gb * 16, gb * 16 + 16)
                    mk.op("dve", lambda e, tu=tu, gs=gs: e.tensor_tensor(out=tu[:], in0=UCP[:, gs], in1=DBC[:, gs, :].unsqueeze(2).to_broadcast([128, 16, 8, 16]),
                                                                         op=ALU.mult), reads=[UCP, DBC], writes=[tu])
                    mk.op("dve", lambda e, tu=tu, y32=y32: e.tensor_tensor(out=y32[:], in0=y32[:], in1=tu[:], op=ALU.add),
                          reads=[(y32.name, 0), (y32.name, 1), tu], writes=[y32])
                    mk.op("dve", lambda e, x2=x2, y32=y32: e.tensor_tensor(out=x2[:], in0=y32[:], in1=y32[:], op=ALU.mult), reads=[y32], writes=[x2])
                    mk.op("dve", lambda e, x2=x2: e.tensor_scalar(out=x2[:], in0=x2[:], scalar1=0.044715, scalar2=1.0, op0=ALU.mult, op1=ALU.add),
                          reads=[x2], writes=[x2])
                    mk.op("dve", lambda e, x2=x2, y32=y32: e.tensor_tensor(out=x2[:], in0=x2[:], in1=y32[:], op=ALU.mult), reads=[x2, y32], writes=[x2])
                    mk.op("act", lambda e, x2=x2: e.activation(out=x2[:], in_=x2[:], func=AF.Sigmoid, scale=1.5957691216057308), reads=[x2], writes=[x2])
                    mk.op("dve", lambda e, x2=x2, y32=y32, gb=gb: e.tensor_tensor(
                        out=YRM[:, :, gb * 256:(gb + 1) * 256].rearrange("p r (g q) -> p g r q", q=16), in0=y32[:], in1=x2[:], op=ALU.mult),
                        reads=[y32, x2], writes=[("YRM", gb)])
                for kc in range(8):
                    bank = PTb[kc % 4]
                    pv = bank[:].bitcast(BF16).rearrange("p (a b) -> p a b", a=8)

                    def ffin(e, kc=kc, pv=pv):
                        ins = None
                        for r in range(8):
                            ins = e.transpose(pv[:, r, :], YRM[:, r, kc * 128:(kc + 1) * 128], C.ident_b[:])
                        return ins
                    mk.op("pe", ffin, reads=[("YRM", kc // 2), C.ident_b], writes=[bank])
                    mk.op("act" if kc % 2 == 0 else "dve",
                          (lambda e, kc=kc, pv=pv: e.activation(out=YT[:, kc, :].rearrange("p (c r) -> p r c", r=8), in_=pv, func=AF.Identity))
                          if kc % 2 == 0 else
                          (lambda e, kc=kc, pv=pv: e.tensor_copy(out=YT[:, kc, :].rearrange("p (c r) -> p r c", r=8), in_=pv)),
                          reads=[bank], writes=[("YT", kc)])
                mk.dma("sp", io.YACT.ap()[:, b * 1024:(b + 1) * 1024].rearrange("(kc p) t -> p kc t", p=128), YT[:],
                       reads=[("YT", kc) for kc in range(8)], writes=[io.YACT.ap()[:, b * 1024:(b + 1) * 1024]])
                mk.op("act", lambda e: e.activation(out=DBC[:, 0, 0:1], in_=DBC[:, 0, 0:1], func=AF.Identity),
                      reads=[DBC, io.YACT.ap()[:, b * 1024:(b + 1) * 1024]], writes=[DBC] + [("YT", kc) for kc in range(8)])


def phase_mix0_out(mk, io, C, src, dst):
    li = 0
    g1 = C.MOD[li][:, 32:48]
    with mk.scope():
        WGL = mk.sb("WGL", [128, 8, 2048], BF16)
        WO = mk.sb("WO0", [128, KC, D], BF16)
        YA = [mk.sb(f"YAo{j}", [128, 8, 512], BF16) for j in range(2)]
        YAT = [mk.sb(f"YATo{j}", [128, 8, 512], BF16) for j in range(2)]
        YS = mk.sb("YSo", [128, 8, 512], BF16)
        SGT = [mk.sb(f"SGTo{j}", [128, 512], F32) for j in range(2)]
        PG = [mk.ps(f"PGo{j}", [128, 512], F32) for j in range(2)]
        PV_ = [mk.ps(f"PVo{j}", [128, 512], F32) for j in range(2)]
        PO = [mk.ps(f"POo{j}", [128, 512], F32) for j in range(2)]
        mk.dma("pool", WGL[:], io.w_glu.ap().rearrange("(kc p) n -> p kc n", p=128))
        for h in range(2):
            mk.dma("pool", WO[:, :, h * 1024:(h + 1) * 1024], io.w_out0.ap()[:, h * 1024:(h + 1) * 1024].rearrange("(kc p) n -> p kc n", p=128),
                   writes=[("WO", h)])
        XR = [mk.sb(f"XRo4{j}", [128, 512], F32) for j in range(4)]
        its = [(t, dc) for t in range(NTOK // 512) for dc in range(KC)]

        def ld_y(t):
            mk.dma("sp", YA[t % 2][:], io.YACT.ap()[:, t * 512:(t + 1) * 512].rearrange("(kc p) t -> p kc t", p=128))
            mk.dma("sp", YAT[t % 2][:], io.YATT.ap()[:, t * 512:(t + 1) * 512].rearrange("(kc p) t -> p kc t", p=128))

        def ld_x(n):
            t, dc = its[n]
            mk.dma("sp", XR[n % 4][:], src.ap()[dc * 128:(dc + 1) * 128, t * 512:(t + 1) * 512])
        ld_y(0)
        ld_x(0)
        ld_x(1)
        n = 0
        for t in range(NTOK // 512):
            ya, yat = YA[t % 2], YAT[t % 2]
            if t + 1 < NTOK // 512:
                ld_y(t + 1)
            for j in range(8):
                pg, pv, sg = PG[j % 2], PV_[j % 2], SGT[j % 2]

                def fgl(e, pg=pg, pv=pv, j=j, ya=ya):
                    ins = None
                    for kc in range(8):
                        e.matmul(pg[:], WGL[:, kc, 1024 + j * 128:1024 + (j + 1) * 128], ya[:, kc, :], start=(kc == 0), stop=(kc == 7))
                    for kc in range(8):
                        ins = e.matmul(pv[:], WGL[:, kc, j * 128:(j + 1) * 128], ya[:, kc, :], start=(kc == 0), stop=(kc == 7))
                    return ins
                mk.op("pe", fgl, reads=[WGL, ya], writes=[pg, pv])
                mk.op("act", lambda e, sg=sg, pg=pg: e.activation(out=sg[:], in_=pg[:], func=AF.Sigmoid), reads=[pg], writes=[sg])
                mk.op("dve", lambda e, sg=sg, pv=pv, j=j: e.tensor_tensor(out=YS[:, j, :], in0=pv[:], in1=sg[:], op=ALU.mult),
                      reads=[pv, sg], writes=[("YS", j)])
            for dc in range(KC):
                po = PO[dc % 2]
                xr = XR[n % 4]
                if n + 2 < len(its):
                    ld_x(n + 2)
                n += 1

                def fo(e, po=po, dc=dc, yat=yat):
                    ins = None
                    for kc in range(KC):
                        rhs = YS[:, kc, :] if kc < 8 else yat[:, kc - 8, :]
                        ins = e.matmul(po[:], WO[:, kc, dc * 128:(dc + 1) * 128], rhs, start=(kc == 0), stop=(kc == KC - 1))
                    return ins
                mk.op("pe", fo, reads=[("WO", 0), ("WO", 1), yat] + [("YS", j) for j in range(8)], writes=[po])
                mk.op("dve", lambda e, xr=xr, po=po, dc=dc: e.scalar_tensor_tensor(
                    out=xr[:], in0=po[:], scalar=g1[:, dc:dc + 1], in1=xr[:], op0=ALU.mult, op1=ALU.add),
                    reads=[po, xr, C.MOD[li]], writes=[xr])
                mk.dma("sp", dst.ap()[dc * 128:(dc + 1) * 128, t * 512:(t + 1) * 512], xr[:])


def phase_mix0(mk, io, C, src, dst, parts=("s5", "attn", "out")):
    if "s5" in parts:
        phase_s5(mk, io, C, src)
    if "attn" in parts:
        phase_attn(mk, io, C, src)
    if "out" in parts:
        phase_mix0_out(mk, io, C, src, dst)
```

```python
from contextlib import ExitStack, contextmanager
import numpy as np
import concourse.bass as bass
import concourse.mybir as mybir
from concourse.bass_utils import run_bass_kernel_spmd

F32 = mybir.dt.float32
BF16 = mybir.dt.bfloat16
I32 = mybir.dt.int32
ALU = mybir.AluOpType
AF = mybir.ActivationFunctionType
AX = mybir.AxisListType

SEM_CHUNK = 30000
DMA_SLOTS = 8
DMA_MAXJ = 1800
ENGS = ("pe", "act", "dve", "pool", "sp")


def _key(x):
    if isinstance(x, (str, tuple)):
        return x
    t = getattr(x, "tensor", None)
    if t is not None:
        if "DRam" in type(t).__name__:
            return (t.name, int(x.offset))
        return t.name
    if "DRam" in type(x).__name__:
        return (x.name, 0)
    return x.name


class MK:
    def __init__(self, nc):
        self.nc = nc
        self.root = ExitStack()
        self.cur = self.root
        self.ops = []
        self.cnt = {e: 0 for e in ENGS}
        self.sems = {e: [] for e in ENGS}
        self.dslots = {}
        self.drr = {}
        self.lastw = {}
        self.readers = {}
        self.seen = {e: {} for e in ENGS}
        self.ncc = 0
        self.uid = 0

    def sb(self, name, shape, dtype=F32):
        self.uid += 1
        return self.cur.enter_context(self.nc.sbuf_tensor(f"{name}_{self.uid}", list(shape), dtype))

    def ps(self, name, shape, dtype=F32):
        self.uid += 1
        nm = f"{name}_{self.uid}"
        if not hasattr(self, "psum_keys"):
            self.psum_keys = set()
        self.psum_keys.add(nm)
        return self.cur.enter_context(self.nc.psum_tensor(nm, list(shape), dtype))

    def _sem(self, name):
        return self.root.enter_context(self.nc.semaphore(name))

    @contextmanager
    def scope(self):
        prev = self.cur
        st = ExitStack()
        self.cur = st
        yield
        self.barrier()
        self.flush()
        st.close()
        self.cur = prev

    def _eng_tok(self, eng, seq):
        i = (seq - 1) // SEM_CHUNK
        while len(self.sems[eng]) <= i:
            self.sems[eng].append(self._sem(f"s_{eng}_{len(self.sems[eng])}"))
        return (("c", eng, i), self.sems[eng][i], (seq - 1) % SEM_CHUNK + 1)

    def _need(self, eng, tok, waits):
        k, h, v = tok
        if eng == "pe" and k[0] == "c" and k[1] == "pe":
            return
        if self.seen[eng].get(k, 0) >= v:
            return
        self.seen[eng][k] = v
        for i, (kk, hh, vv) in enumerate(waits):
            if kk == k:
                if vv < v:
                    waits[i] = (k, h, v)
                return
        waits.append((k, h, v))

    def _deps(self, eng, reads, writes):
        waits = []
        for r in reads:
            k = _key(r)
            t = self.lastw.get(k)
            if t is not None:
                self._need(eng, t, waits)
            if k in getattr(self, "psum_keys", ()):
                for t in self.readers.get(k, ()):
                    if not (t[0][0] == "c" and t[0][1] == eng):
                        self._need(eng, t, waits)
        for w in writes:
            k = _key(w)
            t = self.lastw.get(k)
            if t is not None:
                self._need(eng, t, waits)
            for t in self.readers.get(k, ()):
                self._need(eng, t, waits)
        return waits

    def _commit(self, tok, reads, writes):
        for r in reads:
            self.readers.setdefault(_key(r), []).append(tok)
        for w in writes:
            k = _key(w)
            self.lastw[k] = tok
            self.readers[k] = []

    def op(self, eng, fn, reads=(), writes=()):
        waits = self._deps(eng, reads, writes)
        self.cnt[eng] += 1
        tok = self._eng_tok(eng, self.cnt[eng])
        self.ops.append((eng, fn, waits, (tok[1], 1)))
        self._commit(tok, reads, writes)

    def dma(self, q, out, in_, reads=None, writes=None, **kw):
        reads = [in_] if reads is None else reads
        writes = [out] if writes is None else writes
        waits = self._deps(q, reads, writes)
        slots = self.dslots.setdefault(q, [])
        rr = self.drr.get(q, 0)
        if len(slots) < DMA_SLOTS:
            slots.append([self._sem(f"d_{q}_{len(slots)}"), 0, len(slots)])
            slot = slots[-1]
        else:
            slot = slots[rr % len(slots)]
            if slot[1] >= DMA_MAXJ:
                slot[0] = self._sem(f"d_{q}_r{rr}")
                slot[1] = 0
                slot[2] = 1000 + rr
        self.drr[q] = rr + 1
        key = ("d", q, slot[2])
        if slot[1] > 0:
            self._need(q, (key, slot[0], 16 * slot[1]), waits)
        slot[1] += 1
        tok = (key, slot[0], 16 * slot[1])

        def fn(e, out=out, in_=in_, kw=kw):
            return e.dma_start(out=out, in_=in_, **kw)
        self.ops.append((q, fn, waits, (slot[0], 16)))
        self._commit(tok, reads, writes)
        return tok

    def cc_allgather(self, out_t, in_t, groups):
        waits = self._deps("pool", [in_t], [out_t])
        self.ncc += 1
        sem = self._sem(f"cc_{self.ncc}")
        tok = (("cc", self.ncc), sem, 1)

        def fn(e):
            return e.collective_compute("AllGather", ALU.bypass, replica_groups=groups,
                                        ins=[in_t.ap().opt()], outs=[out_t.ap().opt()])
        self.ops.append(("pool", fn, waits, (sem, 1)))
        self._commit(tok, [in_t], [out_t])

    def barrier(self):
        toks = []
        for e in ENGS:
            if self.cnt[e] > 0:
                toks.append(self._eng_tok(e, self.cnt[e]))
        for q, slots in self.dslots.items():
            for s in slots:
                if s[1] > 0:
                    toks.append((("d", q, s[2]), s[0], 16 * s[1]))
        for k, t in self.lastw.items():
            if t[0][0] == "cc":
                toks.append(t)
        for e in ENGS:
            waits = []
            for t in toks:
                self._need(e, t, waits)
            if waits:
                self.ops.append((e, None, waits, None))

    def wait_all(self, eng, keys):
        waits = []
        for k in keys:
            t = self.lastw.get(_key(k))
            if t is not None:
                self._need(eng, t, waits)
        self.ops.append((eng, None, waits, None))

    def flush(self):
        if not self.ops:
            return
        if not hasattr(self, "_semval"):
            self._semval = {}
        for (eng, fn, waits, inc) in self.ops:
            for (k, h, v) in waits:
                cur = self._semval.get(id(h), 0)
                assert cur >= v, f"wait on future/unreachable value: eng={eng} key={k} need={v} have={cur}"
            if inc is not None:
                self._semval[id(inc[0])] = self._semval.get(id(inc[0]), 0) + inc[1]
        import os as _os2
        if _os2.environ.get("DUMPOPS"):
            for (eng, fn, waits, inc) in self.ops:
                nm = getattr(fn, "__qualname__", str(fn)) if fn is not None else "-"
                print("OP", eng, nm.split(".")[-3:] if fn else "-", [(k, v) for (k, h, v) in waits], "inc", (inc[1] if inc else None))
        per = {e: [] for e in ENGS}
        for o in self.ops:
            per[o[0]].append(o)
        self.ops = []

        def run(engine, lst):
            for (_, fn, waits, inc) in lst:
                for (k, h, v) in waits:
                    engine.wait_ge(h, v)
                if fn is not None:
                    ins = fn(engine)
                    ins.then_inc(inc[0], inc[1])

        with self.nc.Block() as block:
            @block.tensor
            def _(e):
                run(e, per["pe"])

            @block.scalar
            def _(e):
                run(e, per["act"])

            @block.vector
            def _(e):
                run(e, per["dve"])

            @block.gpsimd
            def _(e):
                run(e, per["pool"])

            @block.sync
            def _(e):
                run(e, per["sp"])

    def finish(self):
        self.flush()
        self.root.close()


D = 2048
KC = 16
NTOK = 2048
NCORES = 8
PAIRS = [[0, 1], [2, 3], [4, 5], [6, 7]]
EPS = 1e-6
NE = 32
FF = 256


class Ctx:
    pass


def declare_io(nc, cfg):
    io = Ctx()

    def inp(name, shape, dt=F32):
        t = nc.dram_tensor(name, list(shape), dt, kind="ExternalInput")
        setattr(io, name, t)
        return t
    inp("xT", [D, NTOK])
    inp("cT", [128, KC])
    inp("ada_w", [2, D, 6 * D])
    inp("ada_bT", [2, 128, 96])
    inp("nmix", [2, 128, KC])
    inp("nffn", [2, 128, KC])
    inp("fnorm", [128, KC])
    inp("router_w", [2, D, 36])
    inp("router_b", [2, 128, 36])
    inp("moe_w_gate", [2, NE, D, FF])
    inp("moe_w_up", [2, NE, D, FF])
    inp("moe_w_down", [2, NE, FF, D])
    inp("ident_f", [128, 128])
    io.outT = nc.dram_tensor("outT", [D, NTOK], F32, kind="ExternalOutput")
    io.X = [nc.dram_tensor(f"xs{i}", [D, NTOK], F32) for i in range(4)]
    io.cb = nc.dram_tensor("cb_scr", [2, 2, NE, 1024], F32, kind=("ExternalOutput" if cfg.get("dump") else "Internal"))
    return io


def setup_consts(mk, io, C):
    C.ones_b = mk.sb("ones_b", [128, 128], BF16)
    C.eps = mk.sb("eps", [128, 1], F32)
    C.ident_f = mk.sb("ident_f", [128, 128], F32)
    C.ident_b = mk.sb("ident_b", [128, 128], BF16)
    mk.op("pool", lambda e: e.memset(C.ones_b[:], 1.0), writes=[C.ones_b])
    mk.op("pool", lambda e: e.memset(C.eps[:], EPS), writes=[C.eps])
    mk.dma("sp", C.ident_f[:], io.ident_f.ap())
    mk.op("dve", lambda e: e.tensor_copy(out=C.ident_b[:], in_=C.ident_f[:]), reads=[C.ident_f], writes=[C.ident_b])
    C.MOD = [mk.sb(f"MOD{i}", [128, 96], F32) for i in range(2)]
    C.G1 = [mk.sb(f"G1{i}", [128, KC], F32) for i in range(2)]
    C.G2 = [mk.sb(f"G2{i}", [128, KC], F32) for i in range(2)]
    C.FN = mk.sb("FN", [128, KC], F32)
    C.ZERO = mk.sb("ZERO", [128, KC], F32)
    C.one = mk.sb("one", [128, 1], F32)
    mk.op("pool", lambda e: e.memset(C.one[:], 1.0), writes=[C.one])
    mk.dma("sp", C.FN[:], io.fnorm.ap())
    mk.op("pool", lambda e: e.memset(C.ZERO[:], 0.0), writes=[C.ZERO])


def phase_adaln(mk, io, C):
    with mk.scope():
        CT = mk.sb("CT", [128, KC], F32)
        CA = mk.sb("CA", [128, KC], BF16)
        WA = [mk.sb(f"WA{j}", [128, 6 * D], BF16) for j in range(2)]
        AB = mk.sb("AB", [128, 96], F32)
        NM = mk.sb("NM", [128, KC], F32)
        NF = mk.sb("NF", [128, KC], F32)
        PM = mk.ps("PM", [128, 96], F32)
        mk.dma("sp", CT[:], io.cT.ap())
        mk.op("act", lambda e: e.activation(out=CA[:], in_=CT[:], func=AF.Silu), reads=[CT], writes=[CA])
        for i in range(2):
            mk.dma("sp", AB[:], io.ada_bT.ap()[i])
            mk.dma("sp", NM[:], io.nmix.ap()[i])
            mk.dma("sp", NF[:], io.nffn.ap()[i])
            for kc in range(KC):
                wa = WA[kc % 2]
                mk.dma("pool", wa[:], io.ada_w.ap()[i, kc * 128:(kc + 1) * 128, :])

                def f(e, wa=wa, kc=kc):
                    ins = None
                    for j in range(96):
                        ins = e.matmul(PM[:, j:j + 1], wa[:, j * 128:(j + 1) * 128], CA[:, kc:kc + 1],
                                       start=(kc == 0 and j == 0), stop=(kc == KC - 1 and j == 95),
                                       skip_group_check=True)
                    return ins
                mk.op("pe", f, reads=[wa, CA], writes=[PM])
            MOD = C.MOD[i]
            mk.op("dve", lambda e, MOD=MOD: e.tensor_tensor(out=MOD[:], in0=PM[:], in1=AB[:], op=ALU.add),
                  reads=[PM, AB], writes=[MOD])
            mk.op("dve", lambda e, MOD=MOD, i=i: e.scalar_tensor_tensor(
                out=C.G1[i][:], in0=MOD[:, 16:32], scalar=1.0, in1=NM[:], op0=ALU.add, op1=ALU.mult),
                reads=[MOD, NM], writes=[C.G1[i]])
            mk.op("dve", lambda e, MOD=MOD, i=i: e.scalar_tensor_tensor(
                out=C.G2[i][:], in0=MOD[:, 64:80], scalar=1.0, in1=NF[:], op0=ALU.add, op1=ALU.mult),
                reads=[MOD, NF], writes=[C.G2[i]])


def norm_piece(mk, C, src, col0, ncols, G, SH, XT, SQ, PS, RS, HT_dst, HLO_dst=None, key=None, hlo_key=None):
    hk = key if key is not None else HT_dst
    mk.dma("sp", XT[:], src.ap()[:, col0:col0 + ncols].rearrange("(kc p) t -> p kc t", p=128))
    mk.op("act", lambda e: e.activation(out=SQ[:], in_=XT[:], func=AF.Square), reads=[XT], writes=[SQ])

    def f(e):
        ins = None
        for kc in range(KC):
            ins = e.matmul(PS[:], C.ones_b[:], SQ[:, kc, :], start=(kc == 0), stop=(kc == KC - 1))
        return ins
    mk.op("pe", f, reads=[SQ, C.ones_b], writes=[PS])
    mk.op("act", lambda e: e.activation(out=RS[:], in_=PS[:], func=AF.Sqrt, bias=C.eps[:, 0:1], scale=1.0 / D),
          reads=[PS, C.eps], writes=[RS])
    mk.op("dve", lambda e: e.reciprocal(out=RS[:], in_=RS[:]), reads=[RS], writes=[RS])

    def g(e):
        ins = None
        for kc in range(KC):
            ins = e.scalar_tensor_tensor(out=XT[:, kc, :], in0=XT[:, kc, :], scalar=G[:, kc:kc + 1], in1=RS[:],
                                         op0=ALU.mult, op1=ALU.mult)
        return ins
    mk.op("dve", g, reads=[XT, RS, G], writes=[XT])
    if HLO_dst is None:
        def h(e):
            ins = None
            for kc in range(KC):
                ins = e.activation(out=HT_dst[:, kc, :], in_=XT[:, kc, :], func=AF.Identity,
                                   bias=SH[:, kc:kc + 1], scale=1.0)
            return ins
        mk.op("act", h, reads=[XT, SH], writes=[hk])
    else:
        def h(e):
            ins = None
            for kc in range(KC):
                ins = e.activation(out=HT_dst[:, kc, :], in_=XT[:, kc, :], func=AF.Identity,
                                   bias=SH[:, kc:kc + 1], scale=1.0)
            return ins
        mk.op("act", h, reads=[XT, SH], writes=[hk])

        def hl(e):
            ins = None
            for kc in range(KC):
                ins = e.scalar_tensor_tensor(out=HLO_dst[:, kc, :], in0=XT[:, kc, :], scalar=SH[:, kc:kc + 1],
                                             in1=HT_dst[:, kc, :], op0=ALU.add, op1=ALU.subtract)
            return ins
        mk.op("dve", hl, reads=[XT, SH, hk], writes=[hlo_key if hlo_key is not None else HLO_dst])


def phase_moe(mk, io, C, li, src, dst):
    BLK = 1024
    NP = 256
    G2, SH2, g2 = C.G2[li], C.MOD[li][:, 48:64], C.MOD[li][:, 80:96]
    BIG = 30000.0
    for b in range(NTOK // BLK):
        with mk.scope():
            HT = mk.sb("HT", [128, KC, BLK], BF16)
            WG = [mk.sb(f"WG{j}", [128, KC, FF], BF16) for j in range(2)]
            WU = [mk.sb(f"WU{j}", [128, KC, FF], BF16) for j in range(2)]
            WD = [mk.sb(f"WD{j}", [128, 2, D], BF16) for j in range(4)]

            def load_wts(ex):
                mk.dma("pool", WG[ex % 2][:], io.moe_w_gate.ap()[li, ex].rearrange("(kc p) f -> p kc f", p=128))
                mk.dma("pool", WU[ex % 2][:], io.moe_w_up.ap()[li, ex].rearrange("(kc p) f -> p kc f", p=128))
                mk.dma("pool", WD[ex % 4][:], io.moe_w_down.ap()[li, ex].rearrange("(fc p) d -> p fc d", p=128))
            load_wts(0)
            with mk.scope():
                XTs = [mk.sb(f"XT{j}", [128, KC, NP], F32) for j in range(2)]
                SQs = [mk.sb(f"SQ{j}", [128, KC, NP], BF16) for j in range(2)]
                HLO = mk.sb("HLO", [128, KC, BLK], BF16)
                RSs = [mk.sb(f"RS{j}", [128, NP], F32) for j in range(2)]
                WR = mk.sb("WR", [128, KC, 36], F32)
                WRH = mk.sb("WRH", [128, KC, 36], BF16)
                WRL = mk.sb("WRL", [128, KC, 36], BF16)
                RB = mk.sb("RB", [128, 36], F32)
                COMB = mk.sb("COMB", [128, 8, NE], F32)
                CBT = mk.sb("CBT", [NE, BLK], F32)
                PSs = [mk.ps(f"PSn{j}", [128, NP], F32) for j in range(2)]
                LG = mk.ps("LG", [128, 8, 36], F32)
                PT = mk.ps("PTc", [NE, 512], F32)
                L = mk.sb("L", [128, 8, 36], F32)
                sm = {n: mk.sb(n, [128, 8] + w, F32) for n, w in
                      [("gmax", []), ("gd", [4]), ("gsum", []), ("gval", []), ("ohg", [4]), ("pen", [4]),
                       ("elm", [32]), ("m1", []), ("oh1", [32]), ("elm2", [32]), ("m2", []), ("oh2", [32]), ("dd", []),
                       ("w1", []), ("w2", []), ("t32", [32])]}
                mk.dma("sp", WR[:], io.router_w.ap()[li].rearrange("(kc p) n -> p kc n", p=128))
                mk.dma("sp", RB[:], io.router_b.ap()[li])
                mk.op("dve", lambda e: e.tensor_copy(out=WRH[:], in_=WR[:]), reads=[WR], writes=[WRH])
                mk.op("dve", lambda e: e.tensor_tensor(out=WRL[:], in0=WR[:], in1=WRH[:], op=ALU.subtract),
                      reads=[WR, WRH], writes=[WRL])
                for pc in range(BLK // NP):
                    c0 = b * BLK + pc * NP
                    hts = HT[:, :, pc * NP:(pc + 1) * NP]
                    norm_piece(mk, C, src, c0, NP, G2, SH2, XTs[pc % 2], SQs[pc % 2], PSs[pc % 2], RSs[pc % 2], hts,
                               HLO[:, :, pc * NP:(pc + 1) * NP], key=("HT", pc), hlo_key=("HLO", pc))
                    for sub in range(NP // 128):
                        st = pc * (NP // 128) + sub
                        t0 = pc * NP + sub * 128

                        def f(e, t0=t0, st=st):
                            ins = None
                            n = 0
                            for (a, w) in ((0, WRH), (1, WRH), (0, WRL)):
                                for kc in range(KC):
                                    lhs = HT[:, kc, t0:t0 + 128] if a == 0 else HLO[:, kc, t0:t0 + 128]
                                    ins = e.matmul(LG[:, st, :], lhs, w[:, kc, :], start=(n == 0), stop=(n == 3 * KC - 1))
                                    n += 1
                            return ins
                        mk.op("pe", f, reads=[("HT", pc), ("HLO", pc), WRH, WRL], writes=[LG])
                s = sm
                b8 = lambda ap, w: ap.unsqueeze(2).to_broadcast([128, 8, w])
                mk.op("dve", lambda e: e.tensor_tensor(out=L[:], in0=LG[:], in1=RB[:].unsqueeze(1).to_broadcast([128, 8, 36]), op=ALU.add),
                      reads=[LG, RB], writes=[L])
                mk.op("dve", lambda e: e.tensor_reduce(out=s["gmax"][:], in_=L[:, :, 0:4], axis=AX.X, op=ALU.max), reads=[L], writes=[s["gmax"]])
                mk.op("dve", lambda e: e.tensor_tensor(out=s["gd"][:], in0=L[:, :, 0:4], in1=b8(s["gmax"][:], 4), op=ALU.subtract),
                      reads=[L, s["gmax"]], writes=[s["gd"]])
                mk.op("dve", lambda e: e.tensor_scalar(out=s["ohg"][:], in0=s["gd"][:], scalar1=0.0, scalar2=None, op0=ALU.is_ge),
                      reads=[s["gd"]], writes=[s["ohg"]])
                mk.op("act", lambda e: e.activation(out=s["gd"][:], in_=s["gd"][:], func=AF.Exp), reads=[s["gd"], s["ohg"]], writes=[s["gd"]])
                mk.op("dve", lambda e: e.tensor_reduce(out=s["gsum"][:], in_=s["gd"][:], axis=AX.X, op=ALU.add), reads=[s["gd"]], writes=[s["gsum"]])
                mk.op("dve", lambda e: e.reciprocal(out=s["gval"][:], in_=s["gsum"][:]), reads=[s["gsum"]], writes=[s["gval"]])
                mk.op("dve", lambda e: e.tensor_scalar(out=s["pen"][:], in0=s["ohg"][:], scalar1=-1.0, scalar2=BIG, op0=ALU.add, op1=ALU.mult),
                      reads=[s["ohg"]], writes=[s["pen"]])
                mk.op("dve", lambda e: e.tensor_tensor(
                    out=s["elm"][:].rearrange("p s (g e) -> p s g e", g=4), in0=L[:, :, 4:36].rearrange("p s (g e) -> p s g e", g=4),
                    in1=s["pen"][:].unsqueeze(3).to_broadcast([128, 8, 4, 8]), op=ALU.add), reads=[L, s["pen"]], writes=[s["elm"]])
                mk.op("dve", lambda e: e.tensor_reduce(out=s["m1"][:], in_=s["elm"][:], axis=AX.X, op=ALU.max), reads=[s["elm"]], writes=[s["m1"]])
                mk.op("dve", lambda e: e.tensor_tensor(out=s["oh1"][:], in0=s["elm"][:], in1=b8(s["m1"][:], 32), op=ALU.is_ge),
                      reads=[s["elm"], s["m1"]], writes=[s["oh1"]])
                mk.op("dve", lambda e: e.tensor_scalar(out=s["t32"][:], in0=s["oh1"][:], scalar1=-BIG, scalar2=None, op0=ALU.mult),
                      reads=[s["oh1"]], writes=[s["t32"]])
                mk.op("dve", lambda e: e.tensor_tensor(out=s["elm2"][:], in0=s["elm"][:], in1=s["t32"][:], op=ALU.add),
                      reads=[s["elm"], s["t32"]], writes=[s["elm2"]])
                mk.op("dve", lambda e: e.tensor_reduce(out=s["m2"][:], in_=s["elm2"][:], axis=AX.X, op=ALU.max), reads=[s["elm2"]], writes=[s["m2"]])
                mk.op("dve", lambda e: e.tensor_tensor(out=s["oh2"][:], in0=s["elm2"][:], in1=b8(s["m2"][:], 32), op=ALU.is_ge),
                      reads=[s["elm2"], s["m2"]], writes=[s["oh2"]])
                mk.op("dve", lambda e: e.tensor_tensor(out=s["dd"][:], in0=s["m1"][:], in1=s["m2"][:], op=ALU.subtract),
                      reads=[s["m1"], s["m2"]], writes=[s["dd"]])
                mk.op("act", lambda e: e.activation(out=s["w1"][:], in_=s["dd"][:], func=AF.Sigmoid), reads=[s["dd"]], writes=[s["w1"]])
                mk.op("dve", lambda e: e.tensor_tensor(out=s["w1"][:], in0=s["w1"][:], in1=s["gval"][:], op=ALU.mult),
                      reads=[s["w1"], s["gval"]], writes=[s["w1"]])
                mk.op("dve", lambda e: e.tensor_tensor(out=s["w2"][:], in0=s["gval"][:], in1=s["w1"][:], op=ALU.subtract),
                      reads=[s["w1"], s["gval"]], writes=[s["w2"]])
                mk.op("dve", lambda e: e.tensor_tensor(out=s["t32"][:], in0=s["oh1"][:], in1=b8(s["w1"][:], 32), op=ALU.mult),
                      reads=[s["oh1"], s["w1"], s["elm2"]], writes=[s["t32"]])
                mk.op("dve", lambda e: e.tensor_tensor(out=s["elm2"][:], in0=s["oh2"][:], in1=b8(s["w2"][:], 32), op=ALU.mult),
                      reads=[s["oh2"], s["w2"]], writes=[s["elm2"]])
                mk.op("dve", lambda e: e.tensor_tensor(out=COMB[:], in0=s["t32"][:], in1=s["elm2"][:], op=ALU.add),
                      reads=[s["t32"], s["elm2"]], writes=[COMB])
                for hf in range(2):
                    def f(e, hf=hf):
                        ins = None
                        for j in range(4):
                            ins = e.matmul(PT[:, j * 128:(j + 1) * 128], COMB[:, hf * 4 + j, :], C.ident_f[:],
                                           start=True, stop=True)
                        return ins
                    mk.op("pe", f, reads=[COMB, C.ident_f], writes=[PT])
                    mk.op("dve", lambda e, hf=hf: e.tensor_copy(out=CBT[:, hf * 512:(hf + 1) * 512], in_=PT[:]),
                          reads=[PT], writes=[CBT])
                mk.dma("sp", io.cb.ap()[li, b], CBT[:], writes=[("cb", li, b)])
            accscope = mk.scope()
            accscope.__enter__()
            ACC = mk.sb("ACC", [128, KC, BLK], F32)
            with mk.scope():
                BC = [mk.sb(f"BC{j}", [128, BLK], F32) for j in range(2)]
                HID = [mk.sb(f"HID{j}", [128, 2, 2, 2, 512], BF16) for j in range(2)]
                SG = [mk.sb(f"SG{j}", [128, 512], F32) for j in range(2)]
                T1 = [mk.sb(f"T1{j}", [128, 512], F32) for j in range(2)]
                PA = [mk.ps(f"PA{j}", [128, 512], F32) for j in range(2)]
                PB = [mk.ps(f"PB{j}", [128, 512], F32) for j in range(2)]
                PO = [mk.ps(f"PO{j}", [128, 512], F32) for j in range(2)]
                cnt = 0

                def load_w(ex, wts=True):
                    if wts:
                        load_wts(ex)
                    mk.dma("sp", BC[ex % 2][:], io.cb.ap()[li, b, ex].partition_broadcast(128),
                           reads=[("cb", li, b)])
                load_w(0, wts=False)
                for eg in range(NE // 2):
                    hid = HID[eg % 2]
                    for e2 in range(2):
                        ex = eg * 2 + e2
                        wg, wu, wd, bc = WG[ex % 2], WU[ex % 2], WD[ex % 4], BC[ex % 2]
                        if ex + 1 < NE:
                            load_w(ex + 1)
                        for t in range(2):
                            for fc in range(2):
                                pa, pb, sg, t1 = PA[cnt % 2], PB[cnt % 2], SG[cnt % 2], T1[cnt % 2]
                                cnt += 1

                                def fg(e, w=wg, p=pa, t=t, fc=fc):
                                    ins = None
                                    for kc in range(KC):
                                        ins = e.matmul(p[:], w[:, kc, fc * 128:(fc + 1) * 128],
                                                       HT[:, kc, t * 512:(t + 1) * 512],
                                                       start=(kc == 0), stop=(kc == KC - 1))
                                    return ins
                                mk.op("pe", lambda e, fg=fg: fg(e), reads=[wg, HT], writes=[pa])
                                mk.op("pe", lambda e, fg=fg, wu=wu, pb=pb: fg(e, w=wu, p=pb), reads=[wu, HT], writes=[pb])
                                mk.op("act", lambda e, sg=sg, pa=pa: e.activation(out=sg[:], in_=pa[:], func=AF.Silu),
                                      reads=[pa], writes=[sg])
                                mk.op("dve", lambda e, t1=t1, pb=pb, sg=sg: e.tensor_tensor(
                                    out=t1[:], in0=pb[:], in1=sg[:], op=ALU.mult), reads=[pb, sg], writes=[t1])
                                mk.op("dve", lambda e, t1=t1, bc=bc, hid=hid, t=t, e2=e2, fc=fc: e.tensor_tensor(
                                    out=hid[:, t, e2, fc, :], in0=t1[:], in1=bc[:, t * 512:(t + 1) * 512], op=ALU.mult),
                                    reads=[t1, bc], writes=[(hid.name, t)])
                    for t in range(2):
                        for dc in range(KC):
                            po = PO[dc % 2]

                            def fd(e, po=po, t=t, dc=dc, eg=eg, hid=hid):
                                ins = None
                                n = 0
                                for e2 in range(2):
                                    wd = WD[(eg * 2 + e2) % 4]
                                    for fc in range(2):
                                        ins = e.matmul(po[:], wd[:, fc, dc * 128:(dc + 1) * 128], hid[:, t, e2, fc, :],
                                                       start=(n == 0), stop=(n == 3))
                                        n += 1
                                return ins
                            mk.op("pe", fd, reads=[WD[(eg * 2) % 4], WD[(eg * 2 + 1) % 4], (hid.name, t)], writes=[po])
                            acc = ACC[:, dc, t * 512:(t + 1) * 512]
                            if eg == 0:
                                mk.op("dve", lambda e, acc=acc, po=po: e.tensor_copy(out=acc, in_=po[:]),
                                      reads=[po], writes=[("ACC", t, dc)])
                            else:
                                mk.op("dve", lambda e, acc=acc, po=po: e.tensor_tensor(out=acc, in0=acc, in1=po[:], op=ALU.add),
                                      reads=[po, ("ACC", t, dc)], writes=[("ACC", t, dc)])
            with mk.scope():
                XR = [mk.sb(f"XR{j}", [128, KC, 256], F32) for j in range(2)]

                def r_ld(pc):
                    c0 = b * BLK + pc * 256
                    mk.dma("sp", XR[pc % 2][:], src.ap()[:, c0:c0 + 256].rearrange("(kc p) t -> p kc t", p=128))

                def r_st(pc):
                    xr = XR[pc % 2]
                    c0 = b * BLK + pc * 256

                    def fr(e, xr=xr, pc=pc):
                        ins = None
                        for kc in range(KC):
                            ins = e.scalar_tensor_tensor(out=xr[:, kc, :], in0=ACC[:, kc, pc * 256:(pc + 1) * 256],
                                                         scalar=g2[:, kc:kc + 1], in1=xr[:, kc, :],
                                                         op0=ALU.mult, op1=ALU.add)
                        return ins
                    mk.op("dve", fr, reads=[xr, C.MOD[li]] + [("ACC", pc // 2, dc) for dc in range(KC)], writes=[xr])
                    mk.dma("sp", dst.ap()[:, c0:c0 + 256].rearrange("(kc p) t -> p kc t", p=128), xr[:])
                r_ld(0); r_ld(1); r_st(0); r_ld(2); r_st(1); r_ld(3); r_st(2); r_st(3)
            accscope.__exit__(None, None, None)


def phase_final(mk, io, C, src):
    NP = 256
    with mk.scope():
        XTs = [mk.sb(f"XTf{j}", [128, KC, NP], F32) for j in range(4)]

        def ld_f(pc):
            mk.dma("sp", XTs[pc % 4][:], src.ap()[:, pc * NP:(pc + 1) * NP].rearrange("(kc p) t -> p kc t", p=128))
        ld_f(0)
        ld_f(1)
        SQs = [mk.sb(f"SQf{j}", [128, KC, NP], BF16) for j in range(2)]
        RSs = [mk.sb(f"RSf{j}", [128, NP], F32) for j in range(2)]
        PSs = [mk.ps(f"PSf{j}", [128, NP], F32) for j in range(2)]
        for pc in range(NTOK // NP):
            XT = XTs[pc % 4]
            SQ, RS, PS = SQs[pc % 2], RSs[pc % 2], PSs[pc % 2]
            c0 = pc * NP
            if pc + 2 < NTOK // NP:
                ld_f(pc + 2)
            mk.op("act", lambda e, XT=XT, SQ=SQ: e.activation(out=SQ[:], in_=XT[:], func=AF.Square), reads=[XT], writes=[SQ])

            def f(e, SQ=SQ, PS=PS):
                ins = None
                for kc in range(KC):
                    ins = e.matmul(PS[:], C.ones_b[:], SQ[:, kc, :], start=(kc == 0), stop=(kc == KC - 1))
                return ins
            mk.op("pe", f, reads=[SQ, C.ones_b], writes=[PS])
            mk.op("act", lambda e, RS=RS, PS=PS: e.activation(out=RS[:], in_=PS[:], func=AF.Sqrt, bias=C.eps[:, 0:1], scale=1.0 / D),
                  reads=[PS, C.eps], writes=[RS])
            mk.op("dve", lambda e, RS=RS: e.reciprocal(out=RS[:], in_=RS[:]), reads=[RS], writes=[RS])

            def g(e, XT=XT, RS=RS):
                ins = None
                for kc in range(KC):
                    ins = e.scalar_tensor_tensor(out=XT[:, kc, :], in0=XT[:, kc, :], scalar=C.FN[:, kc:kc + 1],
                                                 in1=RS[:], op0=ALU.mult, op1=ALU.mult)
                return ins
            mk.op("dve", g, reads=[XT, RS, C.FN], writes=[XT])
            mk.dma("sp", io.outT.ap()[:, c0:c0 + NP].rearrange("(kc p) t -> p kc t", p=128), XT[:])
    mk.wait_all("sp", [io.outT])


def copy_x(mk, src, dst):
    with mk.scope():
        T = [mk.sb(f"cp{j}", [128, KC, 256], F32) for j in range(2)]
        for pc in range(NTOK // 256):
            t = T[pc % 2]
            mk.dma("sp", t[:], src.ap()[:, pc * 256:(pc + 1) * 256].rearrange("(kc p) t -> p kc t", p=128))
            mk.dma("sp", dst.ap()[:, pc * 256:(pc + 1) * 256].rearrange("(kc p) t -> p kc t", p=128), t[:])


def build_program(cfg=None):
    cfg = cfg or {}
    nc = bass.Bass("TRN2", target_bir_lowering=False)
    io = declare_io(nc, cfg)
    declare_ssd_io(nc, io, bool(cfg.get("dump")))
    declare_mix0_io(nc, io, bool(cfg.get("dump")))
    mk = MK(nc)
    C = Ctx()
    setup_consts(mk, io, C)
    phase_adaln(mk, io, C)
    phases = cfg.get("phases", ("mix0", "moe0", "mix1", "moe1"))
    cur = io.xT
    k = 0
    for li in range(2):
        if f"mix{li}" in phases:
            if li == 0:
                phase_mix0(mk, io, C, cur, io.X[k], cfg.get("mix0_parts", ("s5", "attn", "out")))
                cur = io.X[k]
                k += 1
            if li == 1:
                phase_ssd(mk, io, C, cur, io.X[k], cfg.get("ssd_stages", 3), cfg.get("s2mode", "full"))
                cur = io.X[k]
                k += 1
        if f"moe{li}" in phases:
            phase_moe(mk, io, C, li, cur, io.X[k])
            cur = io.X[k]
            k += 1
    if cfg.get("dump"):
        io.dbgm = nc.dram_tensor("dbgM", [2, 128, 96], F32, kind="ExternalOutput")
        for i in range(2):
            mk.dma("sp", io.dbgm.ap()[i], C.MOD[i][:])
        io.dbg = nc.dram_tensor("dbgX", [D, NTOK], F32, kind="ExternalOutput")
        copy_x(mk, cur, io.dbg)
    phase_final(mk, io, C, cur)
    mk.finish()
    return nc


def _consts():
    i = np.arange(128)[:, None]
    j = np.arange(256)[None, :]
    valid = (j > i) & (j <= i + 128)
    am = np.where(valid, 0.0, -30000.0).astype(np.float32)
    am0 = np.where(valid & (j >= 128), 0.0, -30000.0).astype(np.float32)
    r = np.arange(128) // 16
    cm = (r[None, :] >= r[:, None]).astype(np.float32)
    jm = np.zeros((128, 128), np.float32)
    jm[np.arange(64) + 64, np.arange(64)] = -1.0
    jm[np.arange(64), np.arange(64) + 64] = 1.0
    kv = np.array([0, -1, -2, -3, -4, -5, -6, -7, 0, 1, 2, 3, 4, 5, 6, 7, 8], np.float32)
    mv = np.array(list(range(-1, -33, -1)) + list(range(0, 32)), np.float32)
    return am, am0, cm, jm, np.ascontiguousarray(np.broadcast_to(kv[None], (128, 17))), np.ascontiguousarray(np.broadcast_to(mv[None], (128, 64)))


_AMASK, _AMASK0, _CMASK, _JMAT, _KVALS, _MVALS = _consts()


def prep_inputs(inputs):
    f = lambda a: np.ascontiguousarray(a, dtype=np.float32)
    x = inputs["x"]
    shared = {
        "ada_w": f(inputs["ada_w"]),
        "ada_bT": f(inputs["ada_b"].reshape(2, 96, 128).transpose(0, 2, 1)),
        "nmix": f(inputs["norm_mix"].reshape(2, KC, 128).transpose(0, 2, 1)),
        "nffn": f(inputs["norm_ffn"].reshape(2, KC, 128).transpose(0, 2, 1)),
        "fnorm": f(inputs["final_norm"].reshape(KC, 128).T),
        "router_w": f(np.concatenate([inputs["moe_r1_w"], inputs["moe_r2_w"]], axis=-1)),
        "router_b": f(np.broadcast_to(np.concatenate([inputs["moe_r1_b"], inputs["moe_r2_b"]], axis=-1)[:, None, :],
                                      (2, 128, 36))),
        "moe_w_gate": f(inputs["moe_w_gate"]),
        "moe_w_up": f(inputs["moe_w_up"]),
        "moe_w_down": f(inputs["moe_w_down"]),
        "ident_f": np.eye(128, dtype=np.float32),
        "ssd_w_in": f(inputs["ssd_w_in"][0]),
        "ssd_convw": f(inputs["ssd_conv_w"][0].T.reshape(48, 128, 4).transpose(1, 0, 2)),
        "ssd_convb": f(inputs["ssd_conv_b"][0].reshape(48, 128).T),
        "ssd_dtb": f(inputs["ssd_dt_bias"][0].reshape(64, 1)),
        "ssd_alog": f(np.broadcast_to(inputs["ssd_a_log"][0][None, :], (128, 64))),
        "ssd_dsk": f(np.broadcast_to(inputs["ssd_d"][0][None, :], (128, 64))),
        "ssd_nw": f(inputs["ssd_norm_w"][0].reshape(32, 128).T),
        "ssd_w_out": f(inputs["ssd_w_out"][0]),
        "tri_f": np.triu(np.ones((128, 128), np.float32)),
        "w_in0": f(inputs["mix_a_w_in"][0]),
        "w_glu": f(inputs["s5_w_glu"][0]),
        "w_out0": f(inputs["mix_a_w_out"][0]),
        "rope_inv": f(np.broadcast_to((1.0 / (np.float32(ROPE_THETA) ** (np.arange(0, 16, 2, dtype=np.float32) / np.float32(16))))[None, :], (128, 8))),
        "sinks": f(np.broadcast_to(inputs["attn_sinks"][0][None, :], (128, 16))),
        "s5_lam_re": f(np.concatenate([inputs["s5_lam_re"][0].T] * 2, axis=0)),
        "s5_lam_im": f(np.concatenate([inputs["s5_lam_im"][0].T] * 2, axis=0)),
        "s5_logdt": f(np.broadcast_to(inputs["s5_log_dt"][0][None, :], (128, 64))),
        "s5_b_re": f(np.concatenate([inputs["s5_b_re"][0].transpose(1, 0, 2)] * 2, axis=0)),
        "s5_b_im": f(np.concatenate([inputs["s5_b_im"][0].transpose(1, 0, 2)] * 2, axis=0)),
        "s5_c_re": f(np.concatenate([inputs["s5_c_re"][0].transpose(2, 0, 1)] * 2, axis=0)),
        "s5_c_im": f(np.concatenate([inputs["s5_c_im"][0].transpose(2, 0, 1)] * 2, axis=0)),
        "s5_d": f(np.broadcast_to(inputs["s5_d"][0][None, :], (128, 1024))),
        "cmask": _CMASK, "jmat": _JMAT, "kvals": _KVALS, "mvals": _MVALS, "amask": _AMASK,
        "negm4": f(np.tile(np.where(np.triu(np.ones((128, 128))) > 0, 0.0, -30000.0), (1, 4))),
    }
    maps = []
    for core in range(NCORES):
        b, s = core // 2, core % 2
        m = dict(shared)
        m["xT"] = f(x[b, s * NTOK:(s + 1) * NTOK, :].T)
        m["cT"] = f(inputs["c"][b].reshape(KC, 128).T)
        m["pmask"] = np.full((128, 1), float(s), np.float32)
        pos = np.ascontiguousarray(inputs["positions"][b, s * NTOK:(s + 1) * NTOK], dtype=np.int32)
        m["pos_tm"] = np.ascontiguousarray(pos.reshape(NB, 128).T)
        m["pos_last"] = np.ascontiguousarray(pos[-128:].reshape(128, 1))
        m["amask_first"] = _AMASK if s == 1 else _AMASK0
        maps.append(m)
    return maps


def kernel(**inputs):
    nc = build_program()
    maps = prep_inputs(inputs)
    res = run_bass_kernel_spmd(nc, maps, core_ids=list(range(NCORES)))
    out = np.empty((4, 4096, D), np.float32)
    for core in range(NCORES):
        b, s = core // 2, core % 2
        out[b, s * NTOK:(s + 1) * NTOK, :] = res.results[core]["outT"].T
    return out


SSD_IN = 4096
SSD_COLS = 10304
NCH = NTOK // 128


def declare_ssd_io(nc, io, dump=False):
    kd = "ExternalOutput" if dump else "Internal"
    def inp(name, shape, dt=F32):
        t = nc.dram_tensor(name, list(shape), dt, kind="ExternalInput")
        setattr(io, name, t)
    inp("ssd_w_in", [D, SSD_COLS])
    inp("ssd_convw", [128, 48, 4])
    inp("ssd_convb", [128, 48])
    inp("ssd_dtb", [64, 1])
    inp("ssd_alog", [128, 64])
    inp("ssd_dsk", [128, 64])
    inp("ssd_nw", [128, 32])
    inp("ssd_w_out", [SSD_IN, D])
    inp("tri_f", [128, 128])
    inp("negm4", [128, 512])
    inp("pmask", [128, 1])
    io.ZS = nc.dram_tensor("ssd_zs", [SSD_IN, NTOK], BF16)
    io.XBC = nc.dram_tensor("ssd_xbc", [6144, NTOK], BF16, kind=kd)
    io.DTT = nc.dram_tensor("ssd_dtt", [64, NTOK], F32, kind=kd)
    io.YN = nc.dram_tensor("ssd_yn", [SSD_IN, NTOK], BF16, kind=kd)
    io.st_src = nc.dram_tensor("ssd_st_src", [128, 4096], F32)
    io.st_dst = nc.dram_tensor("ssd_st_dst", [256, 4096], F32)
    io.ct_src = nc.dram_tensor("ssd_ct_src", [128, 144], F32)
    io.ct_dst = nc.dram_tensor("ssd_ct_dst", [256, 144], F32)


def ssd_stage1(mk, io, C, src):
    li = 1
    G1, SH1 = C.G1[li], C.MOD[li][:, 0:16]
    NP = 256
    with mk.scope():
        HT = mk.sb("HTs", [128, KC, NTOK], BF16)
        if True:
            XT = [mk.sb(f"XTs{j}", [128, KC, NP], F32) for j in range(2)]
            SQ = [mk.sb(f"SQs{j}", [128, KC, NP], BF16) for j in range(2)]
            RS = [mk.sb(f"RSs{j}", [128, NP], F32) for j in range(2)]
            PS = [mk.ps(f"PSs{j}", [128, NP], F32) for j in range(2)]
            W = [mk.sb(f"Wi{j}", [128, KC, 512], BF16) for j in range(2)]
            mk.dma("pool", W[0][:, :, 0:512], io.ssd_w_in.ap()[:, 0:512].rearrange("(kc p) n -> p kc n", p=128))
            for pc in range(NTOK // NP):
                norm_piece(mk, C, src, pc * NP, NP, G1, SH1, XT[pc % 2], SQ[pc % 2], PS[pc % 2], RS[pc % 2],
                           HT[:, :, pc * NP:(pc + 1) * NP], key=("HTs", pc))
        if True:
            CW = mk.sb("CW", [128, 48, 4], F32)
            CB = mk.sb("CB", [128, 48], F32)
            DTB = mk.sb("DTB", [64, 1], F32)
            RAW = [mk.sb(f"RAW{m}", [128, 515], F32) for m in range(4)]
            TAILS = mk.sb("TAILS", [128, 48, 3], F32)
            HEADS = mk.sb("HEADS", [128, 48, 3], F32)
            A1 = [mk.sb(f"A1{j}", [128, 512], F32) for j in range(2)]
            OUT = [mk.sb(f"OUTs{j}", [128, 512], BF16) for j in range(3)]
            DTO = mk.sb("DTO", [64, 512], F32)
            DT1 = mk.sb("DT1", [64, 512], F32)
            PP = [mk.ps(f"PPs{j}", [128, 512], F32) for j in range(3)]
            mk.dma("sp", CW[:], io.ssd_convw.ap())
            mk.dma("sp", CB[:], io.ssd_convb.ap())
            mk.dma("sp", DTB[:], io.ssd_dtb.ap())
            nblk = 21
            cnt = 0
            oc = 0

            def loadw(cb):
                c0 = cb * 512
                wcols = min(512, SSD_COLS - c0)
                mk.dma("pool", W[cb % 2][:, :, 0:wcols],
                       io.ssd_w_in.ap()[:, c0:c0 + wcols].rearrange("(kc p) n -> p kc n", p=128))
            for cb in range(nblk):
                if cb + 1 < nblk:
                    loadw(cb + 1)
                w = W[cb % 2]
                nm = 4 if cb < 20 else 1
                for t in range(NTOK // 512):
                    for m in range(nm):
                        pp = PP[cnt % 3]
                        cnt += 1
                        mrows = 128 if cb < 20 else 64

                        def f(e, pp=pp, w=w, m=m, t=t, mrows=mrows):
                            ins = None
                            for kc in range(KC):
                                ins = e.matmul(pp[0:mrows, :], w[:, kc, m * 128:m * 128 + mrows],
                                               HT[:, kc, t * 512:(t + 1) * 512], start=(kc == 0), stop=(kc == KC - 1))
                            return ins
                        mk.op("pe", f, reads=[w, ("HTs", 2 * t), ("HTs", 2 * t + 1)], writes=[pp])
                        ch = cb * 4 + m
                        if cb < 8:
                            o = OUT[oc % 3]
                            oc += 1
                            mk.op("act", lambda e, o=o, pp=pp: e.activation(out=o[:], in_=pp[:], func=AF.Silu),
                                  reads=[pp], writes=[o])
                            mk.dma("sp", io.ZS.ap()[ch * 128:(ch + 1) * 128, t * 512:(t + 1) * 512], o[:])
                        elif cb < 20:
                            cc = ch - 32
                            raw = RAW[m]
                            eng = "dve"
                            if t == 0:
                                mk.op("pool", lambda e, raw=raw: e.memset(raw[:, 0:3], 0.0), writes=[raw])
                            mk.op("act", lambda e, raw=raw, pp=pp: e.activation(out=raw[:, 3:515], in_=pp[:], func=AF.Identity),
                                  reads=[pp], writes=[raw])
                            if t == 0:
                                mk.op("pool", lambda e, raw=raw, cc=cc: e.tensor_copy(out=HEADS[:, cc, :], in_=raw[:, 3:6]),
                                      reads=[raw], writes=[("HEADS", cc)])
                            a1 = A1[m % 2]

                            def fc(e, raw=raw, a1=a1, cc=cc):
                                e.tensor_scalar(out=a1[:], in0=raw[:, 0:512], scalar1=CW[:, cc, 0:1], scalar2=CB[:, cc:cc + 1],
                                                op0=ALU.mult, op1=ALU.add)
                                return None
                            mk.op(eng, lambda e, raw=raw, a1=a1, cc=cc: e.tensor_scalar(
                                out=a1[:], in0=raw[:, 0:512], scalar1=CW[:, cc, 0:1], scalar2=CB[:, cc:cc + 1],
                                op0=ALU.mult, op1=ALU.add), reads=[raw, CW, CB], writes=[a1])
                            for j in range(1, 4):
                                mk.op(eng, lambda e, raw=raw, a1=a1, cc=cc, j=j: e.scalar_tensor_tensor(
                                    out=a1[:], in0=raw[:, j:j + 512], scalar=CW[:, cc, j:j + 1], in1=a1[:],
                                    op0=ALU.mult, op1=ALU.add), reads=[raw, a1, CW], writes=[a1])
                            o = OUT[oc % 3]
                            oc += 1
                            mk.op("act", lambda e, o=o, a1=a1: e.activation(out=o[:], in_=a1[:], func=AF.Silu),
                                  reads=[a1], writes=[o])
                            mk.dma("sp", io.XBC.ap()[cc * 128:(cc + 1) * 128, t * 512:(t + 1) * 512], o[:])
                            if t < NTOK // 512 - 1:
                                mk.op("pool", lambda e, raw=raw: e.tensor_copy(out=raw[:, 0:3], in_=raw[:, 512:515]),
                                      reads=[raw], writes=[raw])
                            else:
                                mk.op("pool", lambda e, raw=raw, cc=cc: e.tensor_copy(out=TAILS[:, cc, :], in_=raw[:, 512:515]),
                                      reads=[raw], writes=[("TAILS", cc)])
                        else:
                            mk.op("act", lambda e, pp=pp: e.activation(out=DTO[:], in_=pp[0:64, :], func=AF.Identity,
                                                                      bias=DTB[:, 0:1], scale=1.0),
                                  reads=[pp, DTB], writes=[DTO])
                            mk.op("act", lambda e: e.activation(out=DT1[:], in_=DTO[:], func=AF.Abs),
                                  reads=[DTO], writes=[DT1])
                            mk.op("act", lambda e: e.activation(out=DT1[:], in_=DT1[:], func=AF.Exp, scale=-1.0),
                                  reads=[DT1], writes=[DT1])
                            mk.op("act", lambda e: e.activation(out=DT1[:], in_=DT1[:], func=AF.Ln, bias=C.one[0:64, 0:1], scale=1.0),
                                  reads=[DT1, C.one], writes=[DT1])
                            mk.op("dve", lambda e: e.scalar_tensor_tensor(out=DTO[:], in0=DTO[:], scalar=0.0, in1=DT1[:],
                                                                          op0=ALU.max, op1=ALU.add),
                                  reads=[DTO, DT1], writes=[DTO])
                            mk.dma("sp", io.DTT.ap()[:, t * 512:(t + 1) * 512], DTO[:])
            mk.dma("sp", io.ct_src.ap(), TAILS[:].rearrange("p c t -> p (c t)"),
                   reads=[("TAILS", c) for c in range(48)], writes=[io.ct_src])
            mk.cc_allgather(io.ct_dst, io.ct_src, PAIRS)
            PRV = mk.sb("PRV", [128, 48, 6], F32)
            PM_ = mk.sb("PMk", [128, 1], F32)
            FX = mk.sb("FX", [128, 48, 3], F32)
            FXT = mk.sb("FXT", [128, 48, 3], F32)
            FXB = mk.sb("FXB", [128, 48, 3], BF16)
            mk.dma("sp", PM_[:], io.pmask.ap())
            mk.dma("sp", PRV[:, :, 0:3], io.ct_dst.ap()[0:128, :].rearrange("p (c t) -> p c t", t=3))
            mk.op("dve", lambda e: e.tensor_scalar(out=PRV[:, :, 0:3], in0=PRV[:, :, 0:3], scalar1=PM_[:, 0:1], scalar2=None,
                                                   op0=ALU.mult), reads=[PRV, PM_], writes=[PRV])
            mk.op("dve", lambda e: e.tensor_copy(out=PRV[:, :, 3:6], in_=HEADS[:]),
                  reads=[("HEADS", c) for c in range(48)] + [PRV], writes=[PRV])
            mk.op("dve", lambda e: e.tensor_tensor(out=FX[:], in0=PRV[:, :, 0:3], in1=CW[:, :, 0:1].to_broadcast([128, 48, 3]),
                                                   op=ALU.mult), reads=[PRV, CW], writes=[FX])
            for j in range(1, 4):
                mk.op("dve", lambda e, j=j: e.tensor_tensor(out=FXT[:], in0=PRV[:, :, j:j + 3],
                                                            in1=CW[:, :, j:j + 1].to_broadcast([128, 48, 3]), op=ALU.mult),
                      reads=[PRV, CW], writes=[FXT])
                mk.op("dve", lambda e: e.tensor_tensor(out=FX[:], in0=FX[:], in1=FXT[:], op=ALU.add),
                      reads=[FX, FXT], writes=[FX])
            mk.op("dve", lambda e: e.tensor_tensor(out=FX[:], in0=FX[:], in1=CB[:].unsqueeze(2).to_broadcast([128, 48, 3]),
                                                   op=ALU.add), reads=[FX, CB], writes=[FX])
            mk.op("act", lambda e: e.activation(out=FXB[:], in_=FX[:], func=AF.Silu), reads=[FX], writes=[FXB])
            for cc in range(48):
                mk.dma("sp", io.XBC.ap()[cc * 128:(cc + 1) * 128, 0:3], FXB[:, cc, :], reads=[FXB],
                       writes=[io.XBC.ap()[cc * 128:(cc + 1) * 128, 0:512]], allow_slow_non_contiguous=True)


def ssd_stage2(mk, io, C, mode="full"):
    with mk.scope():
        STATE = mk.sb("STATE", [128, 4096], F32)
        STBF = mk.sb("STBF", [128, 4096], BF16)
        TRI = mk.sb("TRI", [128, 128], F32)
        NEGM4 = mk.sb("NEGM4", [128, 512], F32)
        ONESF = mk.sb("ONESF", [128, 128], F32)
        ABC = mk.sb("ABC", [128, 64], F32)
        DSK = mk.sb("DSK", [128, 64], F32)
        NW = mk.sb("NW", [128, 32], F32)
        PMK = mk.sb("PMK2", [128, 1], F32)
        XTc = [mk.sb(f"XTc{j}", [128, 32, 128], BF16) for j in range(2)]
        ZSc = [mk.sb(f"ZSc{j}", [128, 32, 128], BF16) for j in range(2)]
        BTc = [mk.sb(f"BTc{j}", [128, 8, 128], BF16) for j in range(3)]
        CTc = [mk.sb(f"CTc{j}", [128, 8, 128], BF16) for j in range(3)]
        DTc = [mk.sb(f"DTc{j}", [64, 128], F32) for j in range(3)]
        DTt_ = [mk.sb(f"DTt{j}", [128, 64], F32) for j in range(2)]
        ADT_ = [mk.sb(f"ADT{j}", [128, 64], F32) for j in range(2)]
        ACS_ = [mk.sb(f"ACS{j}", [128, 64], F32) for j in range(2)]
        NACS_ = [mk.sb(f"NACS{j}", [128, 64], F32) for j in range(2)]
        ACST_ = [mk.sb(f"ACST{j}", [64, 128], F32) for j in range(2)]
        AEND_ = [mk.sb(f"AEND{j}", [128, 64], F32) for j in range(2)]
        DECST_ = [mk.sb(f"DECST{j}", [128, 64], F32) for j in range(2)]
        EA_ = [mk.sb(f"EA{j}", [128, 64], F32) for j in range(2)]
        DEC_ = [mk.sb(f"DEC{j}", [128, 64], F32) for j in range(2)]
        SEL = mk.sb("SEL", [64, 64, 128], BF16)
        NEGMB = mk.sb("NEGMB", [128, 128], BF16)
        HS_ = [[mk.sb(f"HS{k}_{j}", [64, 128], BF16) for k in range(3)] for j in range(2)]
        RSD = mk.sb("RSD", [64, 128], F32)
        LT = [mk.sb(f"LT{j}", [128, 4, 128], BF16) for j in range(2)]
        MT = [mk.sb(f"MT{j}", [128, 8, 128], BF16) for j in range(2)]
        GT_ = [mk.sb(f"GT{j}", [128, 8, 128], F32) for j in range(2)]
        XTOK = mk.sb("XTOK", [128, 4096], BF16)
        XDT = mk.sb("XDT", [128, 4096], BF16)
        XDD = mk.sb("XDD", [128, 4096], BF16)
        BTOK = mk.sb("BTOK", [128, 8, 128], BF16)
        YB = mk.sb("YB", [128, 4096], BF16)
        TMP = [mk.sb(f"TMPy{j}", [128, 512], F32) for j in range(2)]
        TM2 = [mk.sb(f"TM2y{j}", [128, 512], F32) for j in range(2)]
        YG = mk.sb("YG", [128, 32, 128], F32)
        SQG = mk.sb("SQG", [128, 32, 128], BF16)
        RSTD = mk.sb("RSTD", [128, 8, 128], F32)
        YNo = mk.sb("YNo", [128, 32, 128], BF16)
        B = [mk.ps(f"Bk{j}", [128, 512], F32) for j in range(8)]
        PXb = [mk.ps(f"PXb{j}", [128, 8, 128], BF16) for j in range(0)]

        mk.dma("sp", TRI[:], io.tri_f.ap())
        mk.dma("sp", NEGM4[:], io.negm4.ap())
        mk.dma("sp", ABC[:], io.ssd_alog.ap())
        mk.dma("sp", DSK[:], io.ssd_dsk.ap())
        mk.dma("sp", NW[:], io.ssd_nw.ap())
        mk.dma("sp", PMK[:], io.pmask.ap())
        mk.op("pool", lambda e: e.memset(ONESF[:], 1.0), writes=[ONESF])
        mk.op("dve", lambda e: e.tensor_copy(out=SEL[:], in_=C.ident_f[0:64, 0:64].unsqueeze(2).to_broadcast([64, 64, 128])),
              reads=[C.ident_f], writes=[SEL])
        mk.op("dve", lambda e: e.tensor_copy(out=NEGMB[:], in_=NEGM4[:, 0:128]), reads=[NEGM4], writes=[NEGMB])
        mk.op("act", lambda e: e.activation(out=ABC[:], in_=ABC[:], func=AF.Exp), reads=[ABC], writes=[ABC])
        mk.op("dve", lambda e: e.tensor_scalar(out=ABC[:], in0=ABC[:], scalar1=-1.0, scalar2=None, op0=ALU.mult),
              reads=[ABC], writes=[ABC])

        def bf(bank, shape):
            return bank[:].bitcast(BF16).rearrange("p (a b) -> p a b", a=8)[:, :, 0:128]

        def loads_small(c):
            k3 = c % 3
            cs = slice(c * 128, (c + 1) * 128)
            mk.dma("sp", BTc[k3][:], io.XBC.ap()[4096:5120, cs].rearrange("(kc p) t -> p kc t", p=128))
            mk.dma("sp", CTc[k3][:], io.XBC.ap()[5120:6144, cs].rearrange("(kc p) t -> p kc t", p=128))
            mk.dma("sp", DTc[k3][:], io.DTT.ap()[:, cs])

        def loads(c):
            j = c % 2
            cs = slice(c * 128, (c + 1) * 128)
            for qq in range(4):
                mk.dma("sp", XTc[j][:, qq * 8:(qq + 1) * 8, :],
                       io.XBC.ap()[qq * 1024:(qq + 1) * 1024, cs].rearrange("(kc p) t -> p kc t", p=128),
                       writes=[(XTc[j].name, qq)])

        def loadz(c):
            j = c % 2
            cs = slice(c * 128, (c + 1) * 128)
            for qq in range(4):
                mk.dma("sp", ZSc[j][:, qq * 8:(qq + 1) * 8, :],
                       io.ZS.ap()[qq * 1024:(qq + 1) * 1024, cs].rearrange("(kc p) t -> p kc t", p=128),
                       writes=[(ZSc[j].name, qq)])

        import os as _os
        CUT = int(_os.environ.get("CUT", "99"))
        SUBCUT = int(_os.environ.get("SUBCUT", "99"))

        def chunk(c, full, part):
            j = c % 2
            xt, bt, ct, dt = XTc[j], BTc[c % 3], CTc[c % 3], DTc[c % 3]
            DTt, ADT, ACS, NACS, ACST = DTt_[j], ADT_[j], ACS_[j], NACS_[j], ACST_[j]
            AEND, DECST, EA, DEC, GT = AEND_[j], DECST_[j], EA_[j], DEC_[j], GT_[j]
            if part == "dt":
                chunk_dt(c, full, xt, bt, ct, dt, DTt, ADT, ACS, NACS, ACST, AEND, DECST, EA, DEC, GT)
            else:
                chunk_main(c, full, j, xt, bt, ct, dt, DTt, ADT, ACS, NACS, ACST, AEND, DECST, EA, DEC, GT)

        def chunk_dt(c, full, xt, bt, ct, dt, DTt, ADT, ACS, NACS, ACST, AEND, DECST, EA, DEC, GT):
            mk.op("pe", lambda e: e.matmul(B[7][:, 0:64], dt[:, :], C.ident_f[0:64, 0:64], start=True, stop=True),
                  reads=[dt, C.ident_f], writes=[B[7]])
            if CUT <= 0:
                return
            mk.op("act", lambda e: e.activation(out=DTt[:], in_=B[7][:, 0:64], func=AF.Identity), reads=[B[7]], writes=[DTt])
            mk.op("dve", lambda e: e.tensor_tensor(out=ADT[:], in0=DTt[:], in1=ABC[:], op=ALU.mult),
                  reads=[DTt, ABC], writes=[ADT])

            def f(e):
                e.matmul(B[7][:, 64:128], TRI[:], ADT[:], start=True, stop=True)
                e.matmul(B[7][0:64, 128:256], ADT[:], TRI[:], start=True, stop=True)
                return e.matmul(B[7][:, 256:320], ONESF[:], ADT[:], start=True, stop=True)
            if CUT <= 1:
                return
            mk.op("pe", f, reads=[TRI, ADT, ONESF], writes=[B[7]])
            if CUT <= 2:
                return
            if CUT == 3 and SUBCUT <= 0:
                return
            mk.op("act", lambda e: e.activation(out=ACS[:], in_=B[7][:, 64:128], func=AF.Identity), reads=[B[7]], writes=[ACS])
            if CUT == 3 and SUBCUT <= 1:
                return
            mk.op("dve", lambda e: e.tensor_scalar(out=NACS[:], in0=ACS[:], scalar1=-1.0, scalar2=None, op0=ALU.mult),
                  reads=[ACS], writes=[NACS])
            if CUT == 3 and SUBCUT <= 2:
                return
            mk.op("act", lambda e: e.activation(out=ACST[:], in_=B[7][0:64, 128:256], func=AF.Identity), reads=[B[7]], writes=[ACST])
            if CUT == 3 and SUBCUT <= 3:
                return
            mk.op("dve", lambda e: e.tensor_copy(out=AEND[:], in_=B[7][:, 256:320]), reads=[B[7]], writes=[AEND])
            if CUT == 3 and SUBCUT <= 4:
                return
            mk.op("dve", lambda e: e.tensor_tensor(out=DECST[:], in0=AEND[:], in1=ACS[:], op=ALU.subtract),
                  reads=[AEND, ACS], writes=[DECST])
            if CUT == 3 and SUBCUT <= 5:
                return
            mk.op("act", lambda e: e.activation(out=DECST[:], in_=DECST[:], func=AF.Exp), reads=[DECST], writes=[DECST])
            if CUT == 3 and SUBCUT <= 6:
                return
            mk.op("act", lambda e: e.activation(out=DEC[:], in_=AEND[:], func=AF.Exp), reads=[AEND], writes=[DEC])
            if full:
                HS = HS_[c % 2]
                mk.op("dve", lambda e: e.tensor_copy(out=HS[0][:], in_=ACST[:]), reads=[ACST], writes=[HS[0]])
                mk.op("dve", lambda e: e.tensor_tensor(out=RSD[:], in0=ACST[:], in1=HS[0][:], op=ALU.subtract), reads=[ACST, HS[0]], writes=[RSD])
                mk.op("dve", lambda e: e.tensor_copy(out=HS[1][:], in_=RSD[:]), reads=[RSD], writes=[HS[1]])
                mk.op("dve", lambda e: e.tensor_tensor(out=RSD[:], in0=RSD[:], in1=HS[1][:], op=ALU.subtract), reads=[RSD, HS[1]], writes=[RSD])
                mk.op("dve", lambda e: e.tensor_copy(out=HS[2][:], in_=RSD[:]), reads=[RSD], writes=[HS[2]])
            if full:
                mk.op("act", lambda e: e.activation(out=EA[:], in_=ACS[:], func=AF.Exp), reads=[ACS], writes=[EA])
                for hf in range(2):
                    def fg(e, hf=hf):
                        ins = None
                        for g4 in range(4):
                            g = hf * 4 + g4
                            ins = e.matmul(B[2 + hf][:, g4 * 128:(g4 + 1) * 128], bt[:, g, :], ct[:, g, :], start=True, stop=True)
                        return ins
                    mk.op("pe", fg, reads=[bt, ct], writes=[B[2 + hf]])
                    mk.op("act", lambda e, hf=hf: e.activation(out=GT[:, hf * 4:(hf + 1) * 4, :].rearrange("p a b -> p (a b)"),
                                                                 in_=B[2 + hf][:], func=AF.Identity),
                          reads=[B[2 + hf]], writes=[GT])

        def chunk_main(c, full, j, xt, bt, ct, dt, DTt, ADT, ACS, NACS, ACST, AEND, DECST, EA, DEC, GT):
            for q in range(4):
                bank = B[q % 2]
                pv = bf(bank, None)

                def ft(e, q=q, pv=pv):
                    ins = None
                    for i in range(8):
                        ins = e.transpose(pv[:, i, :], xt[:, q * 8 + i, :], C.ident_b[:])
                    return ins
                mk.op("pe", ft, reads=[(xt.name, q), C.ident_b], writes=[bank])
                if full:
                    mk.op("act", lambda e, q=q, pv=pv: e.activation(
                        out=XTOK[:, q * 1024:(q + 1) * 1024].rearrange("p (a b) -> p a b", a=8), in_=pv, func=AF.Identity),
                        reads=[bank], writes=[("XTOK", q)])
                mk.op("dve", lambda e, q=q, pv=pv: e.tensor_tensor(
                    out=XDT[:, q * 1024:(q + 1) * 1024].rearrange("p (h d) -> p h d", h=16),
                    in0=pv.rearrange("p a (u d) -> p (a u) d", u=2),
                    in1=DTt[:, q * 16:(q + 1) * 16].unsqueeze(2).to_broadcast([128, 16, 64]), op=ALU.mult),
                    reads=[bank, DTt], writes=[("XDT", q)])
            if CUT <= 4:
                return
            mk.op("dve", lambda e: e.tensor_tensor(
                out=XDD[:].rearrange("p (h d) -> p h d", h=64), in0=XDT[:].rearrange("p (h d) -> p h d", h=64),
                in1=DECST[:].unsqueeze(2).to_broadcast([128, 64, 64]), op=ALU.mult),
                reads=[("XDT", q) for q in range(4)] + [DECST], writes=[XDD])
            if CUT <= 5:
                return
            bank = B[0]
            pv = bf(bank, None)

            def fb(e, pv=pv):
                ins = None
                for g in range(8):
                    ins = e.transpose(pv[:, g, :], bt[:, g, :], C.ident_b[:])
                return ins
            mk.op("pe", fb, reads=[bt, C.ident_b], writes=[bank])
            mk.op("act", lambda e, pv=pv: e.activation(out=BTOK[:], in_=pv, func=AF.Identity), reads=[bank], writes=[BTOK])
            if CUT <= 6:
                return
            acnt = [0]

            def lmat(g):
                mt = MT[g % 2]
                HS = HS_[c % 2]
                for q2 in range(2):
                    pa = B[4 + acnt[0] % 2]
                    lt = LT[acnt[0] % 2]
                    acnt[0] += 1

                    def fa(e, pa=pa, g=g, q2=q2):
                        ins = None
                        for i in range(4):
                            h = g * 8 + q2 * 4 + i
                            o = pa[:, i * 128:(i + 1) * 128]
                            for k in range(3):
                                e.matmul(o, SEL[:, h, :], HS[k][:], start=(k == 0), stop=False)
                            ins = e.matmul(o, C.ident_b[:], NEGMB[:], start=False, stop=True)
                        return ins
                    mk.op("pe", fa, reads=[HS[0], HS[1], HS[2], SEL, NEGMB, C.ident_b], writes=[pa])

                    def fe(e, pa=pa, lt=lt, g=g, q2=q2):
                        ins = None
                        for i in range(4):
                            h = g * 8 + q2 * 4 + i
                            ins = e.activation(out=lt[:, i, :], in_=pa[:, i * 128:(i + 1) * 128], func=AF.Exp,
                                               bias=NACS[:, h:h + 1], scale=1.0)
                        return ins
                    mk.op("act", fe, reads=[pa, NACS], writes=[lt])
                    mk.op("dve", lambda e, lt=lt, mt=mt, g=g, q2=q2: e.tensor_tensor(
                        out=mt[:, q2 * 4:(q2 + 1) * 4, :], in0=lt[:],
                        in1=GT[:, g, :].unsqueeze(1).to_broadcast([128, 4, 128]), op=ALU.mult),
                        reads=[lt, GT], writes=[(mt.name, q2)])

            if full:
                lmat(0)
            for g in range(8):
                if full:
                    mt = MT[g % 2]
                    if g + 1 < 8:
                        lmat(g + 1)

                    def fy(e, mt=mt, g=g):
                        ins = None
                        for hh in range(8):
                            h = g * 8 + hh
                            ins = e.matmul(B[6][:, hh * 64:(hh + 1) * 64], mt[:, hh, :], XDT[:, h * 64:(h + 1) * 64],
                                           start=True, stop=True)
                        return ins
                    mk.op("pe", fy, reads=[(mt.name, 0), (mt.name, 1), ("XDT", g // 2)], writes=[B[6]])
                    mk.op("pe", lambda e, g=g: e.matmul(B[7][:], ct[:, g, :], STBF[:, g * 512:(g + 1) * 512], start=True, stop=True),
                          reads=[ct, ("STBF", g)], writes=[B[7]])
                    tmp, tm2 = TMP[g % 2], TM2[g % 2]
                    mk.op("dve", lambda e, tmp=tmp, g=g: e.tensor_tensor(
                        out=tmp[:].rearrange("p (h d) -> p h d", h=8), in0=B[7][:].rearrange("p (h d) -> p h d", h=8),
                        in1=EA[:, g * 8:(g + 1) * 8].unsqueeze(2).to_broadcast([128, 8, 64]), op=ALU.mult),
                        reads=[B[7], EA], writes=[tmp])
                    mk.op("dve", lambda e, tm2=tm2, g=g: e.tensor_tensor(
                        out=tm2[:].rearrange("p (h d) -> p h d", h=8),
                        in0=XTOK[:, g * 512:(g + 1) * 512].rearrange("p (h d) -> p h d", h=8),
                        in1=DSK[:, g * 8:(g + 1) * 8].unsqueeze(2).to_broadcast([128, 8, 64]), op=ALU.mult),
                        reads=[("XTOK", g // 2), DSK], writes=[tm2])
                    mk.op("pool", lambda e, tmp=tmp, tm2=tm2: e.tensor_tensor(out=tm2[:], in0=tm2[:], in1=tmp[:], op=ALU.add),
                          reads=[tmp, tm2], writes=[tm2])
                    mk.op("dve", lambda e, tm2=tm2, g=g: e.tensor_tensor(out=YB[:, g * 512:(g + 1) * 512], in0=B[6][:], in1=tm2[:],
                                                                         op=ALU.add),
                          reads=[B[6], tm2], writes=[("YB", g)])
                pst = B[4 + g % 2] if not full else B[g % 2]
                mk.op("pe", lambda e, pst=pst, g=g: e.matmul(pst[:], BTOK[:, g, :], XDD[:, g * 512:(g + 1) * 512], start=True, stop=True),
                      reads=[BTOK, XDD], writes=[pst])
                eng = "dve"
                mk.op(eng, lambda e, g=g: e.tensor_tensor(
                    out=STATE[:, g * 512:(g + 1) * 512].rearrange("p (h d) -> p h d", h=8),
                    in0=STATE[:, g * 512:(g + 1) * 512].rearrange("p (h d) -> p h d", h=8),
                    in1=DEC[:, g * 8:(g + 1) * 8].unsqueeze(2).to_broadcast([128, 8, 64]), op=ALU.mult),
                    reads=[("ST", g), DEC], writes=[("ST", g)])
                mk.op("dve", lambda e, g=g, pst=pst: e.tensor_tensor(out=STATE[:, g * 512:(g + 1) * 512],
                                                                     in0=STATE[:, g * 512:(g + 1) * 512], in1=pst[:], op=ALU.add),
                      reads=[("ST", g), pst], writes=[("ST", g)])
                if full:
                    mk.op("act", lambda e, g=g: e.activation(out=STBF[:, g * 512:(g + 1) * 512], in_=STATE[:, g * 512:(g + 1) * 512],
                                                             func=AF.Identity),
                          reads=[("ST", g)], writes=[("STBF", g)])
            if not full:
                return
            if CUT <= 28:
                return
            zs = ZSc[j]
            for q in range(4):
                bank = B[q % 2]
                pv = bf(bank, None)

                def fq(e, q=q, pv=pv):
                    ins = None
                    for i in range(8):
                        kc = q * 8 + i
                        ins = e.transpose(pv[:, i, :], YB[:, kc * 128:(kc + 1) * 128], C.ident_b[:])
                    return ins
                mk.op("pe", fq, reads=[("YB", 2 * q), ("YB", 2 * q + 1), C.ident_b], writes=[bank])
                mk.op("dve", lambda e, q=q, pv=pv: e.tensor_tensor(out=YG[:, q * 8:(q + 1) * 8, :], in0=pv,
                                                                    in1=zs[:, q * 8:(q + 1) * 8, :], op=ALU.mult),
                      reads=[bank, (zs.name, q)], writes=[("YG", q)])
                mk.op("act", lambda e, q=q: e.activation(out=SQG[:, q * 8:(q + 1) * 8, :], in_=YG[:, q * 8:(q + 1) * 8, :], func=AF.Square),
                      reads=[("YG", q)], writes=[("SQG", q)])
            if CUT <= 29:
                return
            for hf in range(2):
                def fs(e, hf=hf):
                    ins = None
                    for g4 in range(4):
                        g = hf * 4 + g4
                        for i in range(4):
                            ins = e.matmul(B[2 + hf][:, g4 * 128:(g4 + 1) * 128], C.ones_b[:], SQG[:, g * 4 + i, :],
                                           start=(i == 0), stop=(i == 3))
                    return ins
                mk.op("pe", fs, reads=[("SQG", 2 * hf), ("SQG", 2 * hf + 1), C.ones_b], writes=[B[2 + hf]])
                rs = RSTD[:, hf * 4:(hf + 1) * 4, :].rearrange("p a b -> p (a b)")
                mk.op("act", lambda e, hf=hf, rs=rs: e.activation(out=rs, in_=B[2 + hf][:], func=AF.Sqrt, bias=C.eps[:, 0:1],
                                                                   scale=1.0 / 512.0),
                      reads=[B[2 + hf], C.eps], writes=[("RSTD", hf)])
                mk.op("dve", lambda e, rs=rs: e.reciprocal(out=rs, in_=rs), reads=[("RSTD", hf)], writes=[("RSTD", hf)])

            if CUT <= 30:
                return

            def fn(e):
                ins = None
                for kc in range(32):
                    ins = e.scalar_tensor_tensor(out=YNo[:, kc, :], in0=YG[:, kc, :], scalar=NW[:, kc:kc + 1],
                                                 in1=RSTD[:, kc // 4, :], op0=ALU.mult, op1=ALU.mult)
                return ins
            mk.op("dve", fn, reads=[("YG", q) for q in range(4)] + [("RSTD", 0), ("RSTD", 1), NW], writes=[YNo])
            mk.dma("sp", io.YN.ap()[:, c * 128:(c + 1) * 128].rearrange("(kc p) t -> p kc t", p=128), YNo[:])

        mk.op("pool", lambda e: e.memset(STATE[:], 0.0), writes=[("ST", g) for g in range(8)])

        def run_pass(full):
            loads_small(0)
            if NCH > 1:
                loads_small(1)
            loads(0)
            if full:
                loadz(0)
            chunk(0, full, "dt")
            for c in range(NCH):
                if c + 2 < NCH:
                    loads_small(c + 2)
                if c + 1 < NCH:
                    loads(c + 1)
                    if full:
                        loadz(c + 1)
                    chunk(c + 1, full, "dt")
                chunk(c, full, "main")
        run_pass(False)
        if mode == "pre":
            mk.dma("sp", io.st_src.ap(), STATE[:], reads=[("ST", g) for g in range(8)], writes=[io.st_src])
            return
        mk.dma("sp", io.st_src.ap(), STATE[:], reads=[("ST", g) for g in range(8)], writes=[io.st_src])
        mk.cc_allgather(io.st_dst, io.st_src, PAIRS)
        mk.dma("sp", STATE[:], io.st_dst.ap()[0:128, :], reads=[io.st_dst], writes=[("ST", g) for g in range(8)])
        mk.op("dve", lambda e: e.tensor_scalar(out=STATE[:], in0=STATE[:], scalar1=PMK[:, 0:1], scalar2=None, op0=ALU.mult),
              reads=[("ST", g) for g in range(8)] + [PMK], writes=[("ST", g) for g in range(8)])
        mk.op("act", lambda e: e.activation(out=STBF[:], in_=STATE[:], func=AF.Identity),
              reads=[("ST", g) for g in range(8)], writes=[("STBF", g) for g in range(8)])
        if mode == "precc":
            return
        run_pass(True)


def ssd_stage3(mk, io, C, src, dst):
    li = 1
    g1 = C.MOD[li][:, 32:48]
    with mk.scope():
        YT = [mk.sb(f"YT3{j}", [128, 32, 512], BF16) for j in range(2)]
        W = mk.sb("W3", [128, 32, D], BF16)
        PO = [mk.ps(f"PO3{j}", [128, 512], F32) for j in range(2)]
        for qq in range(4):
            mk.dma("pool", W[:, :, qq * 512:(qq + 1) * 512],
                   io.ssd_w_out.ap()[:, qq * 512:(qq + 1) * 512].rearrange("(kc p) d -> p kc d", p=128), writes=[("W3", qq)])
        XR = [mk.sb(f"XR3b{j}", [128, 512], F32) for j in range(4)]
        its = [(t, dc) for t in range(NTOK // 512) for dc in range(KC)]

        def ld_yt(t):
            mk.dma("sp", YT[t % 2][:], io.YN.ap()[:, t * 512:(t + 1) * 512].rearrange("(kc p) t -> p kc t", p=128))

        def ld_x(n):
            t, dc = its[n]
            mk.dma("sp", XR[n % 4][:], src.ap()[dc * 128:(dc + 1) * 128, t * 512:(t + 1) * 512])
        ld_yt(0)
        ld_x(0)
        ld_x(1)
        for n, (t, dc) in enumerate(its):
            yt = YT[t % 2]
            if dc == 0 and t + 1 < NTOK // 512:
                ld_yt(t + 1)
            if n + 2 < len(its):
                ld_x(n + 2)
            xr = XR[n % 4]
            po = PO[n % 2]

            def f(e, yt=yt, po=po, dc=dc):
                ins = None
                for kc in range(32):
                    ins = e.matmul(po[:], W[:, kc, dc * 128:(dc + 1) * 128], yt[:, kc, :], start=(kc == 0), stop=(kc == 31))
                return ins
            mk.op("pe", f, reads=[("W3", dc // 4), yt], writes=[po])
            mk.op("dve", lambda e, xr=xr, po=po, dc=dc: e.scalar_tensor_tensor(
                out=xr[:], in0=po[:], scalar=g1[:, dc:dc + 1], in1=xr[:], op0=ALU.mult, op1=ALU.add),
                reads=[po, xr, C.MOD[li]], writes=[xr])
            mk.dma("sp", dst.ap()[dc * 128:(dc + 1) * 128, t * 512:(t + 1) * 512], xr[:])


def phase_ssd(mk, io, C, src, dst, stages=3, mode="full"):
    ssd_stage1(mk, io, C, src)
    if stages >= 2:
        ssd_stage2(mk, io, C, mode)
    if stages >= 3:
        ssd_stage3(mk, io, C, src, dst)


ROPE_THETA = 500000.0
TWO_PI = 6.283185307179586
NB = NTOK // 128


def declare_mix0_io(nc, io, dump=False):
    kd = "ExternalOutput" if dump else "Internal"
    def inp(name, shape, dt=F32):
        t = nc.dram_tensor(name, list(shape), dt, kind="ExternalInput")
        setattr(io, name, t)
    inp("w_in0", [D, 2304])
    inp("w_glu", [1024, 2048])
    inp("w_out0", [D, D])
    inp("pos_tm", [128, NB], I32)
    inp("pos_last", [128, 1], I32)
    inp("rope_inv", [128, 8])
    inp("amask", [128, 256])
    inp("amask_first", [128, 256])
    inp("sinks", [128, 16])
    inp("s5_lam_re", [128, 64])
    inp("s5_lam_im", [128, 64])
    inp("s5_logdt", [128, 64])
    inp("s5_b_re", [128, 64, 16])
    inp("s5_b_im", [128, 64, 16])
    inp("s5_c_re", [128, 64, 16])
    inp("s5_c_im", [128, 64, 16])
    inp("s5_d", [128, 1024])
    inp("cmask", [128, 128])
    inp("jmat", [128, 128])
    inp("kvals", [128, 17])
    inp("mvals", [128, 64])
    io.YACT = nc.dram_tensor("m0_yact", [1024, NTOK], BF16, kind=kd)
    io.YATT = nc.dram_tensor("m0_yatt", [1024, NTOK], BF16, kind=kd)
    io.kv_src = nc.dram_tensor("m0_kv_src", [128, 256], F32)
    io.kv_dst = nc.dram_tensor("m0_kv_dst", [256, 256], F32)
    io.s5_src = nc.dram_tensor("m0_s5_src", [128, 64], F32)
    io.s5_dst = nc.dram_tensor("m0_s5_dst", [256, 64], F32)
    io.SL = nc.dram_tensor("m0_sl", [8, 2, 128, 2048], F32)
    io.UT = nc.dram_tensor("m0_ut2", [8, 128, 2048], BF16)
    io.UCM = nc.dram_tensor("m0_ucm2", [2, 128, 8192], BF16)
    io.S5M = nc.dram_tensor("m0_s5m", [4, 128, 8192], BF16)
    io.YGB = nc.dram_tensor("m0_ygb", [2, 128, 8192], BF16)


def sincos(mk, ang, shape, tmp, tmpi, out_sin, out_cos):
    for (off, dst) in ((0.0, out_sin), (0.25, out_cos)):
        mk.op("dve", lambda e, off=off: e.tensor_scalar(out=tmp, in0=ang, scalar1=1.0 / TWO_PI, scalar2=off,
                                                        op0=ALU.mult, op1=ALU.add), reads=[ang], writes=[tmp])
        mk.op("dve", lambda e: e.tensor_copy(out=tmpi, in_=tmp), reads=[tmp], writes=[tmpi])
        mk.op("dve", lambda e, dst=dst: e.tensor_copy(out=dst, in_=tmpi), reads=[tmpi], writes=[dst])
        mk.op("dve", lambda e, dst=dst: e.tensor_tensor(out=tmp, in0=tmp, in1=dst, op=ALU.subtract), reads=[tmp, dst], writes=[tmp])
        mk.op("dve", lambda e, dst=dst: e.tensor_scalar(out=dst, in0=tmp, scalar1=0.5, scalar2=None, op0=ALU.is_gt),
              reads=[tmp], writes=[dst])
        mk.op("dve", lambda e, dst=dst: e.tensor_tensor(out=tmp, in0=tmp, in1=dst, op=ALU.subtract), reads=[tmp, dst], writes=[tmp])
        mk.op("dve", lambda e, dst=dst: e.tensor_scalar(out=dst, in0=tmp, scalar1=-0.5, scalar2=None, op0=ALU.is_lt),
              reads=[tmp], writes=[dst])
        mk.op("dve", lambda e, dst=dst: e.tensor_tensor(out=tmp, in0=tmp, in1=dst, op=ALU.add), reads=[tmp, dst], writes=[tmp])
        mk.op("act", lambda e, dst=dst: e.activation(out=dst, in_=tmp, func=AF.Sin, scale=TWO_PI), reads=[tmp], writes=[dst])


def phase_attn(mk, io, C, src):
    li = 0
    G1, SH1 = C.G1[li], C.MOD[li][:, 0:16]
    with mk.scope():
        W = mk.sb("Wqkv", [128, KC, 1280], BF16)
        KT = mk.sb("KTall", [128, (NB + 1) * 128], BF16)
        VA = mk.sb("VAll", [128, NB + 1, 128], BF16)
        XT = mk.sb("XTa", [128, KC, 128], F32)
        SQ = mk.sb("SQa", [128, KC, 128], BF16)
        RS = mk.sb("RSa", [128, 128], F32)
        HT = mk.sb("HTa", [128, KC, 128], BF16)
        POSI = mk.sb("POSI", [128, NB + 1], I32)
        POSF = mk.sb("POSF", [128, NB + 1], F32)
        INV = mk.sb("INV", [128, 8], F32)
        ANG = mk.sb("ANG", [128, NB + 1, 8], F32)
        TMPA = mk.sb("TMPA", [128, NB + 1, 8], F32)
        TMPI = mk.sb("TMPI", [128, NB + 1, 8], I32)
        SIN = mk.sb("SINt", [128, NB + 1, 8], F32)
        COS = mk.sb("COSt", [128, NB + 1, 8], F32)
        MASK = mk.sb("MASK", [128, 256], F32)
        MASKF = mk.sb("MASKF", [128, 256], F32)
        SINK = mk.sb("SINK", [128, 16], F32)
        PMK = mk.sb("PMKa", [128, 1], F32)
        QF = mk.sb("QF", [128, 1280], F32)
        R1 = mk.sb("R1", [128, 18, 8], F32)
        R2 = mk.sb("R2", [128, 18, 8], F32)
        R3 = mk.sb("R3", [128, 18, 8], F32)
        QB = mk.sb("QB", [128, 8, 2, 64], BF16)
        KB = mk.sb("KB", [128, 128], BF16)
        QT = mk.sb("QT", [128, 8, 128], BF16)
        SC = mk.sb("SC", [128, 8, 256], F32)
        MX = mk.sb("MX", [128, 8], F32)
        NEG = mk.sb("NEGa", [128, 8], F32)
        SUM = mk.sb("SUMa", [128, 8], F32)
        ES = mk.sb("ESa", [128, 8], F32)
        P = mk.sb("Pa", [128, 8, 256], BF16)
        PT = mk.sb("PTa", [128, 2, 8, 128], BF16)
        YTOK = mk.sb("YTOKa", [128, 1024], BF16)
        YTT = mk.sb("YTTa", [128, 8, 128], BF16)
        KVX = mk.sb("KVX", [128, 256], F32)
        B = [mk.ps(f"Ba{j}", [128, 512], F32) for j in range(8)]

        def bfv(bank):
            return bank[:].bitcast(BF16).rearrange("p (a b) -> p a b", a=8)

        mk.dma("pool", W[:], io.w_in0.ap()[:, 1024:2304].rearrange("(kc p) n -> p kc n", p=128))
        mk.dma("sp", POSI[:, 1:NB + 1], io.pos_tm.ap())
        mk.dma("sp", POSI[:, 0:1], io.pos_last.ap())
        mk.dma("sp", INV[:], io.rope_inv.ap())
        mk.dma("sp", MASK[:], io.amask.ap())
        mk.dma("sp", MASKF[:], io.amask_first.ap())
        mk.dma("sp", SINK[:], io.sinks.ap())
        mk.dma("sp", PMK[:], io.pmask.ap())
        mk.op("dve", lambda e: e.tensor_copy(out=POSF[:], in_=POSI[:]), reads=[POSI], writes=[POSF])
        mk.op("dve", lambda e: e.tensor_tensor(out=ANG[:], in0=POSF[:].unsqueeze(2).to_broadcast([128, NB + 1, 8]),
                                               in1=INV[:].unsqueeze(1).to_broadcast([128, NB + 1, 8]), op=ALU.mult),
              reads=[POSF, INV], writes=[ANG])
        sincos(mk, ANG[:], None, TMPA[:], TMPI[:], SIN[:], COS[:])

        def project(col0, ncols_tok, slot, do_q):
            norm_piece(mk, C, src, col0, 128, G1, SH1, XT, SQ, B[7][:, 0:128], RS, HT[:], key=HT)
            ranges = ([(0, 512, B[4]), (512, 512, B[5])] if do_q else []) + [(1024, 256, B[6])]
            for (c0, w, bank) in ranges:
                def f(e, c0=c0, w=w, bank=bank):
                    ins = None
                    for kc in range(KC):
                        ins = e.matmul(bank[:, 0:w], HT[:, kc, :], W[:, kc, c0:c0 + w], start=(kc == 0), stop=(kc == KC - 1))
                    return ins
                mk.op("pe", f, reads=[HT, W], writes=[bank])
                sc = 0.125 if c0 < 1024 else 1.0
                mk.op("act", lambda e, c0=c0, w=w, bank=bank, sc=sc: e.activation(out=QF[:, c0:c0 + w], in_=bank[:, 0:w],
                                                                                func=AF.Identity, scale=sc),
                      reads=[bank], writes=[("QF", c0)])
            h0 = 0 if do_q else 16
            nh = 18 - h0
            qv = QF[:, 0:1152].rearrange("p (h d) -> p h d", d=64)[:, h0:18, :]
            x1, x2 = qv[:, :, 0:8], qv[:, :, 8:16]
            cs = COS[:, slot, :].unsqueeze(1).to_broadcast([128, nh, 8])
            sn = SIN[:, slot, :].unsqueeze(1).to_broadcast([128, nh, 8])
            r1, r2, r3 = R1[:, 0:nh, :], R2[:, 0:nh, :], R3[:, 0:nh, :]
            rd = [("QF", 0), ("QF", 512), ("QF", 1024), COS, SIN]
            mk.op("dve", lambda e: e.tensor_tensor(out=r1, in0=x1, in1=cs, op=ALU.mult), reads=rd, writes=[R1])
            mk.op("dve", lambda e: e.tensor_tensor(out=r2, in0=x2, in1=sn, op=ALU.mult), reads=rd, writes=[R2])
            mk.op("dve", lambda e: e.tensor_tensor(out=r1, in0=r1, in1=r2, op=ALU.subtract), reads=[R1, R2], writes=[R1])
            mk.op("dve", lambda e: e.tensor_tensor(out=r2, in0=x2, in1=cs, op=ALU.mult), reads=rd + [R1], writes=[R2])
            mk.op("dve", lambda e: e.tensor_tensor(out=r3, in0=x1, in1=sn, op=ALU.mult), reads=rd, writes=[R3])
            mk.op("dve", lambda e: e.tensor_tensor(out=x2, in0=r2, in1=r3, op=ALU.add), reads=[R2, R3],
                  writes=[("QF", 0), ("QF", 512), ("QF", 1024)])
            mk.op("dve", lambda e: e.tensor_copy(out=x1, in_=r1), reads=[R1], writes=[("QF", 0), ("QF", 512), ("QF", 1024)])
            mk.op("dve", lambda e: e.tensor_copy(out=KB[:], in_=QF[:, 1024:1152]), reads=[("QF", 1024)], writes=[KB])
            mk.op("pe", lambda e: e.transpose(bfv(B[7])[:, 0, :], KB[:], C.ident_b[:]), reads=[KB, C.ident_b], writes=[B[7]])
            mk.op("act", lambda e, slot=slot: e.activation(out=KT[:, slot * 128:(slot + 1) * 128], in_=bfv(B[7])[:, 0, :],
                                                           func=AF.Identity), reads=[B[7]], writes=[("KT", slot)])
            mk.op("act", lambda e, slot=slot: e.activation(out=VA[:, slot, :], in_=QF[:, 1152:1280], func=AF.Identity),
                  reads=[("QF", 1024)], writes=[("VA", slot)])

        project(NTOK - 128, 128, NB, False)
        mk.op("dve", lambda e: e.tensor_copy(out=KVX[:, 0:128], in_=KT[:, NB * 128:(NB + 1) * 128]), reads=[("KT", NB)], writes=[KVX])
        mk.op("dve", lambda e: e.tensor_copy(out=KVX[:, 128:256], in_=VA[:, NB, :]), reads=[("VA", NB), KVX], writes=[KVX])
        mk.dma("sp", io.kv_src.ap(), KVX[:])
        mk.cc_allgather(io.kv_dst, io.kv_src, PAIRS)
        mk.dma("sp", KVX[:], io.kv_dst.ap()[0:128, :])
        mk.op("dve", lambda e: e.tensor_copy(out=KT[:, 0:128], in_=KVX[:, 0:128]), reads=[KVX], writes=[("KT", 0)])
        mk.op("dve", lambda e: e.tensor_copy(out=VA[:, 0, :], in_=KVX[:, 128:256]), reads=[KVX], writes=[("VA", 0)])

        for blk in range(NB):
            project(blk * 128, 128, blk + 1, True)
            mk.op("dve", lambda e: e.tensor_copy(out=QB[:], in_=QF[:, 0:1024].rearrange("p (u i d) -> p i u d", u=2, i=8)),
                  reads=[("QF", 0), ("QF", 512)], writes=[QB])

            def ftq(e):
                ins = None
                for i in range(8):
                    ins = e.transpose(bfv(B[7])[:, i, :], QB[:, i, :, :].rearrange("p u d -> p (u d)"), C.ident_b[:])
                return ins
            mk.op("pe", ftq, reads=[QB, C.ident_b], writes=[B[7]])
            mk.op("act", lambda e: e.activation(out=QT[:], in_=bfv(B[7]), func=AF.Identity), reads=[B[7]], writes=[QT])
            msk = MASKF if blk == 0 else MASK
            for half in range(2):
                def fs(e, half=half, blk=blk):
                    ins = None
                    for i in range(8):
                        ins = e.matmul(B[i // 2][:, (i % 2) * 256:(i % 2) * 256 + 256],
                                       QT[half * 64:(half + 1) * 64, i, :],
                                       KT[half * 64:(half + 1) * 64, blk * 128:blk * 128 + 256], start=True, stop=True)
                    return ins
                mk.op("pe", fs, reads=[QT, ("KT", blk), ("KT", blk + 1)], writes=[B[0], B[1], B[2], B[3]])
                for jb in range(4):
                    mk.op("dve", lambda e, jb=jb, msk=msk: e.tensor_tensor(
                        out=SC[:, jb * 2:jb * 2 + 2, :], in0=B[jb][:].rearrange("p (a b) -> p a b", a=2),
                        in1=msk[:].unsqueeze(1).to_broadcast([128, 2, 256]), op=ALU.add),
                        reads=[B[jb], msk], writes=[("SC", jb)])
                scr = [("SC", jb) for jb in range(4)]
                mk.op("dve", lambda e: e.tensor_reduce(out=MX[:], in_=SC[:], axis=AX.X, op=ALU.max), reads=scr, writes=[MX])
                mk.op("dve", lambda e, half=half: e.tensor_tensor(out=MX[:], in0=MX[:], in1=SINK[:, half * 8:(half + 1) * 8], op=ALU.max),
                      reads=[MX, SINK], writes=[MX])
                mk.op("dve", lambda e: e.tensor_scalar(out=NEG[:], in0=MX[:], scalar1=-1.0, scalar2=None, op0=ALU.mult),
                      reads=[MX], writes=[NEG])
                mk.op("pool", lambda e: e.memset(SUM[:], 0.0), writes=[SUM])

                def fe(e):
                    ins = None
                    for i in range(8):
                        ins = e.activation(out=P[:, i, :], in_=SC[:, i, :], func=AF.Exp, bias=NEG[:, i:i + 1], scale=1.0,
                                           accum_out=SUM[:, i:i + 1])
                    return ins
                mk.op("act", fe, reads=scr + [NEG, SUM], writes=[P, SUM])
                mk.op("dve", lambda e, half=half: e.tensor_tensor(out=ES[:], in0=SINK[:, half * 8:(half + 1) * 8], in1=MX[:], op=ALU.subtract),
                      reads=[SINK, MX], writes=[ES])
                mk.op("act", lambda e: e.activation(out=ES[:], in_=ES[:], func=AF.Exp), reads=[ES], writes=[ES])
                mk.op("dve", lambda e: e.tensor_tensor(out=ES[:], in0=ES[:], in1=SUM[:], op=ALU.add), reads=[ES, SUM], writes=[ES])
                mk.op("dve", lambda e: e.reciprocal(out=ES[:], in_=ES[:]), reads=[ES], writes=[ES])
                for kj in range(2):
                    bank = B[4 + kj]

                    def fpt(e, kj=kj, bank=bank):
                        ins = None
                        for i in range(8):
                            ins = e.transpose(bfv(bank)[:, i, :], P[:, i, kj * 128:(kj + 1) * 128], C.ident_b[:])
                        return ins
                    mk.op("pe", fpt, reads=[P, C.ident_b], writes=[bank])
                    mk.op("act" if kj == 0 else "dve", (lambda e, kj=kj, bank=bank: e.activation(out=PT[:, kj, :, :], in_=bfv(bank), func=AF.Identity))
                          if kj == 0 else (lambda e, kj=kj, bank=bank: e.tensor_copy(out=PT[:, kj, :, :], in_=bfv(bank))),
                          reads=[bank], writes=[("PT", kj)])

                def fpv(e, half=half, blk=blk):
                    ins = None
                    for i in range(8):
                        for kj in range(2):
                            ins = e.matmul(B[6][:, i * 64:(i + 1) * 64], PT[:, kj, i, :],
                                           VA[:, blk + kj, half * 64:(half + 1) * 64], start=(kj == 0), stop=(kj == 1))
                    return ins
                mk.op("pe", fpv, reads=[("PT", 0), ("PT", 1), ("VA", blk), ("VA", blk + 1)], writes=[B[6]])
                mk.op("dve", lambda e, half=half: e.tensor_tensor(
                    out=YTOK[:, half * 512:(half + 1) * 512].rearrange("p (h d) -> p h d", h=8),
                    in0=B[6][:].rearrange("p (h d) -> p h d", h=8),
                    in1=ES[:].unsqueeze(2).to_broadcast([128, 8, 64]), op=ALU.mult),
                    reads=[B[6], ES], writes=[("YTOK", half)])

            def fty(e):
                ins = None
                for i in range(8):
                    ins = e.transpose(bfv(B[7])[:, i, :], YTOK[:, i * 128:(i + 1) * 128], C.ident_b[:])
                return ins
            mk.op("pe", fty, reads=[("YTOK", 0), ("YTOK", 1), C.ident_b], writes=[B[7]])
            mk.op("act", lambda e: e.activation(out=YTT[:], in_=bfv(B[7]), func=AF.Identity), reads=[B[7]], writes=[YTT])
            mk.dma("sp", io.YATT.ap()[:, blk * 128:(blk + 1) * 128].rearrange("(kc p) t -> p kc t", p=128), YTT[:])


def wrap_turns(mk, t, tmpi, tmpf):
    mk.op("dve", lambda e: e.tensor_copy(out=tmpi, in_=t), reads=[t], writes=[tmpi])
    mk.op("dve", lambda e: e.tensor_copy(out=tmpf, in_=tmpi), reads=[tmpi], writes=[tmpf])
    mk.op("dve", lambda e: e.tensor_tensor(out=t, in0=t, in1=tmpf, op=ALU.subtract), reads=[t, tmpf], writes=[t])
    mk.op("dve", lambda e: e.tensor_scalar(out=tmpf, in0=t, scalar1=0.5, scalar2=None, op0=ALU.is_gt), reads=[t], writes=[tmpf])
    mk.op("dve", lambda e: e.tensor_tensor(out=t, in0=t, in1=tmpf, op=ALU.subtract), reads=[t, tmpf], writes=[t])
    mk.op("dve", lambda e: e.tensor_scalar(out=tmpf, in0=t, scalar1=-0.5, scalar2=None, op0=ALU.is_lt), reads=[t], writes=[tmpf])
    mk.op("dve", lambda e: e.tensor_tensor(out=t, in0=t, in1=tmpf, op=ALU.add), reads=[t, tmpf], writes=[t])


def sincos_turns(mk, t, tmpi, tmpf, t2, out_sin, out_cos):
    mk.op("dve", lambda e: e.tensor_scalar(out=t2, in0=t, scalar1=0.25, scalar2=None, op0=ALU.add), reads=[t], writes=[t2])
    wrap_turns(mk, t, tmpi, tmpf)
    mk.op("act", lambda e: e.activation(out=out_sin, in_=t, func=AF.Sin, scale=TWO_PI), reads=[t], writes=[out_sin])
    wrap_turns(mk, t2, tmpi, tmpf)
    mk.op("act", lambda e: e.activation(out=out_cos, in_=t2, func=AF.Sin, scale=TWO_PI), reads=[t2], writes=[out_cos])


def cmul(mk, outr, outi, ar, ai, br, bi, t1, t2):
    rd = [ar, ai, br, bi]
    mk.op("dve", lambda e: e.tensor_tensor(out=t1, in0=ar, in1=br, op=ALU.mult), reads=rd, writes=[t1])
    mk.op("dve", lambda e: e.tensor_tensor(out=t2, in0=ai, in1=bi, op=ALU.mult), reads=rd, writes=[t2])
    mk.op("dve", lambda e: e.tensor_tensor(out=outr, in0=t1, in1=t2, op=ALU.subtract), reads=[t1, t2], writes=[outr])
    mk.op("dve", lambda e: e.tensor_tensor(out=t1, in0=ar, in1=bi, op=ALU.mult), reads=rd + [outr], writes=[t1])
    mk.op("dve", lambda e: e.tensor_tensor(out=t2, in0=ai, in1=br, op=ALU.mult), reads=rd + [outr], writes=[t2])
    mk.op("dve", lambda e: e.tensor_tensor(out=outi, in0=t1, in1=t2, op=ALU.add), reads=[t1, t2], writes=[outi])


def phase_s5(mk, io, C, src):
    li = 0
    G1, SH1 = C.G1[li], C.MOD[li][:, 0:16]
    NBLK = NTOK // 1024
    NSB = NTOK // 256
    with mk.scope():
        E1R = mk.sb("E1R", [128, 64, 32]); E1I = mk.sb("E1I", [128, 64, 32])
        E2R = mk.sb("E2R", [128, 64, 32]); E2I = mk.sb("E2I", [128, 64, 32])
        RHO0 = mk.sb("RHO0", [128, 64, 32]); RHO = mk.sb("RHO", [128, 64])
        E32R = mk.sb("E32R", [128, 64]); E32I = mk.sb("E32I", [128, 64])
        JM = mk.sb("JM", [128, 128]); W0 = mk.sb("W0", [128, 64]); PMK = mk.sb("PMK5", [128, 1])
        mk.dma("sp", JM[:], io.jmat.ap())
        mk.dma("sp", PMK[:], io.pmask.ap())
        with mk.scope():
            LR = mk.sb("LR", [128, 64]); LI = mk.sb("LI", [128, 64]); DTv = mk.sb("DTv", [128, 64])
            Av = mk.sb("Av", [128, 64]); F0 = mk.sb("F0", [128, 64]); F8 = mk.sb("F8", [128, 64])
            KV = mk.sb("KV", [128, 17]); MV = mk.sb("MV", [128, 64]); CMK = mk.sb("CMK", [128, 128])
            MAG = mk.sb("MAG", [128, 17, 64]); TT = mk.sb("TTp", [128, 17, 64]); TT2 = mk.sb("TT2p", [128, 17, 64])
            TTI = mk.sb("TTIp", [128, 17, 64], I32); TTF = mk.sb("TTFp", [128, 17, 64])
            PR = mk.sb("PRp", [128, 17, 64]); PI = mk.sb("PIp", [128, 17, 64])
            s64 = {n: mk.sb(n, [128, 64]) for n in ("ca", "cb", "den", "cr", "ci", "t1", "t2", "t3")}
            s64i = mk.sb("s64i", [128, 64], I32)
            BR = mk.sb("BRs", [128, 64, 16]); BI = mk.sb("BIs", [128, 64, 16])
            CR = mk.sb("CRs", [128, 64, 16]); CI = mk.sb("CIs", [128, 64, 16])
            BBR = mk.sb("BBR", [128, 64, 16]); BBI = mk.sb("BBI", [128, 64, 16])
            X1 = mk.sb("X1s", [128, 64, 16]); X2 = mk.sb("X2s", [128, 64, 16])
            for t, s in ((LR, io.s5_lam_re), (LI, io.s5_lam_im), (DTv, io.s5_logdt), (KV, io.kvals), (MV, io.mvals),
                         (CMK, io.cmask), (BR, io.s5_b_re), (BI, io.s5_b_im), (CR, io.s5_c_re), (CI, io.s5_c_im)):
                mk.dma("sp", t[:], s.ap())
            mk.op("act", lambda e: e.activation(out=DTv[:], in_=DTv[:], func=AF.Exp), reads=[DTv], writes=[DTv])
            mk.op("dve", lambda e: e.tensor_tensor(out=Av[:], in0=DTv[:], in1=LR[:], op=ALU.mult), reads=[DTv, LR], writes=[Av])
            mk.op("dve", lambda e: e.tensor_tensor(out=F0[:], in0=DTv[:], in1=LI[:], op=ALU.mult), reads=[DTv, LI], writes=[F0])
            mk.op("dve", lambda e: e.tensor_scalar(out=F0[:], in0=F0[:], scalar1=1.0 / TWO_PI, scalar2=None, op0=ALU.mult),
                  reads=[F0], writes=[F0])
            wrap_turns(mk, F0[:], s64i[:], s64["t1"][:])
            mk.op("dve", lambda e: e.tensor_scalar(out=F8[:], in0=F0[:], scalar1=8.0, scalar2=None, op0=ALU.mult), reads=[F0], writes=[F8])
            wrap_turns(mk, F8[:], s64i[:], s64["t1"][:])
            kvb = KV[:].unsqueeze(2).to_broadcast([128, 17, 64])
            mk.op("dve", lambda e: e.tensor_tensor(out=MAG[:], in0=kvb, in1=Av[:].unsqueeze(1).to_broadcast([128, 17, 64]), op=ALU.mult),
                  reads=[KV, Av], writes=[MAG])
            mk.op("act", lambda e: e.activation(out=MAG[:], in_=MAG[:], func=AF.Exp), reads=[MAG], writes=[MAG])
            mk.op("dve", lambda e: e.tensor_tensor(out=TT[:], in0=kvb, in1=F0[:].unsqueeze(1).to_broadcast([128, 17, 64]), op=ALU.mult),
                  reads=[KV, F0], writes=[TT])
            sincos_turns(mk, TT[:], TTI[:], TTF[:], TT2[:], PI[:], PR[:])
            mk.op("dve", lambda e: e.tensor_tensor(out=PR[:], in0=PR[:], in1=MAG[:], op=ALU.mult), reads=[PR, MAG], writes=[PR])
            mk.op("dve", lambda e: e.tensor_tensor(out=PI[:], in0=PI[:], in1=MAG[:], op=ALU.mult), reads=[PI, MAG], writes=[PI])
            mk.op("dve", lambda e: e.tensor_copy(out=RHO[:], in_=MAG[:, 16, :]), reads=[MAG], writes=[RHO])
            mk.op("dve", lambda e: e.tensor_copy(out=RHO0[:], in_=RHO[:].unsqueeze(2).to_broadcast([128, 64, 32])), reads=[RHO], writes=[RHO0])
            mk.op("dve", lambda e: e.memset(RHO0[:, :, 0:1], 0.0), reads=[RHO0], writes=[RHO0])
            s = s64
            mk.op("dve", lambda e: e.tensor_scalar(out=s["ca"][:], in0=PR[:, 9, :], scalar1=-1.0, scalar2=None, op0=ALU.add), reads=[PR], writes=[s["ca"]])
            mk.op("dve", lambda e: e.tensor_copy(out=s["cb"][:], in_=PI[:, 9, :]), reads=[PI], writes=[s["cb"]])
            mk.op("dve", lambda e: e.tensor_tensor(out=s["den"][:], in0=LR[:], in1=LR[:], op=ALU.mult), reads=[LR], writes=[s["den"]])
            mk.op("dve", lambda e: e.tensor_tensor(out=s["t1"][:], in0=LI[:], in1=LI[:], op=ALU.mult), reads=[LI], writes=[s["t1"]])
            mk.op("dve", lambda e: e.tensor_tensor(out=s["den"][:], in0=s["den"][:], in1=s["t1"][:], op=ALU.add), reads=[s["den"], s["t1"]], writes=[s["den"]])
            mk.op("dve", lambda e: e.reciprocal(out=s["den"][:], in_=s["den"][:]), reads=[s["den"]], writes=[s["den"]])
            mk.op("dve", lambda e: e.tensor_scalar(out=s["t3"][:], in0=LI[:], scalar1=-1.0, scalar2=None, op0=ALU.mult), reads=[LI], writes=[s["t3"]])
            cmul(mk, s["cr"][:], s["ci"][:], s["ca"][:], s["cb"][:], LR[:], s["t3"][:], s["t1"][:], s["t2"][:])
            mk.op("dve", lambda e: e.tensor_tensor(out=s["cr"][:], in0=s["cr"][:], in1=s["den"][:], op=ALU.mult), reads=[s["cr"], s["den"]], writes=[s["cr"]])
            mk.op("dve", lambda e: e.tensor_tensor(out=s["ci"][:], in0=s["ci"][:], in1=s["den"][:], op=ALU.mult), reads=[s["ci"], s["den"]], writes=[s["ci"]])
            crb = s["cr"][:].unsqueeze(2).to_broadcast([128, 64, 16])
            cib = s["ci"][:].unsqueeze(2).to_broadcast([128, 64, 16])
            cmul(mk, BBR[:], BBI[:], crb, cib, BR[:], BI[:], X1[:], X2[:])
            with mk.scope():
                ET = mk.sb("ETs", [128, 64, 32]); ET2 = mk.sb("ET2s", [128, 64, 32]); ETF = mk.sb("ETFs", [128, 64, 32])
                ETI = mk.sb("ETIs", [128, 64, 32], I32)
                for (m0, ER_, EI_) in ((0, E1R, E1I), (32, E2R, E2I)):
                    mk.op("dve", lambda e, m0=m0: e.tensor_tensor(out=ET[:], in0=F8[:].unsqueeze(2).to_broadcast([128, 64, 32]),
                                                                  in1=MV[:, m0:m0 + 32].unsqueeze(1).to_broadcast([128, 64, 32]), op=ALU.mult),
                          reads=[F8, MV], writes=[ET])
                    sincos_turns(mk, ET[:], ETI[:], ETF[:], ET2[:], EI_[:], ER_[:])
            mk.op("dve", lambda e: e.tensor_scalar(out=s["t3"][:], in0=F8[:], scalar1=32.0, scalar2=None, op0=ALU.mult), reads=[F8], writes=[s["t3"]])
            sincos_turns(mk, s["t3"][:], s64i[:], s["t1"][:], s["t2"][:], E32I[:], E32R[:])
            sh = [128, 8, 8, 16]
            names = ("UR", "UI", "US", "VR", "VI", "CLR", "CLI", "WR", "WI", "WA", "WB", "Q1", "Q2")
            T = {n: mk.sb("s5" + n, sh) for n in names}
            OUTM = [mk.sb(f"OUTM{j}", [128, 8, 128], BF16) for j in range(4)]
            PB_ = [mk.ps(f"PBs{j}", [128, 512], F32) for j in range(3)]
            for gb in range(8):
                gs = slice(gb * 8, gb * 8 + 8)

                def pw(tab, k0):
                    return tab[:, k0:k0 + 8, gs].rearrange("p r g -> p g r").unsqueeze(3).to_broadcast(sh)

                def bq(tab):
                    return tab[:, gs, :].unsqueeze(2).to_broadcast(sh)
                cmul(mk, T["UR"][:], T["UI"][:], pw(PR, 0), pw(PI, 0), bq(BBR), bq(BBI), T["Q1"][:], T["Q2"][:])
                cmul(mk, T["VR"][:], T["VI"][:], bq(CR), bq(CI), pw(PR, 8), pw(PI, 8), T["Q1"][:], T["Q2"][:])
                cmul(mk, T["CLR"][:], T["CLI"][:], bq(CR), bq(CI), pw(PR, 9), pw(PI, 9), T["Q1"][:], T["Q2"][:])
                p7r = PR[:, 15, gs].unsqueeze(2).unsqueeze(3).to_broadcast(sh)
                p7i = PI[:, 15, gs].unsqueeze(2).unsqueeze(3).to_broadcast(sh)
                cmul(mk, T["WR"][:], T["WI"][:], p7r, p7i, T["UR"][:], T["UI"][:], T["Q1"][:], T["Q2"][:])

                def stack(dst, top, bot, neg_top=False, neg_bot=False):
                    for (ps_, srcT, ng) in ((slice(0, 64), top, neg_top), (slice(64, 128), bot, neg_bot)):
                        if ng:
                            mk.op("dve", lambda e, ps_=ps_, srcT=srcT: e.tensor_scalar(out=dst[ps_], in0=srcT[ps_], scalar1=-1.0,
                                                                                     scalar2=None, op0=ALU.mult),
                                  reads=[srcT], writes=[dst])
                        else:
                            mk.op("dve", lambda e, ps_=ps_, srcT=srcT: e.tensor_copy(out=dst[ps_], in_=srcT[ps_]), reads=[srcT], writes=[dst])
                stack(T["US"], T["UR"], T["UI"])
                stack(T["VR"], T["VR"], T["VI"], neg_bot=True)
                stack(T["CLR"], T["CLR"], T["CLI"], neg_bot=True)
                stack(T["WA"], T["WR"], T["WI"])
                stack(T["WB"], T["WI"], T["WR"], neg_top=True)
                mk.op("act", lambda e: e.activation(out=OUTM[1][:], in_=T["CLR"][:].rearrange("p g r q -> p g (r q)"), func=AF.Identity),
                      reads=[T["CLR"]], writes=[OUTM[1]])
                for g4 in range(2):
                    def fk(e, g4=g4):
                        ins = None
                        for i in range(4):
                            g = g4 * 4 + i
                            us = T["US"][:, g].rearrange("p r q -> p (r q)")
                            e.matmul(PB_[0][:, i * 128:(i + 1) * 128], us, T["VR"][:, g].rearrange("p r q -> p (r q)"), start=True, stop=True)
                            e.matmul(PB_[1][:, i * 128:(i + 1) * 128], T["WA"][:, g].rearrange("p r q -> p (r q)"), C.ident_f[:], start=True, stop=True)
                            ins = e.matmul(PB_[2][:, i * 128:(i + 1) * 128], T["WB"][:, g].rearrange("p r q -> p (r q)"), C.ident_f[:], start=True, stop=True)
                        return ins
                    mk.op("pe", fk, reads=[T["US"], T["VR"], T["WA"], T["WB"], C.ident_f], writes=[PB_[0], PB_[1], PB_[2]])
                    mk.op("dve", lambda e, g4=g4: e.tensor_tensor(
                        out=OUTM[0][:, g4 * 4:(g4 + 1) * 4, :], in0=PB_[0][:].rearrange("p (a b) -> p a b", a=4),
                        in1=CMK[:].unsqueeze(1).to_broadcast([128, 4, 128]), op=ALU.mult), reads=[PB_[0], CMK], writes=[OUTM[0]])
                    mk.op("act", lambda e, g4=g4: e.activation(out=OUTM[2][:, g4 * 4:(g4 + 1) * 4, :].rearrange("p a b -> p (a b)"),
                                                                 in_=PB_[1][:], func=AF.Identity), reads=[PB_[1]], writes=[OUTM[2]])
                    mk.op("act", lambda e, g4=g4: e.activation(out=OUTM[3][:, g4 * 4:(g4 + 1) * 4, :].rearrange("p a b -> p (a b)"),
                                                                 in_=PB_[2][:], func=AF.Identity), reads=[PB_[2]], writes=[OUTM[3]])
                for x in range(4):
                    mk.dma("sp", io.S5M.ap()[x, :, gb * 1024:(gb + 1) * 1024], OUTM[x][:].rearrange("p a b -> p (a b)"))
        with mk.scope():
            WU = mk.sb("WU", [128, KC, 1024], BF16)
            HTb = mk.sb("HTb", [128, KC, 1024], BF16)
            XT = mk.sb("XT5", [128, KC, 256], F32); SQ = mk.sb("SQ5", [128, KC, 256], BF16); RS = mk.sb("RS5", [128, 256], F32)
            UCM = mk.sb("UCM", [128, 8, 1024], BF16)
            UCP = mk.sb("UCP", [128, 64, 8, 16], BF16)
            UT = mk.sb("UT", [128, 64, 128], BF16)
            PS = mk.ps("PS5", [128, 256], F32)
            PU = [mk.ps(f"PU{j}", [128, 512], F32) for j in range(2)]
            PTb = [mk.ps(f"PTb{j}", [128, 512], F32) for j in range(2)]
            mk.dma("pool", WU[:], io.w_in0.ap()[:, 0:1024].rearrange("(kc p) n -> p kc n", p=128))
            for b in range(NBLK):
                for pc in range(4):
                    norm_piece(mk, C, src, b * 1024 + pc * 256, 256, G1, SH1, XT, SQ, PS, RS, HTb[:, :, pc * 256:(pc + 1) * 256], key=HTb)
                n = 0
                for r in range(8):
                    for nh in range(2):
                        pu = PU[n % 2]
                        n += 1

                        def f(e, pu=pu, r=r, nh=nh):
                            ins = None
                            for kc in range(KC):
                                ins = e.matmul(pu[:], HTb[:, kc, r:1024:8], WU[:, kc, nh * 512:(nh + 1) * 512],
                                               start=(kc == 0), stop=(kc == KC - 1))
                            return ins
                        mk.op("pe", f, reads=[HTb, WU], writes=[pu])
                        mk.op("act", lambda e, pu=pu, r=r, nh=nh: e.activation(out=UCM[:, r, nh * 512:(nh + 1) * 512], in_=pu[:], func=AF.Identity),
                              reads=[pu], writes=[("UCM", r, nh)])
                mk.op("dve", lambda e: e.tensor_copy(out=UCP[:], in_=UCM[:].rearrange("p r (g q) -> p g r q", q=16)),
                      reads=[("UCM", r, nh) for r in range(8) for nh in range(2)], writes=[UCP])
                mk.dma("sp", io.UCM.ap()[b], UCP[:].rearrange("p g r q -> p (g r q)"))
                for g8 in range(8):
                    bank = PTb[g8 % 2]
                    pv = bank[:].bitcast(BF16).rearrange("p (a b) -> p a b", a=8)

                    def ft(e, g8=g8, pv=pv):
                        ins = None
                        for i in range(8):
                            ins = e.transpose(pv[:, i, :], UCP[:, g8 * 8 + i].rearrange("p r q -> p (r q)"), C.ident_b[:])
                        return ins
                    mk.op("pe", ft, reads=[UCP, C.ident_b], writes=[bank])
                    mk.op("act" if g8 % 2 == 0 else "dve",
                          (lambda e, g8=g8, pv=pv: e.activation(out=UT[:, g8 * 8:(g8 + 1) * 8, :], in_=pv, func=AF.Identity)) if g8 % 2 == 0
                          else (lambda e, g8=g8, pv=pv: e.tensor_copy(out=UT[:, g8 * 8:(g8 + 1) * 8, :], in_=pv)),
                          reads=[bank], writes=[("UT", g8)])
                for sb in range(4):
                    mk.dma("sp", io.UT.ap()[b * 4 + sb].rearrange("p (g c) -> p g c", c=32), UT[:, :, sb * 32:(sb + 1) * 32],
                           reads=[("UT", g8) for g8 in range(8)], writes=[io.UT.ap()[b * 4 + sb]])

        def scan_step(SLA, SLB, Z, TZ, OUT, need_S, WSH=None, SBF=None, PJ=None, O31=None, PJ1=None, t64=None):
            mk.op("dve", lambda e: e.tensor_tensor(out=Z[:], in0=E1R[:], in1=SLA[:], op=ALU.mult), reads=[E1R, SLA], writes=[Z])
            mk.op("dve", lambda e: e.tensor_tensor(out=TZ[:], in0=E1I[:], in1=SLB[:], op=ALU.mult), reads=[E1I, SLB], writes=[TZ])
            mk.op("dve", lambda e: e.tensor_tensor(out=Z[:], in0=Z[:], in1=TZ[:], op=ALU.add), reads=[Z, TZ], writes=[Z])
            mk.op("dve", lambda e: e.tensor_tensor(out=t64[:], in0=RHO[:], in1=W0[:], op=ALU.mult), reads=[RHO, W0], writes=[t64])
            mk.op("dve", lambda e: e.tensor_tensor(out=Z[:, :, 0], in0=Z[:, :, 0], in1=t64[:], op=ALU.add), reads=[Z, t64], writes=[Z])
            mk.op("dve", lambda e: e.tensor_tensor_scan(out=OUT[:].rearrange("p g c -> p (g c)"), data0=RHO0[:].rearrange("p g c -> p (g c)"),
                                                        data1=Z[:].rearrange("p g c -> p (g c)"), initial=0.0, op0=ALU.mult, op1=ALU.add),
                  reads=[RHO0, Z], writes=[OUT])
            if need_S:
                mk.op("dve", lambda e: e.tensor_copy(out=WSH[:, :, 0], in_=W0[:]), reads=[W0], writes=[("WSH", 0)])
                mk.op("dve", lambda e: e.tensor_copy(out=WSH[:, :, 1:32], in_=OUT[:, :, 0:31]), reads=[OUT], writes=[("WSH", 1)])
                for q in range(4):
                    mk.op("pe", lambda e, q=q: e.matmul(PJ[q][:], JM[:], WSH[:, q * 16:(q + 1) * 16, :].rearrange("p g c -> p (g c)"),
                                                        start=True, stop=True), reads=[JM, ("WSH", 0), ("WSH", 1)], writes=[PJ[q]])
                    mk.op("dve", lambda e, q=q: e.tensor_tensor(out=TZ[:, q * 16:(q + 1) * 16, :].rearrange("p g c -> p (g c)"), in0=PJ[q][:],
                                                                in1=E2I[:, q * 16:(q + 1) * 16, :].rearrange("p g c -> p (g c)"), op=ALU.mult),
                          reads=[PJ[q], E2I], writes=[("TZ", q)])
                mk.op("dve", lambda e: e.tensor_tensor(out=Z[:], in0=E2R[:], in1=WSH[:], op=ALU.mult),
                      reads=[E2R, ("WSH", 0), ("WSH", 1), Z], writes=[Z])
                mk.op("dve", lambda e: e.tensor_tensor(out=SBF[:], in0=Z[:], in1=TZ[:], op=ALU.add),
                      reads=[Z] + [("TZ", q) for q in range(4)] + [TZ], writes=[SBF])
            mk.op("dve", lambda e: e.tensor_copy(out=O31[:], in_=OUT[:, :, 31]), reads=[OUT], writes=[O31])
            mk.op("pe", lambda e: e.matmul(PJ1[:, 0:64], JM[:], O31[:], start=True, stop=True), reads=[JM, O31], writes=[PJ1])
            mk.op("dve", lambda e: e.tensor_tensor(out=t64[:], in0=PJ1[:, 0:64], in1=E32I[:], op=ALU.mult), reads=[PJ1, E32I], writes=[t64])
            mk.op("dve", lambda e: e.tensor_tensor(out=W0[:], in0=O31[:], in1=E32R[:], op=ALU.mult), reads=[O31, E32R], writes=[W0])
            mk.op("dve", lambda e: e.tensor_tensor(out=W0[:], in0=W0[:], in1=t64[:], op=ALU.add), reads=[W0, t64], writes=[W0])

        with mk.scope():
            BLA = mk.sb("BLA", [128, 64, 128], BF16); BLB = mk.sb("BLB", [128, 64, 128], BF16)
            UTs = [mk.sb(f"UTs{j}", [128, 64, 32], BF16) for j in range(2)]
            SLA = mk.sb("SLA", [128, 64, 32]); SLB = mk.sb("SLB", [128, 64, 32])
            Z = mk.sb("Zs", [128, 64, 32]); TZ = mk.sb("TZs", [128, 64, 32]); OUT = mk.sb("OUTs5", [128, 64, 32])
            O31 = mk.sb("O31", [128, 64]); t64 = mk.sb("t64", [128, 64])
            PA_ = [mk.ps(f"PA5{j}", [128, 512], F32) for j in range(2)]
            PB2 = [mk.ps(f"PB5{j}", [128, 512], F32) for j in range(2)]
            PJ1 = mk.ps("PJ1", [128, 512], F32)
            mk.dma("sp", BLA[:].rearrange("p a b -> p (a b)"), io.S5M.ap()[2])
            mk.dma("sp", BLB[:].rearrange("p a b -> p (a b)"), io.S5M.ap()[3])
            mk.op("pool", lambda e: e.memset(W0[:], 0.0), writes=[W0])
            for sbi in range(NSB):
                uts = UTs[sbi % 2]
                mk.dma("sp", uts[:].rearrange("p g c -> p (g c)"), io.UT.ap()[sbi])
                for gq in range(4):
                    pa, pb = PA_[gq % 2], PB2[gq % 2]

                    def f1(e, gq=gq, pa=pa, pb=pb, uts=uts):
                        ins = None
                        for i in range(16):
                            g = gq * 16 + i
                            e.matmul(pa[:, i * 32:(i + 1) * 32], BLA[:, g, :], uts[:, g, :], start=True, stop=True)
                            ins = e.matmul(pb[:, i * 32:(i + 1) * 32], BLB[:, g, :], uts[:, g, :], start=True, stop=True)
                        return ins
                    mk.op("pe", f1, reads=[BLA, BLB, uts], writes=[pa, pb])
                    mk.op("act", lambda e, gq=gq, pa=pa: e.activation(out=SLA[:, gq * 16:(gq + 1) * 16, :].rearrange("p g c -> p (g c)"),
                                                                       in_=pa[:], func=AF.Identity), reads=[pa], writes=[("SLA", gq)])
                    mk.op("dve", lambda e, gq=gq, pb=pb: e.tensor_copy(out=SLB[:, gq * 16:(gq + 1) * 16, :].rearrange("p g c -> p (g c)"), in_=pb[:]),
                          reads=[pb], writes=[("SLB", gq)])
                mk.dma("sp", io.SL.ap()[sbi, 0], SLA[:].rearrange("p g c -> p (g c)"), reads=[("SLA", q) for q in range(4)], writes=[io.SL.ap()[sbi, 0]])
                mk.dma("sp", io.SL.ap()[sbi, 1], SLB[:].rearrange("p g c -> p (g c)"), reads=[("SLB", q) for q in range(4)], writes=[io.SL.ap()[sbi, 1]])
                mk.op("dve", lambda e: e.tensor_copy(out=t64[:], in_=W0[:]),
                      reads=[("SLA", q) for q in range(4)] + [("SLB", q) for q in range(4)] + [W0], writes=[t64, SLA, SLB])
                scan_step(SLA, SLB, Z, TZ, OUT, False, O31=O31, PJ1=PJ1, t64=t64)
            mk.dma("sp", io.s5_src.ap(), W0[:])
            mk.cc_allgather(io.s5_dst, io.s5_src, PAIRS)
            mk.dma("sp", W0[:], io.s5_dst.ap()[0:128, :])
            mk.op("dve", lambda e: e.tensor_scalar(out=W0[:], in0=W0[:], scalar1=PMK[:, 0:1], scalar2=None, op0=ALU.mult),
                  reads=[W0, PMK], writes=[W0])
        with mk.scope():
            KM = mk.sb("KM", [128, 64, 128], BF16); CL = mk.sb("CLm", [128, 64, 128], BF16)
            UTs = [mk.sb(f"UTt{j}", [128, 64, 32], BF16) for j in range(2)]
            SLAs = [mk.sb(f"SLAt{j}", [128, 64, 32]) for j in range(2)]
            SLBs = [mk.sb(f"SLBt{j}", [128, 64, 32]) for j in range(2)]
            Z = mk.sb("Zt", [128, 64, 32]); TZ = mk.sb("TZt", [128, 64, 32]); OUT = mk.sb("OUTt", [128, 64, 32])
            WSH = mk.sb("WSH", [128, 64, 32]); SBF = mk.sb("SBF", [128, 64, 32], BF16)
            YGB = mk.sb("YGB", [128, 64, 128], BF16)
            O31 = mk.sb("O31t", [128, 64]); t64 = mk.sb("t64t", [128, 64])
            PJ = [mk.ps(f"PJ{j}", [128, 512], F32) for j in range(4)]
            PY = [mk.ps(f"PY5{j}", [128, 512], F32) for j in range(2)]
            PJ1 = mk.ps("PJ1t", [128, 512], F32)
            mk.dma("sp", KM[:].rearrange("p a b -> p (a b)"), io.S5M.ap()[0])
            mk.dma("sp", CL[:].rearrange("p a b -> p (a b)"), io.S5M.ap()[1])
            for sbi in range(NSB):
                uts, sla, slb = UTs[sbi % 2], SLAs[sbi % 2], SLBs[sbi % 2]
                mk.dma("sp", uts[:].rearrange("p g c -> p (g c)"), io.UT.ap()[sbi])
                mk.dma("sp", sla[:].rearrange("p g c -> p (g c)"), io.SL.ap()[sbi, 0])
                mk.dma("sp", slb[:].rearrange("p g c -> p (g c)"), io.SL.ap()[sbi, 1])
                scan_step(sla, slb, Z, TZ, OUT, True, WSH=WSH, SBF=SBF, PJ=PJ, O31=O31, PJ1=PJ1, t64=t64)
                sb = sbi % 4
                for gq in range(4):
                    py = PY[gq % 2]

                    def f2(e, gq=gq, py=py, uts=uts):
                        ins = None
                        for i in range(16):
                            g = gq * 16 + i
                            e.matmul(py[:, i * 32:(i + 1) * 32], KM[:, g, :], uts[:, g, :], start=True, stop=False)
                            ins = e.matmul(py[:, i * 32:(i + 1) * 32], CL[:, g, :], SBF[:, g, :], start=False, stop=True)
                        return ins
                    mk.op("pe", f2, reads=[KM, CL, uts, SBF], writes=[py])
                    mk.op("act", lambda e, gq=gq, py=py, sb=sb: e.activation(
                        out=YGB[:, gq * 16:(gq + 1) * 16, sb * 32:(sb + 1) * 32], in_=py[:].rearrange("p (g c) -> p g c", c=32), func=AF.Identity),
                        reads=[py], writes=[("YGB", gq)])
                if sb == 3:
                    b = sbi // 4
                    mk.dma("sp", io.YGB.ap()[b], YGB[:].rearrange("p a b -> p (a b)"), reads=[("YGB", q) for q in range(4)], writes=[io.YGB.ap()[b]])
                    mk.op("act", lambda e: e.activation(out=t64[:, 0:1], in_=t64[:, 0:1], func=AF.Identity),
                          reads=[t64, io.YGB.ap()[b]], writes=[t64] + [("YGB", q) for q in range(4)])
        with mk.scope():
            YGB = mk.sb("YGBl", [128, 64, 128], BF16)
            UCP = mk.sb("UCPl", [128, 64, 8, 16], BF16)
            DBC = mk.sb("DBC", [128, 64, 16], F32)
            Y32 = [mk.sb(f"Y32{j}", [128, 16, 8, 16]) for j in range(2)]
            TU = [mk.sb(f"TU{j}", [128, 16, 8, 16]) for j in range(2)]
            X2_ = [mk.sb(f"X2g{j}", [128, 16, 8, 16]) for j in range(2)]
            YRM = mk.sb("YRM", [128, 8, 1024], BF16)
            YT = mk.sb("YT5", [128, 8, 1024], BF16)
            PTb = [mk.ps(f"PTc{j}", [128, 512], F32) for j in range(4)]
            mk.dma("sp", DBC[:].rearrange("p g q -> p (g q)"), io.s5_d.ap())
            for b in range(NBLK):
                mk.dma("sp", YGB[:].rearrange("p a b -> p (a b)"), io.YGB.ap()[b])
                mk.dma("sp", UCP[:].rearrange("p g r q -> p (g r q)"), io.UCM.ap()[b])
                for gb in range(4):
                    y32, tu, x2 = Y32[gb % 2], TU[gb % 2], X2_[gb % 2]
                    for hh in range(2):
                        bank = PTb[(gb * 2 + hh) % 4]
                        pv = bank[:].bitcast(BF16).rearrange("p (a b) -> p a b", a=8)

                        def fbt(e, gb=gb, hh=hh, pv=pv):
                            ins = None
                            for i in range(8):
                                ins = e.transpose(pv[:, i, :], YGB[:, gb * 16 + hh * 8 + i, :], C.ident_b[:])
                            return ins
                        mk.op("pe", fbt, reads=[YGB, C.ident_b], writes=[bank])
                        mk.op("act", lambda e, hh=hh, pv=pv, y32=y32: e.activation(
                            out=y32[:, hh * 8:(hh + 1) * 8].rearrange("p g r q -> p g (r q)"), in_=pv, func=AF.Identity),
                            reads=[bank], writes=[(y32.name, hh)])
                    gs = slice(gb * 16, gb * 16 + 16)
                    mk.op("dve", lambda e, tu=tu, gs=gs: e.tensor_tensor(out=tu[:], in0=UCP[:, gs], in1=DBC[:, gs, :].unsqueeze(2).to_broadcast([128, 16, 8, 16]),
                                                                         op=ALU.mult), reads=[UCP, DBC], writes=[tu])
                    mk.op("dve", lambda e, tu=tu, y32=y32: e.tensor_tensor(out=y32[:], in0=y32[:], in1=tu[:], op=ALU.add),
                          reads=[(y32.name, 0), (y32.name, 1), tu], writes=[y32])
                    mk.op("dve", lambda e, x2=x2, y32=y32: e.tensor_tensor(out=x2[:], in0=y32[:], in1=y32[:], op=ALU.mult), reads=[y32], writes=[x2])
                    mk.op("dve", lambda e, x2=x2: e.tensor_scalar(out=x2[:], in0=x2[:], scalar1=0.044715, scalar2=1.0, op0=ALU.mult, op1=ALU.add),
                          reads=[x2], writes=[x2])
                    mk.op("dve", lambda e, x2=x2, y32=y32: e.tensor_tensor(out=x2[:], in0=x2[:], in1=y32[:], op=ALU.mult), reads=[x2, y32], writes=[x2])
                    mk.op("act", lambda e, x2=x2: e.activation(out=x2[:], in_=x2[:], func=AF.Sigmoid, scale=1.5957691216057308), reads=[x2], writes=[x2])
                    mk.op("dve", lambda e, x2=x2, y32=y32, gb=gb: e.tensor_tensor(
                        out=YRM[:, :, gb * 256:(gb + 1) * 256].rearrange("p r (g q) -> p g r q", q=16), in0=y32[:], in1=x2[:], op=ALU.mult),
                        reads=[y32, x2], writes=[("YRM", gb)])
                for kc in range(8):
                    bank = PTb[kc % 4]
                    pv = bank[:].bitcast(BF16).rearrange("p (a b) -> p a b", a=8)

                    def ffin(e, kc=kc, pv=pv):
                        ins = None
                        for r in range(8):
                            ins = e.transpose(pv[:, r, :], YRM[:, r, kc * 128:(kc + 1) * 128], C.ident_b[:])
                        return ins
                    mk.op("pe", ffin, reads=[("YRM", kc // 2), C.ident_b], writes=[bank])
                    mk.op("act" if kc % 2 == 0 else "dve",
                          (lambda e, kc=kc, pv=pv: e.activation(out=YT[:, kc, :].rearrange("p (c r) -> p r c", r=8), in_=pv, func=AF.Identity))
                          if kc % 2 == 0 else
                          (lambda e, kc=kc, pv=pv: e.tensor_copy(out=YT[:, kc, :].rearrange("p (c r) -> p r c", r=8), in_=pv)),
                          reads=[bank], writes=[("YT", kc)])
                mk.dma("sp", io.YACT.ap()[:, b * 1024:(b + 1) * 1024].rearrange("(kc p) t -> p kc t", p=128), YT[:],
                       reads=[("YT", kc) for kc in range(8)], writes=[io.YACT.ap()[:, b * 1024:(b + 1) * 1024]])
                mk.op("act", lambda e: e.activation(out=DBC[:, 0, 0:1], in_=DBC[:, 0, 0:1], func=AF.Identity),
                      reads=[DBC, io.YACT.ap()[:, b * 1024:(b + 1) * 1024]], writes=[DBC] + [("YT", kc) for kc in range(8)])


def phase_mix0_out(mk, io, C, src, dst):
    li = 0
    g1 = C.MOD[li][:, 32:48]
    with mk.scope():
        WGL = mk.sb("WGL", [128, 8, 2048], BF16)
        WO = mk.sb("WO0", [128, KC, D], BF16)
        YA = [mk.sb(f"YAo{j}", [128, 8, 512], BF16) for j in range(2)]
        YAT = [mk.sb(f"YATo{j}", [128, 8, 512], BF16) for j in range(2)]
        YS = mk.sb("YSo", [128, 8, 512], BF16)
        SGT = [mk.sb(f"SGTo{j}", [128, 512], F32) for j in range(2)]
        PG = [mk.ps(f"PGo{j}", [128, 512], F32) for j in range(2)]
        PV_ = [mk.ps(f"PVo{j}", [128, 512], F32) for j in range(2)]
        PO = [mk.ps(f"POo{j}", [128, 512], F32) for j in range(2)]
        mk.dma("pool", WGL[:], io.w_glu.ap().rearrange("(kc p) n -> p kc n", p=128))
        for h in range(2):
            mk.dma("pool", WO[:, :, h * 1024:(h + 1) * 1024], io.w_out0.ap()[:, h * 1024:(h + 1) * 1024].rearrange("(kc p) n -> p kc n", p=128),
                   writes=[("WO", h)])
        XR = [mk.sb(f"XRo4{j}", [128, 512], F32) for j in range(4)]
        its = [(t, dc) for t in range(NTOK // 512) for dc in range(KC)]

        def ld_y(t):
            mk.dma("sp", YA[t % 2][:], io.YACT.ap()[:, t * 512:(t + 1) * 512].rearrange("(kc p) t -> p kc t", p=128))
            mk.dma("sp", YAT[t % 2][:], io.YATT.ap()[:, t * 512:(t + 1) * 512].rearrange("(kc p) t -> p kc t", p=128))

        def ld_x(n):
            t, dc = its[n]
            mk.dma("sp", XR[n % 4][:], src.ap()[dc * 128:(dc + 1) * 128, t * 512:(t + 1) * 512])
        ld_y(0)
        ld_x(0)
        ld_x(1)
        n = 0
        for t in range(NTOK // 512):
            ya, yat = YA[t % 2], YAT[t % 2]
            if t + 1 < NTOK // 512:
                ld_y(t + 1)
            for j in range(8):
                pg, pv, sg = PG[j % 2], PV_[j % 2], SGT[j % 2]

                def fgl(e, pg=pg, pv=pv, j=j, ya=ya):
                    ins = None
                    for kc in range(8):
                        e.matmul(pg[:], WGL[:, kc, 1024 + j * 128:1024 + (j + 1) * 128], ya[:, kc, :], start=(kc == 0), stop=(kc == 7))
                    for kc in range(8):
                        ins = e.matmul(pv[:], WGL[:, kc, j * 128:(j + 1) * 128], ya[:, kc, :], start=(kc == 0), stop=(kc == 7))
                    return ins
                mk.op("pe", fgl, reads=[WGL, ya], writes=[pg, pv])
                mk.op("act", lambda e, sg=sg, pg=pg: e.activation(out=sg[:], in_=pg[:], func=AF.Sigmoid), reads=[pg], writes=[sg])
                mk.op("dve", lambda e, sg=sg, pv=pv, j=j: e.tensor_tensor(out=YS[:, j, :], in0=pv[:], in1=sg[:], op=ALU.mult),
                      reads=[pv, sg], writes=[("YS", j)])
            for dc in range(KC):
                po = PO[dc % 2]
                xr = XR[n % 4]
                if n + 2 < len(its):
                    ld_x(n + 2)
                n += 1

                def fo(e, po=po, dc=dc, yat=yat):
                    ins = None
                    for kc in range(KC):
                        rhs = YS[:, kc, :] if kc < 8 else yat[:, kc - 8, :]
                        ins = e.matmul(po[:], WO[:, kc, dc * 128:(dc + 1) * 128], rhs, start=(kc == 0), stop=(kc == KC - 1))
                    return ins
                mk.op("pe", fo, reads=[("WO", 0), ("WO", 1), yat] + [("YS", j) for j in range(8)], writes=[po])
                mk.op("dve", lambda e, xr=xr, po=po, dc=dc: e.scalar_tensor_tensor(
                    out=xr[:], in0=po[:], scalar=g1[:, dc:dc + 1], in1=xr[:], op0=ALU.mult, op1=ALU.add),
                    reads=[po, xr, C.MOD[li]], writes=[xr])
                mk.dma("sp", dst.ap()[dc * 128:(dc + 1) * 128, t * 512:(t + 1) * 512], xr[:])


def phase_mix0(mk, io, C, src, dst, parts=("s5", "attn", "out")):
    if "s5" in parts:
        phase_s5(mk, io, C, src)
    if "attn" in parts:
        phase_attn(mk, io, C, src)
    if "out" in parts:
        phase_mix0_out(mk, io, C, src, dst)
```
